# Optimizing a Trainium2 kernel written in Bass

```python
import math
import jax, jax.numpy as jnp
from jax import lax
import numpy as np

D_MODEL = 1024
BATCH = 8
SEQ = 8192
DEPTH = 1

GRID_W = 64
CTX_LEN = 256
CHUNK = 64

HG_HEADS = 8
HG_DK = 128
HG_DV = 128
HG_WIDTH = HG_HEADS * HG_DK

MA_INNER = 2 * D_MODEL
MA_HEAD_DIM = 64
MA_HEADS = MA_INNER // MA_HEAD_DIM
MA_GROUPS = 8
MA_STATE = 128
MA_CONV = 5
MA_XB = MA_INNER + MA_GROUPS * MA_STATE
MA_XBC = MA_XB + MA_GROUPS * MA_STATE

PEER_HEADS = 8
PEER_NKEYS = 128
PEER_EXPERTS = PEER_NKEYS * PEER_NKEYS
PEER_TOPK = 16
PEER_DKEY = 256
PEER_BLOCK = 128

DN_ALPHA = (2.0 * DEPTH) ** 0.25
DN_BETA = (8.0 * DEPTH) ** -0.25
LN_EPS = 1e-6

STATE_SIZES = (HG_WIDTH, HG_WIDTH, HG_WIDTH, MA_XB, MA_HEADS, MA_HEADS)
READ_SIZES = (HG_WIDTH, HG_WIDTH, MA_GROUPS * MA_STATE, MA_INNER, D_MODEL, D_MODEL)
STATE_DIM = sum(STATE_SIZES)
IN_DIM = STATE_DIM + sum(READ_SIZES)

kernel_name = 'hybrid_hgrn2_ssd_peer_dit_block'


def _split(a, sizes):
    return jnp.split(a, np.cumsum(sizes)[:-1].tolist(), axis=-1)


def _flip(a):
    return jnp.flip(a, axis=1)


def layer_norm(x):
    xf = x.astype(jnp.float32)
    mu = jnp.mean(xf, -1, keepdims=True)
    var = jnp.mean(jnp.square(xf - mu), -1, keepdims=True)
    return ((xf - mu) * lax.rsqrt(var + LN_EPS)).astype(x.dtype)


def rms_norm(x, w):
    xf = x.astype(jnp.float32)
    return xf * lax.rsqrt(jnp.mean(xf * xf, -1, keepdims=True) + LN_EPS) * w.astype(jnp.float32)


def modulate(x, shift, scale):
    return layer_norm(x) * (1 + scale) + shift


def post_ln(z, g, b):
    return layer_norm(z) * g + b


def to_col_major(a, rows):
    b = a.shape[0]
    return a.reshape(b, rows, GRID_W, *a.shape[2:]).swapaxes(1, 2).reshape(a.shape)


def to_row_major(a, rows):
    b = a.shape[0]
    return a.reshape(b, GRID_W, rows, *a.shape[2:]).swapaxes(1, 2).reshape(a.shape)


def centred_dwconv(a, w, bias, rows):
    b, t, ch = a.shape
    seqs = a if rows is None else a.reshape(b * GRID_W, rows, ch)
    pad = MA_CONV // 2
    y = lax.conv_general_dilated(seqs, w.T[:, None, :].astype(a.dtype), (1,), [(pad, pad)],
                                 dimension_numbers=('NWC', 'WIO', 'NWC'), feature_group_count=ch)
    return (y + bias.astype(a.dtype)).reshape(b, t, ch)


def hgrn2_forget(f_raw, lb):
    f = lb + (1 - lb) * jax.nn.sigmoid(f_raw.astype(jnp.float32))
    return jnp.log(f), 1 - f


def hgrn2_chunk_scan(q, k, v, logf, s0):
    b, t, h, _ = q.shape
    n = t // CHUNK
    causal = jnp.tril(jnp.ones((CHUNK, CHUNK), bool))

    def chunks(a):
        return a.reshape(b, n, CHUNK, h, a.shape[-1]).transpose(1, 0, 3, 2, 4)

    def step(s, inp):
        qc, kc, vc, gc = inp
        gcum = jnp.cumsum(gc, axis=2)
        decay = jnp.exp(jnp.where(causal[:, :, None],
                                  gcum[:, :, :, None, :] - gcum[:, :, None, :, :], -jnp.inf))
        att = jnp.einsum('bhtd,bhtsd,bhsd->bhts', qc, decay, kc)
        glast = gcum[:, :, -1:, :]
        o = (jnp.einsum('bhts,bhsv->bhtv', att, vc)
             + jnp.einsum('bhtd,bhdv->bhtv', qc * jnp.exp(gcum), s))
        s = (jnp.exp(glast[:, :, 0, :, None]) * s
             + jnp.einsum('bhsd,bhsv->bhdv', kc * jnp.exp(glast - gcum), vc))
        return s, o

    s_fin, o = lax.scan(step, s0, (chunks(q), chunks(k), chunks(v), chunks(logf)))
    return o.transpose(1, 0, 3, 2, 4).reshape(b, t, h, v.shape[-1]), s_fin


def hgrn2_final_state(k, v, logf):
    gcum = jnp.cumsum(logf, axis=1)
    return jnp.einsum('bthd,bthv->bhdv', k * jnp.exp(gcum[:, -1:] - gcum), v)


def ssd_chunk_scan(xh, dt, a, bm, cm, h0):
    b, t, h, p = xh.shape
    g, nst = bm.shape[2], bm.shape[3]
    hpg = h // g
    n = t // CHUNK
    causal = jnp.tril(jnp.ones((CHUNK, CHUNK), bool))
    xdt = (xh * dt[..., None]).reshape(b, n, CHUNK, h, p).transpose(1, 0, 2, 3, 4)
    loga = (dt * a).reshape(b, n, CHUNK, h).transpose(1, 0, 3, 2)
    bch = bm.reshape(b, n, CHUNK, g, nst).transpose(1, 0, 2, 3, 4)
    cch = cm.reshape(b, n, CHUNK, g, nst).transpose(1, 0, 2, 3, 4)

    def step(hs, inp):
        xc, lc, bc, cc = inp
        lcum = jnp.cumsum(lc, axis=-1)
        lmat = jnp.exp(jnp.where(causal, lcum[..., :, None] - lcum[..., None, :], -jnp.inf))
        lmat = lmat.reshape(b, g, hpg, CHUNK, CHUNK)
        xg = xc.reshape(b, CHUNK, g, hpg, p)
        cb = jnp.einsum('btgn,bsgn->bgts', cc, bc)
        y_intra = jnp.einsum('bgts,bgjts,bsgjp->btgjp', cb, lmat, xg)
        hg = hs.reshape(b, g, hpg, p, nst)
        y_inter = jnp.einsum('btgn,bgjpn,bgjt->btgjp', cc, hg, jnp.exp(lcum).reshape(b, g, hpg, CHUNK))
        llast = lcum[..., -1]
        dec_out = jnp.exp(llast[..., None] - lcum).reshape(b, g, hpg, CHUNK)
        hs = (jnp.exp(llast)[..., None, None] * hs
              + jnp.einsum('bsgn,bgjs,bsgjp->bgjpn', bc, dec_out, xg).reshape(b, h, p, nst))
        return hs, (y_intra + y_inter).reshape(b, CHUNK, h, p)

    h_fin, y = lax.scan(step, h0, (xdt, loga, bch, cch))
    return y.transpose(1, 0, 2, 3, 4).reshape(b, t, h, p), h_fin


def ssd_final_state(xh, dt, a, bm):
    b, t, h, p = xh.shape
    g, nst = bm.shape[2], bm.shape[3]
    lcum = jnp.cumsum(dt * a, axis=1)
    wgt = dt * jnp.exp(lcum[:, -1:] - lcum)
    xg = (xh * wgt[..., None]).reshape(b, t, g, h // g, p)
    return jnp.einsum('btgn,btgjp->bgjpn', bm, xg).reshape(b, h, p, nst)


def state_side(hs, lw, lb, rows):
    b, t, _ = hs.shape
    f_f, f_b, iv, xb, dt_f, dt_b = _split(hs, STATE_SIZES)
    heads = lambda a: a.reshape(b, t, HG_HEADS, a.shape[-1] // HG_HEADS)
    logf_f, k_f = hgrn2_forget(f_f, lb[0])
    logf_b, k_b = hgrn2_forget(f_b, lb[1])
    if rows is not None:
        xb, dt_f, dt_b = to_col_major(xb, rows), to_col_major(dt_f, rows), to_col_major(dt_b, rows)
    xb = jax.nn.silu(centred_dwconv(xb, lw['ma_conv_w'][:MA_XB], lw['ma_conv_b'][:MA_XB], rows))
    xh, bm = _split(xb, (MA_INNER, MA_GROUPS * MA_STATE))
    dtb = lw['ma_dt_bias'].astype(jnp.float32)
    dt_f = jax.nn.softplus(dt_f.astype(jnp.float32) + dtb[0])
    dt_b = jax.nn.softplus(dt_b.astype(jnp.float32) + dtb[1])
    hg = (heads(k_f), heads(logf_f), heads(k_b), heads(logf_b), heads(iv))
    ma = (xh.reshape(b, t, MA_HEADS, MA_HEAD_DIM), bm.reshape(b, t, MA_GROUPS, MA_STATE), dt_f, dt_b)
    return hg, ma


def context_states(uc, lw, lb):
    hs = jnp.einsum('btd,de->bte', uc, lw['w_in'][:, :STATE_DIM])
    (k_f, lf_f, k_b, lf_b, iv), (xh, bm, dt_f, dt_b) = state_side(hs, lw, lb, None)
    a = -jnp.exp(lw['ma_a_log'].astype(jnp.float32))
    return (hgrn2_final_state(k_f, iv, lf_f),
            hgrn2_final_state(_flip(k_b), _flip(iv), _flip(lf_b)),
            ssd_final_state(xh, dt_f, a[0], bm),
            ssd_final_state(_flip(xh), _flip(dt_b), a[1], _flip(bm)))


def token_mix(u, lw, lb, s_init, rows):
    b, t, _ = u.shape
    proj = jnp.einsum('btd,de->bte', u, lw['w_in'])
    (k_f, lf_f, k_b, lf_b, iv), (xh, bm, dt_f, dt_b) = state_side(proj[..., :STATE_DIM], lw, lb, rows)
    q, og, cm, z, ga, gb = _split(proj[..., STATE_DIM:], READ_SIZES)
    q = jax.nn.silu(q).reshape(b, t, HG_HEADS, HG_DK)
    o_f, s_hf = hgrn2_chunk_scan(q, k_f, iv, lf_f, s_init[0])
    o_b, s_hb = hgrn2_chunk_scan(_flip(q), _flip(k_b), _flip(iv), _flip(lf_b), s_init[1])
    o = rms_norm(o_f + _flip(o_b), lw['hg_norm_w']).astype(u.dtype)
    o = o * jax.nn.silu(og.reshape(b, t, HG_HEADS, HG_DV))
    y_a = jnp.einsum('bte,ed->btd', o.reshape(b, t, HG_WIDTH), lw['w_branch_a'])
    if rows is not None:
        cm = to_col_major(cm, rows)
    cm = jax.nn.silu(centred_dwconv(cm, lw['ma_conv_w'][MA_XB:], lw['ma_conv_b'][MA_XB:], rows))
    cm = cm.reshape(b, t, MA_GROUPS, MA_STATE)
    a = -jnp.exp(lw['ma_a_log'].astype(jnp.float32))
    y_f, h_f = ssd_chunk_scan(xh, dt_f, a[0], bm, cm, s_init[2])
    y_b, h_b = ssd_chunk_scan(_flip(xh), _flip(dt_b), a[1], _flip(bm), _flip(cm), s_init[3])
    y = y_f + _flip(y_b) + xh * lw['ma_d'][:, None]
    y = y.reshape(b, t, MA_INNER).astype(u.dtype)
    if rows is not None:
        y = to_row_major(y, rows)
    yz = (y * jax.nn.silu(z)).reshape(b, t, MA_GROUPS, MA_INNER // MA_GROUPS)
    yz = (rms_norm(yz, jnp.ones((), jnp.float32)).reshape(b, t, MA_INNER) * lw['ma_norm_w']).astype(u.dtype)
    y_b2 = jnp.einsum('bte,ed->btd', yz, lw['w_branch_b'])
    m = jax.nn.sigmoid(ga) * y_a + jax.nn.sigmoid(gb) * y_b2
    out = jnp.einsum('btd,de->bte', m, lw['w_out'])
    return out, (s_hf, s_hb, h_f, h_b)


def peer_ffn(u, wq, subkeys, table_u, table_v):
    b, t, d = u.shape
    half = PEER_DKEY // 2

    def block(tb):
        q = jnp.einsum('pd,de->pe', tb, wq).reshape(-1, PEER_HEADS, 2, half)
        s = jnp.einsum('phik,hink->phin', q, subkeys).astype(jnp.float32)
        s1, i1 = lax.top_k(s[:, :, 0], PEER_TOPK)
        s2, i2 = lax.top_k(s[:, :, 1], PEER_TOPK)
        cand = (s1[..., :, None] + s2[..., None, :]).reshape(s1.shape[0], PEER_HEADS, -1)
        cidx = (i1[..., :, None] * PEER_NKEYS + i2[..., None, :]).reshape(s1.shape[0], PEER_HEADS, -1)
        top_s, pos = lax.top_k(cand, PEER_TOPK)
        idx = jnp.take_along_axis(cidx, pos, axis=-1)
        gate = jax.nn.softmax(top_s, axis=-1)
        ue = jnp.take(table_u, idx, axis=0)
        ve = jnp.take(table_v, idx, axis=0)
        act = jax.nn.gelu(jnp.einsum('pd,phkd->phk', tb, ue).astype(jnp.float32), approximate=False)
        return jnp.einsum('phk,phkd->pd', (gate * act).astype(tb.dtype), ve)

    out = lax.map(block, u.reshape(-1, PEER_BLOCK, d))
    return out.reshape(b, t, d)


def trunk_layer(x, xc, c, c_ctx, lw, lb, last):
    mod = jnp.einsum('bd,de->be', jax.nn.silu(c), lw['w_ada']) + lw['b_ada']
    sh1, sc1, g1, sh2, sc2, g2 = jnp.split(mod[:, None, :], 6, axis=-1)
    modc = jnp.einsum('d,de->e', jax.nn.silu(c_ctx), lw['w_ada']) + lw['b_ada']
    csh1, csc1, cg1, csh2, csc2, cg2 = jnp.split(modc, 6)
    rows = x.shape[1] // GRID_W
    uc = modulate(xc, csh1, csc1)
    if last:
        states = context_states(uc, lw, lb)
    else:
        bsz = xc.shape[0]
        zero = (jnp.zeros((bsz, HG_HEADS, HG_DK, HG_DV), jnp.float32),
                jnp.zeros((bsz, HG_HEADS, HG_DK, HG_DV), jnp.float32),
                jnp.zeros((bsz, MA_HEADS, MA_HEAD_DIM, MA_STATE), jnp.float32),
                jnp.zeros((bsz, MA_HEADS, MA_HEAD_DIM, MA_STATE), jnp.float32))
        mix_c, states = token_mix(uc, lw, lb, zero, None)
    mix, _ = token_mix(modulate(x, sh1, sc1), lw, lb, states, rows)
    x = post_ln(DN_ALPHA * x + g1 * mix, lw['ln1_g'], lw['ln1_b'])
    ffn = peer_ffn(modulate(x, sh2, sc2), lw['peer_wq'], lw['peer_subkeys'], lw['peer_u'], lw['peer_v'])
    x = post_ln(DN_ALPHA * x + g2 * ffn, lw['ln2_g'], lw['ln2_b'])
    if not last:
        xc = post_ln(DN_ALPHA * xc + cg1 * mix_c, lw['ln1_g'], lw['ln1_b'])
        ffn_c = peer_ffn(modulate(xc, csh2, csc2), lw['peer_wq'], lw['peer_subkeys'], lw['peer_u'], lw['peer_v'])
        xc = post_ln(DN_ALPHA * xc + cg2 * ffn_c, lw['ln2_g'], lw['ln2_b'])
    return x, xc


def setup_inputs(seed: int = 0) -> dict:
    key = jax.random.key(seed)
    ks = jax.random.split(key, 32)
    f32 = jnp.float32
    nrm = lambda k, shape, s: jax.random.normal(k, shape, f32) * s
    L, D = DEPTH, D_MODEL
    dt0 = jnp.exp(jax.random.uniform(ks[12], (L, 2, MA_HEADS), f32, math.log(1e-3), math.log(1e-1)))
    return {
        'x': nrm(ks[0], (BATCH, SEQ, D), 1.0),
        'c': nrm(ks[1], (BATCH, D), 1.0),
        'ctx': nrm(ks[2], (BATCH, CTX_LEN, D), 1.0),
        'c_ctx': nrm(ks[3], (D,), 1.0),
        'w_ada': nrm(ks[4], (L, D, 6 * D), 0.5 * D ** -0.5),
        'b_ada': nrm(ks[5], (L, 6 * D), 0.02),
        'w_in': nrm(ks[6], (L, D, IN_DIM), D ** -0.5),
        'hg_lb_logits': nrm(ks[7], (2, L + 1, HG_WIDTH), 0.5),
        'hg_norm_w': 1.0 + nrm(ks[8], (L, HG_DV), 0.02),
        'ma_conv_w': nrm(ks[9], (L, MA_XBC, MA_CONV), MA_CONV ** -0.5),
        'ma_conv_b': nrm(ks[10], (L, MA_XBC), 0.02),
        'ma_dt_bias': dt0 + jnp.log(-jnp.expm1(-dt0)),
        'ma_a_log': jnp.log(jax.random.uniform(ks[11], (L, 2, MA_HEADS), f32, 1.0, 16.0)),
        'ma_d': 1.0 + nrm(ks[13], (L, MA_HEADS), 0.02),
        'ma_norm_w': 1.0 + nrm(ks[14], (L, MA_INNER), 0.02),
        'w_branch_a': nrm(ks[15], (L, HG_WIDTH, D), HG_WIDTH ** -0.5),
        'w_branch_b': nrm(ks[16], (L, MA_INNER, D), MA_INNER ** -0.5),
        'w_out': nrm(ks[17], (L, D, D), DN_BETA * D ** -0.5),
        'ln1_g': 1.0 + nrm(ks[18], (L, D), 0.02),
        'ln1_b': nrm(ks[19], (L, D), 0.02),
        'peer_wq': nrm(ks[20], (L, D, PEER_HEADS * PEER_DKEY), D ** -0.5),
        'peer_subkeys': nrm(ks[21], (L, PEER_HEADS, 2, PEER_NKEYS, PEER_DKEY // 2), (PEER_DKEY // 2) ** -0.5),
        'peer_u': nrm(ks[22], (L, PEER_EXPERTS, D), D ** -0.5),
        'peer_v': nrm(ks[23], (L, PEER_EXPERTS, D), DN_BETA),
        'ln2_g': 1.0 + nrm(ks[24], (L, D), 0.02),
        'ln2_b': nrm(ks[25], (L, D), 0.02),
    }


def reference(x, c, ctx, c_ctx, w_ada, b_ada, w_in, hg_lb_logits, hg_norm_w, ma_conv_w, ma_conv_b,
              ma_dt_bias, ma_a_log, ma_d, ma_norm_w, w_branch_a, w_branch_b, w_out, ln1_g, ln1_b,
              peer_wq, peer_subkeys, peer_u, peer_v, ln2_g, ln2_b):
    lb_all = jnp.cumsum(jax.nn.softmax(hg_lb_logits.astype(jnp.float32), axis=1), axis=1)
    xc = ctx
    for l in range(DEPTH):
        lw = {'w_ada': w_ada[l], 'b_ada': b_ada[l], 'w_in': w_in[l], 'hg_norm_w': hg_norm_w[l],
              'ma_conv_w': ma_conv_w[l], 'ma_conv_b': ma_conv_b[l], 'ma_dt_bias': ma_dt_bias[l],
              'ma_a_log': ma_a_log[l], 'ma_d': ma_d[l], 'ma_norm_w': ma_norm_w[l],
              'w_branch_a': w_branch_a[l], 'w_branch_b': w_branch_b[l], 'w_out': w_out[l],
              'ln1_g': ln1_g[l], 'ln1_b': ln1_b[l], 'peer_wq': peer_wq[l], 'peer_subkeys': peer_subkeys[l],
              'peer_u': peer_u[l], 'peer_v': peer_v[l], 'ln2_g': ln2_g[l], 'ln2_b': ln2_b[l]}
        x, xc = trunk_layer(x, xc, c, c_ctx, lw, lb_all[:, l], l == DEPTH - 1)
    return x
```

```python
import numpy as np
from contextlib import ExitStack
import concourse.bass as bass
import concourse.mybir as mybir
from concourse.bass_utils import run_bass_kernel_spmd

F32 = mybir.dt.float32
BF16 = mybir.dt.bfloat16
I32 = mybir.dt.int32
U32 = mybir.dt.uint32
AF = mybir.ActivationFunctionType
ALU = mybir.AluOpType
AX = mybir.AxisListType

D = 1024
T = 8192
CTX = 256
TT = T + CTX
GW = 64
ROWS = T // GW
CH = 64
NCH = T // CH
STATE_DIM = 6208
IN_DIM = 13376
EPS = 1e-6
DN_ALPHA = 2.0 ** 0.25

C_FF, C_FB, C_IV, C_XB, C_DTF, C_DTB = 0, 1024, 2048, 3072, 6144, 6176
C_Q, C_OG, C_CM, C_Z, C_GA, C_GB = 6208, 7232, 8256, 9280, 11328, 12352


class Buf:
    __slots__ = ("t", "lw", "rd", "name")

    def __init__(self, t, name):
        self.t = t
        self.name = name
        self.lw = None
        self.rd = {}

    def __getitem__(self, idx):
        return self.t[idx]


class K:
    def __init__(self, nc, es, n_dma_sems=40):
        self.nc = nc
        self.es = es
        self.eng = {"pe": nc.tensor, "act": nc.scalar, "dve": nc.vector, "pool": nc.gpsimd, "sp": nc.sync}
        self.epoch = 0
        self.sem = {e: es.enter_context(nc.semaphore("prog0_" + e)) for e in self.eng}
        self.cnt = {e: 0 for e in self.eng}
        self.waited = {e: {} for e in self.eng}
        self.dsem = [es.enter_context(nc.semaphore("dma%d" % i)) for i in range(n_dma_sems)]
        self.dval = [0] * n_dma_sems
        self.dnext = 0
        self.out_dmas = []
        self.scopes = [es]

    def sb(self, name, shape, dt):
        t = self.scopes[-1].enter_context(self.nc.sbuf_tensor(name, list(shape), dt))
        return Buf(t, name)

    def ps(self, name, shape, dt=F32):
        t = self.scopes[-1].enter_context(self.nc.psum_tensor(name, list(shape), dt))
        return Buf(t, name)

    def push(self):
        self.scopes.append(ExitStack())

    def pop(self):
        self.barrier()
        self.scopes.pop().close()

    def barrier(self):
        for e in self.eng:
            for i, v in enumerate(self.dval):
                self._wait(e, ("d", i), v)
            for f in self.eng:
                if f != e:
                    self._wait(e, ("e", f, self.epoch), self.cnt[f])
        if max(self.cnt.values()) < 25000:
            return
        self.epoch += 1
        self.sem = {e: self.es.enter_context(self.nc.semaphore("prog%d_%s" % (self.epoch, e))) for e in self.eng}
        self.cnt = {e: 0 for e in self.eng}
        for e in self.eng:
            self.waited[e] = {kk: vv for kk, vv in self.waited[e].items() if kk[0] == "d"}

    def dram(self, name, shape, dt, kind="Internal"):
        t = self.nc.dram_tensor(name, list(shape), dt, kind=kind)
        return Buf(t.ap(), name)

    def _wait(self, e, key, val):
        if val <= 0:
            return
        w = self.waited[e]
        if w.get(key, 0) >= val:
            return
        w[key] = val
        if key[0] == "d":
            self.eng[e].wait_ge(self.dsem[key[1]], val)
        else:
            assert key[2] == self.epoch
            self.eng[e].wait_ge(self.sem[key[1]], val)

    def _deps(self, e, r, w):
        deps = {}
        for b in r:
            if b.lw is not None:
                k, v = b.lw
                deps[k] = max(deps.get(k, 0), v)
        for b in w:
            if b.lw is not None:
                k, v = b.lw
                deps[k] = max(deps.get(k, 0), v)
            for k, v in b.rd.items():
                deps[k] = max(deps.get(k, 0), v)
        for k, v in deps.items():
            if k[0] == "e":
                if k[2] != self.epoch:
                    continue
                if k[1] == "pe" and e == "pe":
                    continue
            self._wait(e, k, v)

    def _mark(self, key, val, r, w):
        for b in r:
            b.rd[key] = max(b.rd.get(key, 0), val)
        for b in w:
            b.lw = (key, val)
            b.rd = {}

    def op(self, e, fn, r=(), w=()):
        self._deps(e, r, w)
        inst = fn(self.eng[e])
        self.cnt[e] += 1
        inst.then_inc(self.sem[e], 1)
        assert self.cnt[e] < 60000, "semaphore epoch overflow: add a barrier"
        self._mark(("e", e, self.epoch), self.cnt[e], r, w)
        return inst

    def dma(self, q, out, in_, r=(), w=(), is_out=False, **kw):
        self._deps(q, r, w)
        i = self.dnext
        self.dnext = (self.dnext + 1) % len(self.dsem)
        self._wait(q, ("d", i), self.dval[i])
        inst = self.eng[q].dma_start(out=out, in_=in_, **kw)
        self.dval[i] += 16
        inst.then_inc(self.dsem[i], 16)
        self._mark(("d", i), self.dval[i], r, w)
        if is_out:
            self.out_dmas.append((i, self.dval[i]))

    def finish(self):
        for i, v in enumerate(self.dval):
            self._wait("sp", ("d", i), v)
        for e in self.eng:
            if e != "sp":
                self._wait("sp", ("e", e, self.epoch), self.cnt[e])


def feat(v):
    v = np.asarray(v, np.float32).reshape(-1, 128)
    return np.ascontiguousarray(v.T)


def make_consts():
    c = {}
    c["ident"] = np.eye(128, dtype=np.float32)
    s = np.arange(64)
    c["incl_f"] = (s[:, None] <= s[None, :]).astype(np.float32)
    c["incl_b"] = (s[:, None] >= s[None, :]).astype(np.float32)
    c["strict_f"] = (s[:, None] > s[None, :]).astype(np.float32)
    c["strict_b"] = (s[:, None] < s[None, :]).astype(np.float32)
    c["ones64"] = np.ones((64, 128), np.float32)
    s2 = np.arange(128)
    c["incl_f128"] = (s2[:, None] <= s2[None, :]).astype(np.float32)
    c["incl_b128"] = (s2[:, None] >= s2[None, :]).astype(np.float32)
    c["strict_f128"] = (s2[:, None] > s2[None, :]).astype(np.float32)
    c["strict_b128"] = (s2[:, None] < s2[None, :]).astype(np.float32)
    return c


def build(debug=(), stages=99):
    nc = bass.Bass("TRN2", target_bir_lowering=False)
    es = ExitStack()
    k = K(nc, es)
    dbg = set(debug)

    def din(name, shape, dt=F32):
        return nc.dram_tensor(name, list(shape), dt, kind="ExternalInput").ap()

    def scratch(name, shape, dt=F32):
        kind = "ExternalOutput" if name in dbg else "Internal"
        return k.dram("d_" + name, shape, dt, kind=kind)

    x = din("x", [T, D])
    ctx = din("ctx", [CTX, D])
    cvec = din("cvec", [128, 8, 2])
    w_ada = din("w_ada", [D, 6 * D])
    b_ada = din("b_ada", [128, 48])
    w_in = din("w_in", [D, IN_DIM])
    ident_d = din("ident", [128, 128])
    hg_lb = din("hg_lb", [2, 2, 1024])
    hg_nw = din("hg_nw", [128, 1])
    conv_w = din("conv_w", [128, 32, 5])
    conv_b = din("conv_b", [128, 32])
    ssd_vec = din("ssd_vec", [3, 64])
    ma_nw = din("ma_nw", [2048])
    w_ba = din("w_ba", [1024, 1024]); w_bb = din("w_bb", [2048, 1024]); w_o = din("w_o", [1024, 1024])
    ln1_g = din("ln1_g", [1024]); ln1_b = din("ln1_b", [1024])
    peer_wq = din("peer_wq", [1024, 2048]); peer_sub = din("peer_sub", [16, 128, 128])
    peer_u = din("peer_u", [16384, 1024]); peer_v = din("peer_v", [16384, 1024])
    ln2_g = din("ln2_g", [1024]); ln2_b = din("ln2_b", [1024])
    iota_c = din("iota_c", [128])
    cmask = {nm: din(nm, [64, 64]) for nm in ("incl_f", "incl_b", "strict_f", "strict_b")}
    cmask128 = {nm: din(nm + "128", [128, 128]) for nm in ("incl_f", "incl_b", "strict_f", "strict_b")}

    out = nc.dram_tensor("out", [T, D], F32, kind="ExternalOutput").ap()
    out_buf = Buf(out, "out")

    ident_f = k.sb("ident_f", [128, 128], F32)
    ident_b = k.sb("ident_b", [128, 128], BF16)
    k.dma("sp", ident_f[:], ident_d[:, :], w=[ident_f])
    k.op("dve", lambda e: e.tensor_copy(out=ident_b[:], in_=ident_f[:]), r=[ident_f], w=[ident_b])

    modT = k.sb("modT", [128, 48, 2], F32)
    ops1 = k.sb("ops1", [128, 8, 2], F32)
    ops2 = k.sb("ops2", [128, 8, 2], F32)
    eps_t = k.sb("eps_t", [128, 1], F32)
    k.op("dve", lambda e: e.memset(eps_t[:], EPS), w=[eps_t])
    k.push()
    cv = k.sb("cv", [128, 8, 2], F32)
    scv = k.sb("scv", [128, 8, 2], F32)
    bada = k.sb("bada", [128, 48], F32)
    k.dma("sp", cv[:], cvec[:, :, :], w=[cv])
    k.dma("sp", bada[:], b_ada[:, :], w=[bada])
    k.op("act", lambda e: e.activation(out=scv[:], in_=cv[:], func=AF.Silu), r=[cv], w=[scv])
    wada_v = w_ada.rearrange("(kk p) e -> p kk e", p=128)
    NP0 = 8
    PW = 6 * D // NP0
    wst = [k.sb("wada_st%d" % i, [128, 8, PW], F32) for i in range(2)]
    mod_ps = k.ps("mod_ps", [128, 48, 2], F32)
    for pc in range(NP0):
        st = wst[pc % 2]
        k.dma("sp", st[:], wada_v[:, :, pc * PW:(pc + 1) * PW], w=[st])
        for bb in range(PW // 128):
            blk = pc * (PW // 128) + bb
            for kk in range(8):
                k.op("pe", lambda e, st=st, bb=bb, kk=kk, blk=blk: e.matmul(
                    mod_ps[:, blk, :], lhsT=st[:, kk, bb * 128:(bb + 1) * 128], rhs=scv[:, kk, :],
                    start=(kk == 0), stop=(kk == 7)), r=[st, scv], w=[mod_ps])
    k.op("dve", lambda e: e.tensor_tensor(out=modT[:], in0=mod_ps[:], in1=bada[:].unsqueeze(2).to_broadcast([128, 48, 2]),
                                          op=ALU.add), r=[mod_ps, bada], w=[modT])
    k.op("dve", lambda e: e.tensor_scalar(out=ops1[:], in0=modT[:, 8:16, :], scalar1=1.0, scalar2=None, op0=ALU.add),
         r=[modT], w=[ops1])
    k.op("dve", lambda e: e.tensor_scalar(out=ops2[:], in0=modT[:, 32:40, :], scalar1=1.0, scalar2=None, op0=ALU.add),
         r=[modT], w=[ops2])

    if "modT" in dbg:
        modT_d = scratch("modT", [128, 96])
        k.dma("sp", modT_d[:, :], modT[:].rearrange("p a b -> p (a b)"), r=[modT], w=[modT_d], is_out=True)
    k.pop()

    k.push()
    uT = k.sb("uT", [128, 8, TT], BF16)
    k.push()
    xt = [k.sb("xt%d" % i, [128, D], F32) for i in range(2)]
    xn = [k.sb("xn%d" % i, [128, D], BF16) for i in range(2)]
    st6 = [k.sb("st6_%d" % i, [128, 2, 6], F32) for i in range(2)]
    mv = [k.sb("mv%d" % i, [128, 2], F32) for i in range(2)]
    rstd = [k.sb("rstd%d" % i, [128, 1], F32) for i in range(2)]
    nmr = [k.sb("nmr%d" % i, [128, 1], F32) for i in range(2)]
    tp_ps = [k.ps("tp_ps%d" % i, [128, 8, 128], BF16) for i in range(2)]

    def ln_tile(src_ap, i, xt_, xn_, st6_, mv_, rstd_, nmr_):
        k.dma("sp", xt_[:], src_ap, w=[xt_])
        for h in range(2):
            k.op("dve", lambda e, h=h: e.bn_stats(out=st6_[:, h, :], in_=xt_[:, h * 512:(h + 1) * 512]), r=[xt_], w=[st6_])
        k.op("dve", lambda e: e.bn_aggr(out=mv_[:], in_=st6_[:].rearrange("p a b -> p (a b)")), r=[st6_], w=[mv_])
        k.op("act", lambda e: e.activation(out=rstd_[:], in_=mv_[:, 1:2], func=AF.Sqrt, bias=eps_t[:], scale=1.0), r=[mv_, eps_t], w=[rstd_])
        k.op("dve", lambda e: e.reciprocal(out=rstd_[:], in_=rstd_[:]), r=[rstd_], w=[rstd_])
        k.op("dve", lambda e: e.scalar_tensor_tensor(out=nmr_[:], in0=mv_[:, 0:1], scalar=-1.0, in1=rstd_[:], op0=ALU.mult, op1=ALU.mult),
             r=[mv_, rstd_], w=[nmr_])
        k.op("act", lambda e: e.activation(out=xn_[:], in_=xt_[:], func=AF.Identity, bias=nmr_[:], scale=rstd_[:]),
             r=[xt_, nmr_, rstd_], w=[xn_])

    NT = T // 128
    x_cm = x.rearrange("(r c) d -> c r d", c=GW)

    def make_uT(order):
      for i in range(NT + CTX // 128):
          j = i % 2
          if i < NT:
              src = x[i * 128:(i + 1) * 128, :] if order == "row" else x_cm[i]
              col = 0
              t0 = i * 128
          else:
              src = ctx[(i - NT) * 128:(i - NT + 1) * 128, :]
              col = 1
              t0 = T + (i - NT) * 128
          ln_tile(src, i, xt[j], xn[j], st6[j], mv[j], rstd[j], nmr[j])
          for kk in range(8):
              k.op("pe", lambda e, kk=kk, j=j: e.transpose(out=tp_ps[j][:, kk, :], in_=xn[j][:, kk * 128:(kk + 1) * 128], identity=ident_b[:]),
                   r=[xn[j], ident_b], w=[tp_ps[j]])
          for kk in range(8):
              eng = "act" if kk % 2 == 0 else "dve"
              if eng == "act":
                  k.op("act", lambda e, kk=kk, j=j, col=col, t0=t0: e.activation(
                      out=uT[:, kk, t0:t0 + 128], in_=tp_ps[j][:, kk, :], func=AF.Identity,
                      bias=modT[:, kk, col:col + 1], scale=ops1[:, kk, col:col + 1]), r=[tp_ps[j], modT, ops1], w=[uT])
              else:
                  k.op("dve", lambda e, kk=kk, j=j, col=col, t0=t0: e.tensor_scalar(
                      out=uT[:, kk, t0:t0 + 128], in0=tp_ps[j][:, kk, :], scalar1=ops1[:, kk, col:col + 1],
                      scalar2=modT[:, kk, col:col + 1], op0=ALU.mult, op1=ALU.add), r=[tp_ps[j], modT, ops1], w=[uT])


    make_uT("row")
    k.pop()
    if "uT" in dbg:
        uT_d = scratch("uT", [128, 8 * TT], BF16)
        k.dma("sp", uT_d[:, :], uT[:].rearrange("p a b -> p (a b)"), r=[uT], w=[uT_d], is_out=True)


    d_FF = scratch("FF", [TT, 1024]); d_FB = scratch("FB", [TT, 1024]); d_IV = scratch("IV", [TT, 1024], BF16)
    d_QT = scratch("QT", [1024, T]); d_OGT = scratch("OGT", [1024, T])
    d_GAT = scratch("GAT", [1024, T]); d_GBT = scratch("GBT", [1024, T])
    d_XBT = scratch("XBT", [3072, TT]); d_DT = scratch("DT", [TT, 64]); d_CT = scratch("CT", [1024, T])
    d_Z = scratch("Z", [T, 2048])
    k.push()
    w_in_v = w_in.rearrange("(kk p) e -> p kk e", p=128)
    wst = [k.sb("win_st%d" % i, [128, 8, 512], F32) for i in range(1)]
    wbf = [k.sb("win_bf%d" % i, [128, 8, 512], BF16) for i in range(2)]
    stg = [k.sb("stg%d" % i, [128, 2048], F32) for i in range(2)]
    stgb = [k.sb("stgb%d" % i, [128, 512], BF16) for i in range(2)]
    pp = [k.ps("pp%d" % i, [128, 512], F32) for i in range(3)]
    ptmp = k.sb("ptmp", [128, T], BF16)
    uT_cm = uT[:, :, 0:T].rearrange("p k (r c) -> p k c r", c=GW)
    cnt = {"pc": 0, "pp": 0, "stg": 0, "stgb": 0}

    def load_piece(c0, ncols):
        i = cnt["pc"] % 2
        cnt["pc"] += 1
        k.dma("sp", wst[0][:, :, 0:ncols], w_in_v[:, :, c0:c0 + ncols], w=[wst[0]])
        for kk in range(8):
            eng = "pool" if kk % 2 == 0 else "dve"
            k.op(eng, lambda e, kk=kk, i=i: e.tensor_copy(out=wbf[i][:, kk, 0:ncols], in_=wst[0][:, kk, 0:ncols]),
                 r=[wst[0]], w=[wbf[i]])
        return wbf[i]

    def evac(dst_ap, src_ap, func, r, w):
        if func is None:
            k.op("dve", lambda e: e.tensor_copy(out=dst_ap, in_=src_ap), r=r, w=w)
        else:
            k.op("act", lambda e: e.activation(out=dst_ap, in_=src_ap, func=func), r=r, w=w)

    def proj_fm(c0, ncols, dst, drow0, order, func, with_ctx):
        for p0 in range(0, ncols, 512):
            wb = load_piece(c0 + p0, 512)
            for eb in range(4):
                nblk = 16 + (1 if with_ctx else 0)
                for tb in range(nblk):
                    q = tb % 4
                    if q == 0:
                        sg = stg[cnt["stg"] % 2]
                        cnt["stg"] += 1
                    ps_ = pp[cnt["pp"] % 3]
                    cnt["pp"] += 1
                    n = 512 if tb < 16 else CTX
                    for kk in range(8):
                        if tb == 16:
                            rhs = uT[:, kk, T:TT]
                        elif order == "row":
                            rhs = uT[:, kk, tb * 512:(tb + 1) * 512]
                        else:
                            rhs = uT[:, kk, tb * 512:(tb + 1) * 512]
                        k.op("pe", lambda e, kk=kk, rhs=rhs, ps_=ps_, n=n, wb=wb, eb=eb: e.matmul(
                            ps_[:, 0:n], lhsT=wb[:, kk, eb * 128:(eb + 1) * 128], rhs=rhs, start=(kk == 0), stop=(kk == 7)),
                            r=[wb, uT], w=[ps_])
                    evac(sg[:, q * 512:q * 512 + n], ps_[:, 0:n], func, [ps_], [sg])
                    r0 = drow0 + p0 + eb * 128
                    if tb == 16:
                        k.dma("sp", dst[r0:r0 + 128, T:TT], sg[:, 0:CTX], r=[sg], w=[dst])
                    elif q == 3:
                        k.dma("sp", dst[r0:r0 + 128, (tb - 3) * 512:(tb + 1) * 512], sg[:, :], r=[sg], w=[dst])

    def proj_tm(c0, ncols, dsts, order, func, with_ctx, bf=False):
        for p0 in range(0, ncols, 512):
            pw = min(512, ncols - p0)
            wb = load_piece(c0 + p0, pw)
            ntile = NT + (CTX // 128 if with_ctx else 0)
            for tt in range(ntile):
                ps_ = pp[cnt["pp"] % 3]
                cnt["pp"] += 1
                for kk in range(8):
                    if tt >= NT:
                        lhsT = uT[:, kk, T + (tt - NT) * 128:T + (tt - NT + 1) * 128]
                    elif order == "row":
                        lhsT = uT[:, kk, tt * 128:(tt + 1) * 128]
                    else:
                        lhsT = uT[:, kk, tt * 128:(tt + 1) * 128]
                    k.op("pe", lambda e, kk=kk, lhsT=lhsT, ps_=ps_, wb=wb, pw=pw: e.matmul(
                        ps_[:, 0:pw], lhsT=lhsT, rhs=wb[:, kk, 0:pw], start=(kk == 0), stop=(kk == 7)),
                        r=[wb, uT], w=[ps_])
                if bf:
                    sg = stgb[cnt["stgb"] % 2]
                    cnt["stgb"] += 1
                else:
                    sg = stg[cnt["stg"] % 2]
                    cnt["stg"] += 1
                evac(sg[:, 0:pw], ps_[:, 0:pw], func, [ps_], [sg])
                pos0 = tt * 128
                for (dst, lo, hi, dcol0) in dsts:
                    a = max(lo, p0)
                    b = min(hi, p0 + pw)
                    if a < b:
                        k.dma("sp", dst[pos0:pos0 + 128, dcol0 + a - lo:dcol0 + b - lo], sg[:, a - p0:b - p0], r=[sg], w=[dst])

    sel = stages if isinstance(stages, (set, list, tuple)) else None
    def on(name):
        return sel is None and stages >= 1 or (sel is not None and name in sel)
    if on("a"):
        proj_tm(C_FF, 2048, [(d_FF, 0, 1024, 0), (d_FB, 1024, 2048, 0)], "row", None, True)
    if on("b"):
        proj_tm(C_IV, 1024, [(d_IV, 0, 1024, 0)], "row", None, True, bf=True)
    if on("c"):
        proj_fm(C_Q, 1024, d_QT, 0, "row", AF.Silu, False)
        proj_fm(C_OG, 1024, d_OGT, 0, "row", AF.Silu, False)
        proj_fm(C_GA, 1024, d_GAT, 0, "row", AF.Sigmoid, False)
        proj_fm(C_GB, 1024, d_GBT, 0, "row", AF.Sigmoid, False)
    for kk in range(8):
        k.op("dve", lambda e, kk=kk: e.tensor_copy(out=ptmp[:], in_=uT[:, kk, 0:T]), r=[uT], w=[ptmp])
        k.op("pool", lambda e, kk=kk: e.tensor_copy(out=uT[:, kk, 0:T].rearrange("p (c r) -> p c r", r=ROWS),
                                                   in_=ptmp[:].rearrange("p (r c) -> p c r", c=GW)), r=[ptmp], w=[uT])
    if on("d"):
        proj_fm(C_XB, 3072, d_XBT, 0, "col", None, True)
        proj_fm(C_CM, 1024, d_CT, 0, "col", None, False)
    if on("e"):
        proj_tm(C_DTF, 64, [(d_DT, 0, 64, 0)], "col", None, True)
        proj_tm(C_Z, 2048, [(d_Z, 0, 2048, 0)], "col", AF.Silu, False)
    k.pop()
    k.pop()

    d_OF = scratch("OF", [1024, T]); d_OB = scratch("OB", [1024, T])
    d_ONT = scratch("ONT", [1024, T], BF16)
    if sel is None and stages >= 2 or (sel is not None and "h" in sel):
        k.push()
        lbl = [k.sb("lbl%d" % i, [64, 2, 1024], F32) for i in range(2)]
        lb = [k.sb("lb%d" % i, [64, 1024], F32) for i in range(2)]
        oml = [k.sb("oml%d" % i, [64, 1024], F32) for i in range(2)]
        msk = {}
        for nm in ("incl_f", "incl_b", "strict_f", "strict_b"):
            msk[nm] = k.sb("m_" + nm, [64, 64], F32)
            k.dma("sp", msk[nm][:], cmask[nm][:, :], w=[msk[nm]])
        for d_ in range(2):
            k.dma("sp", lbl[d_][:], hg_lb[d_].partition_broadcast(64), w=[lbl[d_]])
            k.op("dve", lambda e, d_=d_: e.tensor_tensor(out=lb[d_][:], in0=lbl[d_][:, 0, :], in1=lbl[d_][:, 1, :], op=ALU.subtract),
                 r=[lbl[d_]], w=[lb[d_]])
            k.op("act", lambda e, d_=d_: e.activation(out=lb[d_][:], in_=lb[d_][:], func=AF.Sigmoid), r=[lb[d_]], w=[lb[d_]])
            k.op("dve", lambda e, d_=d_: e.tensor_scalar(out=oml[d_][:], in0=lb[d_][:], scalar1=-1.0, scalar2=1.0, op0=ALU.mult, op1=ALU.add),
                 r=[lb[d_]], w=[oml[d_]])
        NB = 4
        fr = [k.sb("fr%d" % i, [64, NB, 1024], F32) for i in range(2)]
        vv = [k.sb("vv%d" % i, [64, NB, 1024], BF16) for i in range(2)]
        qs = [k.sb("qs%d" % i, [128, 8, NB * 64], F32) for i in range(2)]
        lf = k.sb("lf", [64, NB, 1024], F32)
        km = k.sb("km", [64, NB, 1024], F32)
        ost = k.sb("ost", [128, 8, NB * 64], F32)
        E1 = k.sb("E1", [128, 8, 64], F32)
        E2 = k.sb("E2", [64, 1024], F32)
        QtT = k.sb("QtT", [128, 8, 64], BF16)
        Kt = k.sb("Kt", [64, 1024], BF16)
        Kh = k.sb("Kh", [64, 1024], BF16)
        KtT = k.sb("KtT", [128, 8, 64], BF16)
        attm = k.sb("attm", [64, 8, 64], BF16)
        S = k.sb("S", [128, 8, 128], F32)
        Sbf = k.sb("Sbf", [128, 8, 128], BF16)
        gcT_ps = k.ps("gcT_ps", [128, 8, 64], F32)
        gs_ps = k.ps("gs_ps", [64, 1024], F32)
        ktT_ps = k.ps("ktT_ps", [128, 8, 128], BF16)
        att_ps = k.ps("att_ps", [64, 8, 64], F32)
        oT_ps = k.ps("oT_ps", [128, 8, 64], F32)
        dS_ps = k.ps("dS_ps", [128, 8, 128], F32)

        for d_ in range(2):
            incl = msk["incl_f" if d_ == 0 else "incl_b"]
            strict = msk["strict_f" if d_ == 0 else "strict_b"]
            d_F = d_FF if d_ == 0 else d_FB
            d_O = d_OF if d_ == 0 else d_OB
            glast_col = 63 if d_ == 0 else 0
            k.op("pool", lambda e: e.memset(S[:], 0.0), w=[S])
            k.op("pool", lambda e: e.memset(Sbf[:], 0.0), w=[Sbf])
            if d_ == 0:
                blocks = [(T, CTX // 64, True)] + [(b * NB * 64, NB, False) for b in range(NCH // NB)]
            else:
                blocks = [(T, CTX // 64, True)] + [(b * NB * 64, NB, False) for b in reversed(range(NCH // NB))]
            for bi, (t0, nc_, is_ctx) in enumerate(blocks):
                j = bi % 2
                nt = nc_ * 64
                k.dma("sp", fr[j][:, 0:nc_, :], d_F[t0:t0 + nt, :].rearrange("(c s) e -> s c e", s=64), r=[d_F], w=[fr[j]])
                k.dma("sp", vv[j][:, 0:nc_, :], d_IV[t0:t0 + nt, :].rearrange("(c s) e -> s c e", s=64), r=[d_IV], w=[vv[j]])
                if not is_ctx:
                    k.dma("sp", qs[j][:], d_QT[:, t0:t0 + nt].rearrange("(h p) t -> p h t", p=128), r=[d_QT], w=[qs[j]])
                k.op("act", lambda e: e.activation(out=fr[j][:, 0:nc_, :], in_=fr[j][:, 0:nc_, :], func=AF.Sigmoid), r=[fr[j]], w=[fr[j]])
                k.op("dve", lambda e: e.tensor_tensor(out=fr[j][:, 0:nc_, :], in0=fr[j][:, 0:nc_, :],
                                                      in1=oml[d_][:].unsqueeze(1).to_broadcast([64, nc_, 1024]), op=ALU.mult),
                     r=[fr[j], oml[d_]], w=[fr[j]])
                k.op("dve", lambda e: e.tensor_tensor(out=fr[j][:, 0:nc_, :], in0=fr[j][:, 0:nc_, :],
                                                      in1=lb[d_][:].unsqueeze(1).to_broadcast([64, nc_, 1024]), op=ALU.add),
                     r=[fr[j], lb[d_]], w=[fr[j]])
                k.op("act", lambda e: e.activation(out=lf[:, 0:nc_, :], in_=fr[j][:, 0:nc_, :], func=AF.Ln), r=[fr[j]], w=[lf])
                k.op("dve", lambda e: e.tensor_scalar(out=km[:, 0:nc_, :], in0=fr[j][:, 0:nc_, :], scalar1=-1.0, scalar2=1.0,
                                                      op0=ALU.mult, op1=ALU.add), r=[fr[j]], w=[km])
                corder = list(range(nc_)) if d_ == 0 else list(reversed(range(nc_)))
                for c in corder:
                    for h in range(8):
                        k.op("pe", lambda e, h=h, c=c: e.matmul(gcT_ps[:, h, :], lhsT=lf[:, c, h * 128:(h + 1) * 128], rhs=incl[:],
                                                                start=True, stop=True), r=[lf, incl], w=[gcT_ps])
                    k.op("act", lambda e: e.activation(out=E1[:], in_=gcT_ps[:], func=AF.Exp), r=[gcT_ps], w=[E1])
                    if not is_ctx:
                        for hf in range(2):
                            k.op("pe", lambda e, hf=hf, c=c: e.matmul(gs_ps[:, hf * 512:(hf + 1) * 512], lhsT=incl[:],
                                                                      rhs=lf[:, c, hf * 512:(hf + 1) * 512], start=True, stop=True),
                                 r=[lf, incl], w=[gs_ps])
                        k.op("act", lambda e: e.activation(out=E2[:], in_=gs_ps[:], func=AF.Exp, scale=-1.0), r=[gs_ps], w=[E2])
                        k.op("dve", lambda e, c=c: e.tensor_tensor(out=Kt[:], in0=km[:, c, :], in1=E2[:], op=ALU.mult), r=[km, E2], w=[Kt])
                        k.op("dve", lambda e, c=c: e.tensor_tensor(out=QtT[:], in0=qs[j][:, :, c * 64:(c + 1) * 64], in1=E1[:], op=ALU.mult),
                             r=[qs[j], E1], w=[QtT])
                        for h in range(8):
                            k.op("pe", lambda e, h=h: e.transpose(out=ktT_ps[:, h, 0:64], in_=Kt[:, h * 128:(h + 1) * 128], identity=ident_b[0:64, 0:64]),
                                 r=[Kt, ident_b], w=[ktT_ps])
                        k.op("act", lambda e: e.copy(out=KtT[:], in_=ktT_ps[:, :, 0:64]), r=[ktT_ps], w=[KtT])
                        for h in range(8):
                            k.op("pe", lambda e, h=h: e.matmul(att_ps[:, h, :], lhsT=KtT[:, h, :], rhs=QtT[:, h, :], start=True, stop=True),
                                 r=[KtT, QtT], w=[att_ps])
                        k.op("dve", lambda e: e.tensor_tensor(out=attm[:], in0=att_ps[:], in1=incl[:].unsqueeze(1).to_broadcast([64, 8, 64]),
                                                              op=ALU.mult), r=[att_ps, incl], w=[attm])
                        for h in range(8):
                            k.op("pe", lambda e, h=h, c=c: e.matmul(oT_ps[:, h, :], lhsT=vv[j][:, c, h * 128:(h + 1) * 128], rhs=attm[:, h, :],
                                                                    start=True, stop=False), r=[vv[j], attm], w=[oT_ps])
                            k.op("pe", lambda e, h=h: e.matmul(oT_ps[:, h, :], lhsT=Sbf[:, h, :], rhs=QtT[:, h, :], start=False, stop=True),
                                 r=[Sbf, QtT], w=[oT_ps])
                        k.op("act", lambda e, c=c: e.copy(out=ost[:, :, c * 64:(c + 1) * 64], in_=oT_ps[:]), r=[oT_ps], w=[ost])
                    for hf in range(2):
                        k.op("pe", lambda e, hf=hf, c=c: e.matmul(gs_ps[:, hf * 512:(hf + 1) * 512], lhsT=strict[:],
                                                                  rhs=lf[:, c, hf * 512:(hf + 1) * 512], start=True, stop=True),
                             r=[lf, strict], w=[gs_ps])
                    k.op("act", lambda e: e.activation(out=E2[:], in_=gs_ps[:], func=AF.Exp), r=[gs_ps], w=[E2])
                    k.op("dve", lambda e, c=c: e.tensor_tensor(out=Kh[:], in0=km[:, c, :], in1=E2[:], op=ALU.mult), r=[km, E2], w=[Kh])
                    for h in range(8):
                        k.op("pe", lambda e, h=h, c=c: e.matmul(dS_ps[:, h, :], lhsT=Kh[:, h * 128:(h + 1) * 128], rhs=vv[j][:, c, h * 128:(h + 1) * 128],
                                                                start=True, stop=True), r=[Kh, vv[j]], w=[dS_ps])
                    k.op("dve", lambda e: e.tensor_tensor(out=S[:], in0=S[:], in1=E1[:, :, glast_col:glast_col + 1].to_broadcast([128, 8, 128]),
                                                          op=ALU.mult), r=[S, E1], w=[S])
                    k.op("dve", lambda e: e.tensor_tensor(out=S[:], in0=S[:], in1=dS_ps[:], op=ALU.add), r=[S, dS_ps], w=[S])
                    k.op("act", lambda e: e.copy(out=Sbf[:], in_=S[:]), r=[S], w=[Sbf])
                if not is_ctx:
                    k.dma("sp", d_O[:, t0:t0 + nt].rearrange("(h p) t -> p h t", p=128), ost[:], r=[ost], w=[d_O])
            k.barrier()
            if "S%d" % d_ in dbg:
                dS_d = scratch("S%d" % d_, [128, 1024])
                k.dma("sp", dS_d[:, :], S[:].rearrange("p a b -> p (a b)"), r=[S], w=[dS_d])
        k.pop()

    if sel is None and stages >= 2 or (sel is not None and "n" in sel):
        k.push()
        ones_f = k.sb("ones_f", [128, 128], F32)
        k.op("pool", lambda e: e.memset(ones_f[:], 1.0), w=[ones_f])
        nw = k.sb("nw", [128, 1], F32)
        k.dma("sp", nw[:], hg_nw[:, :], w=[nw])
        ofb = [k.sb("ofb%d" % i, [128, 8, 512], F32) for i in range(2)]
        obb = [k.sb("obb%d" % i, [128, 8, 512], F32) for i in range(2)]
        ogb = [k.sb("ogb%d" % i, [128, 8, 512], F32) for i in range(2)]
        sq = k.sb("sq", [128, 8, 512], F32)
        rr = [k.sb("rr%d" % i, [128, 512], F32) for i in range(2)]
        onb = [k.sb("onb%d" % i, [128, 8, 512], BF16) for i in range(2)]
        ssq_ps = [k.ps("ssq_ps%d" % i, [128, 512], F32) for i in range(2)]
        for b_ in range(T // 512):
            j = b_ % 2
            t0 = b_ * 512
            k.dma("sp", ofb[j][:], d_OF[:, t0:t0 + 512].rearrange("(h p) t -> p h t", p=128), r=[d_OF], w=[ofb[j]])
            k.dma("sp", obb[j][:], d_OB[:, t0:t0 + 512].rearrange("(h p) t -> p h t", p=128), r=[d_OB], w=[obb[j]])
            k.dma("sp", ogb[j][:], d_OGT[:, t0:t0 + 512].rearrange("(h p) t -> p h t", p=128), r=[d_OGT], w=[ogb[j]])
            k.op("pool", lambda e: e.tensor_tensor(out=ofb[j][:], in0=ofb[j][:], in1=obb[j][:], op=ALU.add), r=[ofb[j], obb[j]], w=[ofb[j]])
            k.op("act", lambda e: e.activation(out=sq[:], in_=ofb[j][:], func=AF.Square), r=[ofb[j]], w=[sq])
            for h in range(8):
                jj = h % 2
                k.op("pe", lambda e: e.matmul(ssq_ps[jj][:], lhsT=ones_f[:], rhs=sq[:, h, :], start=True, stop=True), r=[ones_f, sq], w=[ssq_ps[jj]])
                k.op("act", lambda e: e.activation(out=rr[jj][:], in_=ssq_ps[jj][:], func=AF.Sqrt, bias=eps_t[:], scale=1.0 / 128.0),
                     r=[ssq_ps[jj], eps_t], w=[rr[jj]])
                k.op("dve", lambda e: e.reciprocal(out=rr[jj][:], in_=rr[jj][:]), r=[rr[jj]], w=[rr[jj]])
                k.op("dve", lambda e: e.tensor_tensor(out=rr[jj][:], in0=rr[jj][:], in1=ofb[j][:, h, :], op=ALU.mult), r=[rr[jj], ofb[j]], w=[rr[jj]])
                k.op("dve", lambda e: e.scalar_tensor_tensor(out=onb[j][:, h, :], in0=rr[jj][:], scalar=nw[:, 0:1], in1=ogb[j][:, h, :],
                                                             op0=ALU.mult, op1=ALU.mult), r=[rr[jj], nw, ogb[j]], w=[onb[j]])
            k.dma("sp", d_ONT[:, t0:t0 + 512].rearrange("(h p) t -> p h t", p=128), onb[j][:], r=[onb[j]], w=[d_ONT])
        k.pop()
    if "ONs" in dbg:
        dd = scratch("ONs", [1024, 256], BF16)
        k.dma("sp", dd[:, 0:128], d_ONT[:, 0:128], r=[d_ONT], w=[dd])
        k.dma("sp", dd[:, 128:256], d_ONT[:, T - 128:T], r=[d_ONT], w=[dd])

    d_XH = scratch("XH", [TT, 2048], BF16)
    d_BM = scratch("BM", [TT, 1024], BF16)
    d_BT = scratch("BT", [1024, TT], BF16)
    d_CT2 = scratch("CT2", [1024, T], BF16)
    if sel is None and stages >= 3 or (sel is not None and "v" in sel):
        k.push()
        cw = k.sb("cw", [128, 32, 5], F32)
        cb = k.sb("cb", [128, 32], F32)
        k.dma("sp", cw[:], conv_w[:, :, :], w=[cw])
        k.dma("sp", cb[:], conv_b[:, :], w=[cb])
        pad = [k.sb("pad%d" % i, [128, 4, 132], F32) for i in range(4)]
        padc = [k.sb("padc%d" % i, [128, 1, 260], F32) for i in range(4)]
        acc = [k.sb("acc%d" % i, [128, 512], F32) for i in range(4)]
        res = [k.sb("res%d" % i, [128, 512], BF16) for i in range(4)]
        tok = [k.sb("tok%d" % i, [128, 4, 3072], BF16) for i in range(2)]
        tps = [k.ps("tps%d" % i, [128, 4, 128], BF16) for i in range(4)]
        for i in range(4):
            k.op("pool", lambda e, i=i: e.memset(pad[i][:], 0.0), w=[pad[i]])
            k.op("pool", lambda e, i=i: e.memset(padc[i][:], 0.0), w=[padc[i]])
        it = 0
        for blk in range(T // 512 + 1):
            is_ctx = blk == T // 512
            ncol, L = (1, 256) if is_ctx else (4, 128)
            p0 = T if is_ctx else blk * 512
            n = ncol * L
            npt = n // 128
            tk = tok[blk % 2]
            for ct in range(32):
                if is_ctx and ct >= 24:
                    continue
                j = it % 4
                it += 1
                pb = padc[j] if is_ctx else pad[j]
                src = d_XBT[ct * 128:(ct + 1) * 128, p0:p0 + n] if ct < 24 else d_CT[(ct - 24) * 128:(ct - 23) * 128, p0:p0 + n]
                k.dma("sp", pb[:, :, 2:2 + L], src.rearrange("p (c r) -> p c r", r=L), r=[d_XBT if ct < 24 else d_CT], w=[pb])
                av = acc[j][:, 0:n].rearrange("p (c r) -> p c r", r=L)
                k.op("dve", lambda e: e.tensor_scalar(out=av, in0=pb[:, :, 0:L], scalar1=cw[:, ct, 0:1], scalar2=cb[:, ct:ct + 1],
                                                      op0=ALU.mult, op1=ALU.add), r=[pb, cw, cb], w=[acc[j]])
                for kk in range(1, 5):
                    k.op("dve", lambda e, kk=kk: e.scalar_tensor_tensor(out=av, in0=pb[:, :, kk:kk + L], scalar=cw[:, ct, kk:kk + 1], in1=av,
                                                                        op0=ALU.mult, op1=ALU.add), r=[pb, cw, acc[j]], w=[acc[j]])
                k.op("act", lambda e: e.activation(out=res[j][:, 0:n], in_=acc[j][:, 0:n], func=AF.Silu), r=[acc[j]], w=[res[j]])
                if ct >= 16:
                    if ct < 24:
                        k.dma("sp", d_BT[(ct - 16) * 128:(ct - 15) * 128, p0:p0 + n], res[j][:, 0:n], r=[res[j]], w=[d_BT])
                    else:
                        k.dma("sp", d_CT2[(ct - 24) * 128:(ct - 23) * 128, p0:p0 + n], res[j][:, 0:n], r=[res[j]], w=[d_CT2])
                if ct < 24:
                    tp = tps[j]
                    for a in range(npt):
                        k.op("pe", lambda e, a=a: e.transpose(out=tp[:, a, :], in_=res[j][:, a * 128:(a + 1) * 128], identity=ident_b[:]),
                             r=[res[j], ident_b], w=[tp])
                    k.op("act", lambda e: e.copy(out=tk[:, 0:npt, ct * 128:(ct + 1) * 128], in_=tp[:, 0:npt, :]), r=[tp], w=[tk])
            k.dma("sp", d_XH[p0:p0 + n, :].rearrange("(a p) e -> p a e", p=128), tk[:, 0:npt, 0:2048], r=[tk], w=[d_XH])
            k.dma("sp", d_BM[p0:p0 + n, :].rearrange("(a p) e -> p a e", p=128), tk[:, 0:npt, 2048:3072], r=[tk], w=[d_BM])
        k.pop()
    for nm_, src_, w_ in (("XHs", d_XH, 2048), ("BMs", d_BM, 1024)):
        if nm_ in dbg:
            dd = scratch(nm_, [384, w_], BF16)
            k.dma("sp", dd[0:128, :], src_[0:128, :], r=[src_], w=[dd])
            k.dma("sp", dd[128:256, :], src_[T - 128:T, :], r=[src_], w=[dd])
            k.dma("sp", dd[256:384, :], src_[TT - 128:TT, :], r=[src_], w=[dd])
    if "CTs" in dbg:
        dd = scratch("CTs", [1024, 256], BF16)
        k.dma("sp", dd[:, 0:128], d_CT2[:, 0:128], r=[d_CT2], w=[dd])
        k.dma("sp", dd[:, 128:256], d_CT2[:, T - 128:T], r=[d_CT2], w=[dd])

    d_YF = scratch("YF", [T, 2048]); d_YB = scratch("YB", [T, 2048])
    if sel is None and stages >= 3 or (sel is not None and "s" in sel):
        k.push()
        msk = {}
        for nm in ("incl_f", "incl_b", "strict_f", "strict_b"):
            msk[nm] = k.sb("m2_" + nm, [128, 128], F32)
            k.dma("sp", msk[nm][:], cmask128[nm][:, :], w=[msk[nm]])
        ones_s = k.sb("ones_s", [128, 128], F32)
        k.op("pool", lambda e: e.memset(ones_s[:], 1.0), w=[ones_s])
        sv = k.sb("sv", [128, 3, 64], F32)
        k.dma("sp", sv[:], ssd_vec.partition_broadcast(128), w=[sv])
        na = k.sb("na", [128, 64], F32)
        k.op("act", lambda e: e.activation(out=na[:], in_=sv[:, 1, :], func=AF.Exp), r=[sv], w=[na])
        k.op("dve", lambda e: e.tensor_scalar(out=na[:], in0=na[:], scalar1=-1.0, scalar2=None, op0=ALU.mult), r=[na], w=[na])
        NB = 2
        CS = 128
        xh_b = [k.sb("xh_b%d" % i, [128, NB, 2048], BF16) for i in range(2)]
        bm_b = [k.sb("bm_b%d" % i, [128, NB, 1024], BF16) for i in range(2)]
        BT_b = [k.sb("BT_b%d" % i, [128, 8, NB * CS], BF16) for i in range(2)]
        CT_b = [k.sb("CT_b%d" % i, [128, 8, NB * CS], BF16) for i in range(2)]
        dt_b = [k.sb("dt_b%d" % i, [128, NB, 32], F32) for i in range(2)]
        la = k.sb("la", [128, NB, 32], F32)
        yst = k.sb("yst", [128, NB, 2048], F32)
        xdt = k.sb("xdt", [128, 32, 64], BF16)
        xdd = k.sb("xdd", [128, 32, 64], BF16)
        Am = k.sb("Am", [128, 32, 128], F32)
        cbm = [k.sb("cbm%d" % i, [128, 2, 128], F32) for i in range(2)]
        Lh = [k.sb("Lh%d" % i, [128, 8, 128], F32) for i in range(2)]
        Mt = [k.sb("Mt%d" % i, [128, 8, 128], BF16) for i in range(2)]
        tmp = [k.sb("tmp%d" % i, [128, 8, 64], F32) for i in range(2)]
        ex = k.sb("ex", [128, 2, 32], F32)
        EL = k.sb("EL", [128, 32], F32)
        hT = k.sb("hT", [128, 32, 64], F32)
        hTbf = k.sb("hTbf", [128, 32, 64], BF16)
        small_ps = k.ps("small_ps", [128, 3, 32], F32)
        cb_ps = k.ps("cb_ps", [128, 2, 128], F32)
        X_ps = k.ps("X_ps", [128, 8, 128], F32)
        Y_ps = k.ps("Y_ps", [128, 512], F32)
        Z_ps = k.ps("Z_ps", [128, 512], F32)
        S_ps = k.ps("S_ps", [128, 512], F32)

        for d_ in range(2):
            incl = msk["incl_f" if d_ == 0 else "incl_b"]
            strict = msk["strict_f" if d_ == 0 else "strict_b"]
            d_Y = d_YF if d_ == 0 else d_YB
            k.op("pool", lambda e: e.memset(hT[:], 0.0), w=[hT])
            k.op("pool", lambda e: e.memset(hTbf[:], 0.0), w=[hTbf])
            nblk = T // (NB * CS)
            if d_ == 0:
                blocks = [(T, True)] + [(b * NB * CS, False) for b in range(nblk)]
            else:
                blocks = [(T, True)] + [(b * NB * CS, False) for b in reversed(range(nblk))]
            for bi, (p0, is_ctx) in enumerate(blocks):
                j = bi % 2
                n = NB * CS
                k.dma("sp", xh_b[j][:], d_XH[p0:p0 + n, :].rearrange("(c s) e -> s c e", s=CS), r=[d_XH], w=[xh_b[j]])
                k.dma("sp", bm_b[j][:], d_BM[p0:p0 + n, :].rearrange("(c s) e -> s c e", s=CS), r=[d_BM], w=[bm_b[j]])
                k.dma("sp", dt_b[j][:], d_DT[p0:p0 + n, d_ * 32:(d_ + 1) * 32].rearrange("(c s) h -> s c h", s=CS), r=[d_DT], w=[dt_b[j]])
                if not is_ctx:
                    k.dma("sp", BT_b[j][:], d_BT[:, p0:p0 + n].rearrange("(g q) s -> q g s", q=128), r=[d_BT], w=[BT_b[j]])
                    k.dma("sp", CT_b[j][:], d_CT2[:, p0:p0 + n].rearrange("(g q) s -> q g s", q=128), r=[d_CT2], w=[CT_b[j]])
                k.op("dve", lambda e: e.tensor_tensor(out=dt_b[j][:], in0=dt_b[j][:],
                                                      in1=sv[:, 0, d_ * 32:(d_ + 1) * 32].unsqueeze(1).to_broadcast([128, NB, 32]), op=ALU.add),
                     r=[dt_b[j], sv], w=[dt_b[j]])
                k.op("act", lambda e: e.activation(out=dt_b[j][:], in_=dt_b[j][:], func=AF.Exp), r=[dt_b[j]], w=[dt_b[j]])
                k.op("act", lambda e: e.activation(out=dt_b[j][:], in_=dt_b[j][:], func=AF.Ln, bias=1.0, scale=1.0), r=[dt_b[j]], w=[dt_b[j]])
                k.op("dve", lambda e: e.tensor_tensor(out=la[:], in0=dt_b[j][:],
                                                      in1=na[:, d_ * 32:(d_ + 1) * 32].unsqueeze(1).to_broadcast([128, NB, 32]), op=ALU.mult),
                     r=[dt_b[j], na], w=[la])
                corder = list(range(NB)) if d_ == 0 else list(reversed(range(NB)))
                for c in corder:
                    cs = slice(c * CS, (c + 1) * CS)
                    k.op("pe", lambda e: e.matmul(small_ps[:, 0, :], lhsT=incl[:], rhs=la[:, c, :], start=True, stop=True), r=[incl, la], w=[small_ps])
                    k.op("pe", lambda e: e.matmul(small_ps[:, 1, :], lhsT=strict[:], rhs=la[:, c, :], start=True, stop=True), r=[strict, la], w=[small_ps])
                    k.op("pe", lambda e: e.matmul(small_ps[:, 2, :], lhsT=ones_s[:], rhs=la[:, c, :], start=True, stop=True), r=[ones_s, la], w=[small_ps])
                    k.op("act", lambda e: e.activation(out=ex[:], in_=small_ps[:, 0:2, :], func=AF.Exp), r=[small_ps], w=[ex])
                    k.op("act", lambda e: e.activation(out=EL[:], in_=small_ps[:, 2, :], func=AF.Exp), r=[small_ps], w=[EL])
                    k.op("dve", lambda e: e.tensor_tensor(out=xdt[:], in0=xh_b[j][:, c, :].rearrange("s (h p) -> s h p", p=64),
                                                          in1=dt_b[j][:, c, :].unsqueeze(2).to_broadcast([128, 32, 64]), op=ALU.mult),
                         r=[xh_b[j], dt_b[j]], w=[xdt])
                    k.op("pool", lambda e: e.tensor_tensor(out=xdd[:], in0=xdt[:], in1=ex[:, 1, :].unsqueeze(2).to_broadcast([128, 32, 64]), op=ALU.mult),
                         r=[xdt, ex], w=[xdd])
                    if not is_ctx:
                        k.op("dve", lambda e: e.tensor_tensor(out=Am[:], in0=la[:, c, :].unsqueeze(2).to_broadcast([128, 32, 128]),
                                                              in1=strict[:].unsqueeze(1).to_broadcast([128, 32, 128]), op=ALU.mult), r=[la, strict], w=[Am])
                    for q in range(4):
                        hs = slice(q * 8, (q + 1) * 8)
                        qq = q % 2
                        if not is_ctx:
                            for gg in range(2):
                                g = q * 2 + gg
                                k.op("pe", lambda e, gg=gg, g=g: e.matmul(cb_ps[:, gg, :], lhsT=BT_b[j][:, g, cs], rhs=CT_b[j][:, g, cs], start=True, stop=True),
                                     r=[BT_b[j], CT_b[j]], w=[cb_ps])
                            k.op("dve", lambda e: e.tensor_tensor(out=cbm[qq][:], in0=cb_ps[:], in1=incl[:].unsqueeze(1).to_broadcast([128, 2, 128]), op=ALU.mult),
                                 r=[cb_ps, incl], w=[cbm[qq]])
                            for hh in range(8):
                                k.op("pe", lambda e, hh=hh: e.matmul(X_ps[:, hh, :], lhsT=Am[:, q * 8 + hh, :], rhs=incl[:], start=True, stop=True),
                                     r=[Am, incl], w=[X_ps])
                            k.op("act", lambda e: e.activation(out=Lh[qq][:], in_=X_ps[:], func=AF.Exp), r=[X_ps], w=[Lh[qq]])
                            k.op("dve", lambda e: e.tensor_tensor(out=Mt[qq][:].rearrange("s (g a) t -> s g a t", a=4),
                                                                  in0=Lh[qq][:].rearrange("s (g a) t -> s g a t", a=4),
                                                                  in1=cbm[qq][:].unsqueeze(2).to_broadcast([128, 2, 4, 128]), op=ALU.mult),
                                 r=[Lh[qq], cbm[qq]], w=[Mt[qq]])
                            for hh in range(8):
                                k.op("pe", lambda e, hh=hh: e.matmul(Y_ps[:, hh * 64:(hh + 1) * 64], lhsT=Mt[qq][:, hh, :], rhs=xdt[:, q * 8 + hh, :],
                                                                     start=True, stop=True), r=[Mt[qq], xdt], w=[Y_ps])
                            for gg in range(2):
                                g = q * 2 + gg
                                k.op("pe", lambda e, gg=gg, g=g: e.matmul(Z_ps[:, gg * 256:(gg + 1) * 256], lhsT=CT_b[j][:, g, cs],
                                                                          rhs=hTbf[:, g * 4:(g + 1) * 4, :].rearrange("q a p -> q (a p)"),
                                                                          start=True, stop=True), r=[CT_b[j], hTbf], w=[Z_ps])
                            k.op("dve", lambda e: e.tensor_tensor(out=tmp[qq][:], in0=Z_ps[:].rearrange("s (h p) -> s h p", p=64),
                                                                  in1=ex[:, 0, hs].unsqueeze(2).to_broadcast([128, 8, 64]), op=ALU.mult),
                                 r=[Z_ps, ex], w=[tmp[qq]])
                            yv = yst[:, c, q * 512:(q + 1) * 512]
                            k.op("dve", lambda e: e.tensor_tensor(out=yv, in0=Y_ps[:], in1=tmp[qq][:].rearrange("s h p -> s (h p)"), op=ALU.add),
                                 r=[Y_ps, tmp[qq]], w=[yst])
                            if d_ == 0:
                                k.op("pool", lambda e: e.tensor_tensor(out=tmp[qq][:], in0=xh_b[j][:, c, q * 512:(q + 1) * 512].rearrange("s (h p) -> s h p", p=64),
                                                                      in1=sv[:, 2, hs].unsqueeze(2).to_broadcast([128, 8, 64]), op=ALU.mult),
                                     r=[xh_b[j], sv], w=[tmp[qq]])
                                k.op("pool", lambda e: e.tensor_tensor(out=yv, in0=yv, in1=tmp[qq][:].rearrange("s h p -> s (h p)"), op=ALU.add),
                                     r=[yst, tmp[qq]], w=[yst])
                        for gg in range(2):
                            g = q * 2 + gg
                            k.op("pe", lambda e, gg=gg, g=g: e.matmul(S_ps[:, gg * 256:(gg + 1) * 256], lhsT=bm_b[j][:, c, g * 128:(g + 1) * 128],
                                                                      rhs=xdd[:, g * 4:(g + 1) * 4, :].rearrange("s a p -> s (a p)"),
                                                                      start=True, stop=True), r=[bm_b[j], xdd], w=[S_ps])
                        k.op("dve", lambda e: e.tensor_tensor(out=hT[:, hs, :], in0=hT[:, hs, :], in1=EL[:, hs].unsqueeze(2).to_broadcast([128, 8, 64]),
                                                              op=ALU.mult), r=[hT, EL], w=[hT])
                        k.op("dve", lambda e: e.tensor_tensor(out=hT[:, hs, :], in0=hT[:, hs, :], in1=S_ps[:].rearrange("q (h p) -> q h p", p=64),
                                                              op=ALU.add), r=[hT, S_ps], w=[hT])
                        k.op("act", lambda e: e.copy(out=hTbf[:, hs, :], in_=hT[:, hs, :]), r=[hT], w=[hTbf])
                if not is_ctx:
                    k.dma("sp", d_Y[p0:p0 + n, :].rearrange("(c s) e -> s c e", s=CS), yst[:], r=[yst], w=[d_Y])
            k.barrier()
        k.pop()
    for nm_, src_ in (("YFs", d_YF), ("YBs", d_YB)):
        if nm_ in dbg:
            dd = scratch(nm_, [256, 2048])
            k.dma("sp", dd[0:128, :], src_[0:128, :], r=[src_], w=[dd])
            k.dma("sp", dd[128:256, :], src_[T - 128:T, :], r=[src_], w=[dd])

    d_YZ = scratch("YZ", [T, 2048], BF16)
    if sel is None and stages >= 3 or (sel is not None and "g" in sel):
        k.push()
        mnw = k.sb("mnw", [128, 2048], F32)
        k.dma("sp", mnw[:], ma_nw.partition_broadcast(128), w=[mnw])
        yfb = [k.sb("yfb%d" % i, [128, 2048], F32) for i in range(2)]
        ybb = [k.sb("ybb%d" % i, [128, 2048], F32) for i in range(2)]
        zb = [k.sb("zb%d" % i, [128, 2048], F32) for i in range(2)]
        junk = k.sb("junk", [128, 256], F32)
        ssq = [k.sb("ssq%d" % i, [128, 8], F32) for i in range(2)]
        yzo = [k.sb("yzo%d" % i, [128, 2048], BF16) for i in range(2)]
        d_YZ_v = d_YZ.t.rearrange("(r c) e -> r c e", c=GW)
        for c in range(GW):
            j = c % 2
            k.dma("sp", yfb[j][:], d_YF[c * 128:(c + 1) * 128, :], r=[d_YF], w=[yfb[j]])
            k.dma("sp", ybb[j][:], d_YB[c * 128:(c + 1) * 128, :], r=[d_YB], w=[ybb[j]])
            k.dma("sp", zb[j][:], d_Z[c * 128:(c + 1) * 128, :], r=[d_Z], w=[zb[j]])
            k.op("pool", lambda e: e.tensor_tensor(out=yfb[j][:], in0=yfb[j][:], in1=ybb[j][:], op=ALU.add), r=[yfb[j], ybb[j]], w=[yfb[j]])
            k.op("dve", lambda e: e.tensor_tensor(out=yfb[j][:], in0=yfb[j][:], in1=zb[j][:], op=ALU.mult), r=[yfb[j], zb[j]], w=[yfb[j]])
            for g in range(8):
                k.op("act", lambda e, g=g: e.activation(out=junk[:], in_=yfb[j][:, g * 256:(g + 1) * 256], func=AF.Square, accum_out=ssq[j][:, g:g + 1]),
                     r=[yfb[j]], w=[junk, ssq[j]])
            k.op("act", lambda e: e.activation(out=ssq[j][:], in_=ssq[j][:], func=AF.Sqrt, bias=eps_t[:], scale=1.0 / 256.0), r=[ssq[j], eps_t], w=[ssq[j]])
            k.op("dve", lambda e: e.reciprocal(out=ssq[j][:], in_=ssq[j][:]), r=[ssq[j]], w=[ssq[j]])
            k.op("dve", lambda e: e.tensor_tensor(out=yfb[j][:].rearrange("p (g q) -> p g q", q=256), in0=yfb[j][:].rearrange("p (g q) -> p g q", q=256),
                                                  in1=ssq[j][:].unsqueeze(2).to_broadcast([128, 8, 256]), op=ALU.mult), r=[yfb[j], ssq[j]], w=[yfb[j]])
            k.op("pool", lambda e: e.tensor_tensor(out=yzo[j][:], in0=yfb[j][:], in1=mnw[:], op=ALU.mult), r=[yfb[j], mnw], w=[yzo[j]])
            k.dma("sp", d_YZ_v[:, c, :], yzo[j][:], r=[yzo[j]], w=[d_YZ])
        k.pop()
    if "YZs" in dbg:
        dd = scratch("YZs", [256, 2048], BF16)
        k.dma("sp", dd[0:128, :], d_YZ[0:128, :], r=[d_YZ], w=[dd])
        k.dma("sp", dd[128:256, :], d_YZ[T - 128:T, :], r=[d_YZ], w=[dd])

    d_X1 = scratch("X1", [T, 1024]); d_U2T = scratch("U2T", [1024, T], BF16)

    def bcast_rows(cols_ap, dst, tag):
        k.push()
        tpp = k.ps("bc_tp" + tag, [8, 128], F32)
        bcp = k.ps("bc_ps" + tag, [128, 1024], F32)
        gT = k.sb("bc_gT" + tag, [8, 128], F32)
        blk = k.sb("bc_blk" + tag, [8, 8, 128], F32)
        on8 = k.sb("bc_on" + tag, [8, 128], F32)
        k.op("pool", lambda e: e.memset(on8[:], 1.0), w=[on8])
        k.op("pe", lambda e: e.transpose(out=tpp[:], in_=cols_ap, identity=ident_f[:]), r=[modT, ident_f], w=[tpp])
        k.op("dve", lambda e: e.tensor_copy(out=gT[:], in_=tpp[:]), r=[tpp], w=[gT])
        k.op("dve", lambda e: e.tensor_tensor(out=blk[:], in0=gT[:].unsqueeze(1).to_broadcast([8, 8, 128]),
                                              in1=ident_f[0:8, 0:8].unsqueeze(2).to_broadcast([8, 8, 128]), op=ALU.mult), r=[gT, ident_f], w=[blk])
        for hf in range(2):
            k.op("pe", lambda e, hf=hf: e.matmul(bcp[:, hf * 512:(hf + 1) * 512], lhsT=on8[:], rhs=blk[:].rearrange("a b c -> a (b c)")[:, hf * 512:(hf + 1) * 512],
                                                 start=True, stop=True), r=[on8, blk], w=[bcp])
            k.op("dve", lambda e, hf=hf: e.tensor_copy(out=dst[:, hf * 512:(hf + 1) * 512], in_=bcp[:, hf * 512:(hf + 1) * 512]), r=[bcp], w=[dst])
        k.pop()

    def ln_sb(src, dst_ap, dst, st6_, mv_, rstd_, nmr_):
        for h in range(2):
            k.op("dve", lambda e, h=h: e.bn_stats(out=st6_[:, h, :], in_=src[:, h * 512:(h + 1) * 512]), r=[src], w=[st6_])
        k.op("dve", lambda e: e.bn_aggr(out=mv_[:], in_=st6_[:].rearrange("p a b -> p (a b)")), r=[st6_], w=[mv_])
        k.op("act", lambda e: e.activation(out=rstd_[:], in_=mv_[:, 1:2], func=AF.Sqrt, bias=eps_t[:], scale=1.0), r=[mv_, eps_t], w=[rstd_])
        k.op("dve", lambda e: e.reciprocal(out=rstd_[:], in_=rstd_[:]), r=[rstd_], w=[rstd_])
        k.op("dve", lambda e: e.scalar_tensor_tensor(out=nmr_[:], in0=mv_[:, 0:1], scalar=-1.0, in1=rstd_[:], op0=ALU.mult, op1=ALU.mult),
             r=[mv_, rstd_], w=[nmr_])
        k.op("act", lambda e: e.activation(out=dst_ap, in_=src[:], func=AF.Identity, bias=nmr_[:], scale=rstd_[:]), r=[src, nmr_, rstd_], w=[dst])

    if sel is None and stages >= 4 or (sel is not None and "m" in sel):
        k.push()
        wa = k.sb("wa", [128, 8, 1024], BF16)
        wb = k.sb("wb", [128, 16, 1024], BF16)
        wo = k.sb("wo", [128, 8, 1024], BF16)
        g1b = k.sb("g1b", [128, 1024], F32)
        l1g = k.sb("l1g", [128, 1024], F32)
        l1b = k.sb("l1b", [128, 1024], F32)
        k.dma("sp", l1g[:], ln1_g.partition_broadcast(128), w=[l1g])
        k.dma("sp", l1b[:], ln1_b.partition_broadcast(128), w=[l1b])
        k.push()
        wstg = [k.sb("wstg%d" % i, [128, 4, 1024], F32) for i in range(2)]
        ci = 0
        for (dst, src, nk) in ((wa, w_ba, 8), (wb, w_bb, 16), (wo, w_o, 8)):
            sv_ = src.rearrange("(kk p) e -> p kk e", p=128)
            for k0 in range(0, nk, 4):
                st = wstg[ci % 2]
                ci += 1
                k.dma("sp", st[:], sv_[:, k0:k0 + 4, :], w=[st])
                for q in range(4):
                    k.op("pool" if q % 2 == 0 else "dve", lambda e, q=q: e.tensor_copy(out=dst[:, k0 + q, :], in_=st[:, q, :]), r=[st], w=[dst])
        k.pop()
        bcast_rows(modT[:, 16:24, 0], g1b, "g1")
        TB = 256
        ont2 = [k.sb("ont%d" % i, [128, 8, TB], BF16) for i in range(2)]
        gat2 = [k.sb("gat%d" % i, [128, 8, TB], F32) for i in range(2)]
        gbt2 = [k.sb("gbt%d" % i, [128, 8, TB], F32) for i in range(2)]
        yzt2 = [k.sb("yzt%d" % i, [128, 2, 2048], BF16) for i in range(2)]
        xtl2 = [k.sb("xtl%d" % i, [128, 2, 1024], F32) for i in range(2)]
        yzT = k.sb("yzT", [128, 16, TB], BF16)
        m1_2 = [k.sb("s4_m1_%d" % i, [128, TB], F32) for i in range(2)]
        m2_2 = [k.sb("s4_m2_%d" % i, [128, TB], F32) for i in range(2)]
        mT = k.sb("mT", [128, 8, TB], BF16)
        t1 = k.sb("s4_t1", [128, 1024], F32)
        xr = k.sb("s4_xr", [128, 1024], F32)
        xn1 = k.sb("s4_xn1", [128, 1024], F32)
        x1t = k.sb("x1t", [128, 1024], F32)
        xn2 = k.sb("s4_xn2", [128, 1024], BF16)
        u2b = k.sb("u2b", [128, 8, TB], BF16)
        st6_ = k.sb("s4_st6", [128, 2, 6], F32); mv_ = k.sb("s4_mv", [128, 2], F32)
        rstd_ = k.sb("s4_rstd", [128, 1], F32); nmr_ = k.sb("s4_nmr", [128, 1], F32)
        ya_ps2 = [k.ps("ya_ps%d" % i, [128, TB], F32) for i in range(2)]
        yb_ps2 = [k.ps("yb_ps%d" % i, [128, TB], F32) for i in range(2)]
        trp = k.ps("trp", [128, 8, 128], BF16)
        mix_ps = k.ps("mix_ps", [128, 1024], F32)
        u2p = k.ps("u2p", [128, 8, 128], BF16)
        if "MIXs" in dbg:
            d_MIX = scratch("MIXs", [256, 1024])
            mixs = k.sb("mixs", [128, 1024], F32)
        for b_ in range(T // TB):
            t0 = b_ * TB
            ont, gat, gbt, yzt, xtl = ont2[b_ % 2], gat2[b_ % 2], gbt2[b_ % 2], yzt2[b_ % 2], xtl2[b_ % 2]
            k.dma("sp", ont[:], d_ONT[:, t0:t0 + TB].rearrange("(kk p) t -> p kk t", p=128), r=[d_ONT], w=[ont])
            k.dma("sp", gat[:], d_GAT[:, t0:t0 + TB].rearrange("(kk p) t -> p kk t", p=128), r=[d_GAT], w=[gat])
            k.dma("sp", gbt[:], d_GBT[:, t0:t0 + TB].rearrange("(kk p) t -> p kk t", p=128), r=[d_GBT], w=[gbt])
            k.dma("sp", yzt[:], d_YZ[t0:t0 + TB, :].rearrange("(a p) e -> p a e", p=128), r=[d_YZ], w=[yzt])
            k.dma("sp", xtl[:], x[t0:t0 + TB, :].rearrange("(a p) e -> p a e", p=128), w=[xtl])
            for a in range(TB // 128):
                for hf in range(2):
                    for q in range(8):
                        k.op("pe", lambda e, q=q: e.transpose(out=trp[:, q, :], in_=yzt[:, a, (hf * 8 + q) * 128:(hf * 8 + q + 1) * 128], identity=ident_b[:]),
                             r=[yzt, ident_b], w=[trp])
                    k.op("act", lambda e: e.copy(out=yzT[:, hf * 8:(hf + 1) * 8, a * 128:(a + 1) * 128], in_=trp[:]), r=[trp], w=[yzT])
            for db in range(8):
                ya_ps, yb_ps, m1, m2 = ya_ps2[db % 2], yb_ps2[db % 2], m1_2[db % 2], m2_2[db % 2]
                for kk in range(8):
                    k.op("pe", lambda e, kk=kk: e.matmul(ya_ps[:], lhsT=wa[:, kk, db * 128:(db + 1) * 128], rhs=ont[:, kk, :], start=(kk == 0), stop=(kk == 7)),
                         r=[wa, ont], w=[ya_ps])
                for kk in range(16):
                    k.op("pe", lambda e, kk=kk: e.matmul(yb_ps[:], lhsT=wb[:, kk, db * 128:(db + 1) * 128], rhs=yzT[:, kk, :], start=(kk == 0), stop=(kk == 15)),
                         r=[wb, yzT], w=[yb_ps])
                k.op("dve", lambda e: e.tensor_tensor(out=m1[:], in0=ya_ps[:], in1=gat[:, db, :], op=ALU.mult), r=[ya_ps, gat], w=[m1])
                k.op("dve", lambda e: e.tensor_tensor(out=m2[:], in0=yb_ps[:], in1=gbt[:, db, :], op=ALU.mult), r=[yb_ps, gbt], w=[m2])
                k.op("pool", lambda e: e.tensor_tensor(out=mT[:, db, :], in0=m1[:], in1=m2[:], op=ALU.add), r=[m1, m2], w=[mT])
            for a in range(TB // 128):
                for hf in range(2):
                    for kk in range(8):
                        k.op("pe", lambda e, kk=kk: e.matmul(mix_ps[:, hf * 512:(hf + 1) * 512], lhsT=mT[:, kk, a * 128:(a + 1) * 128],
                                                             rhs=wo[:, kk, hf * 512:(hf + 1) * 512], start=(kk == 0), stop=(kk == 7)), r=[mT, wo], w=[mix_ps])
                if "MIXs" in dbg and (b_ == 0 or b_ == T // TB - 1) and a == (0 if b_ == 0 else 1):
                    for hf in range(2):
                        k.op("dve", lambda e, hf=hf: e.tensor_copy(out=mixs[:, hf * 512:(hf + 1) * 512], in_=mix_ps[:, hf * 512:(hf + 1) * 512]), r=[mix_ps], w=[mixs])
                    o_ = 0 if b_ == 0 else 128
                    k.dma("sp", d_MIX[o_:o_ + 128, :], mixs[:], r=[mixs], w=[d_MIX])
                for hf in range(2):
                    k.op("dve", lambda e, hf=hf: e.tensor_tensor(out=t1[:, hf * 512:(hf + 1) * 512], in0=mix_ps[:, hf * 512:(hf + 1) * 512],
                                                                 in1=g1b[:, hf * 512:(hf + 1) * 512], op=ALU.mult), r=[mix_ps, g1b], w=[t1])
                k.op("dve", lambda e: e.scalar_tensor_tensor(out=xr[:], in0=xtl[:, a, :], scalar=DN_ALPHA, in1=t1[:], op0=ALU.mult, op1=ALU.add),
                     r=[xtl, t1], w=[xr])
                ln_sb(xr, xn1[:], xn1, st6_, mv_, rstd_, nmr_)
                k.op("pool", lambda e: e.tensor_tensor(out=xn1[:], in0=xn1[:], in1=l1g[:], op=ALU.mult), r=[xn1, l1g], w=[xn1])
                k.op("pool", lambda e: e.tensor_tensor(out=x1t[:], in0=xn1[:], in1=l1b[:], op=ALU.add), r=[xn1, l1b], w=[x1t])
                k.dma("sp", d_X1[t0 + a * 128:t0 + (a + 1) * 128, :], x1t[:], r=[x1t], w=[d_X1])
                ln_sb(x1t, xn2[:], xn2, st6_, mv_, rstd_, nmr_)
                for kk in range(8):
                    k.op("pe", lambda e, kk=kk: e.transpose(out=u2p[:, kk, :], in_=xn2[:, kk * 128:(kk + 1) * 128], identity=ident_b[:]),
                         r=[xn2, ident_b], w=[u2p])
                for kk in range(8):
                    k.op("act", lambda e, kk=kk: e.activation(out=u2b[:, kk, a * 128:(a + 1) * 128], in_=u2p[:, kk, :], func=AF.Identity,
                                                              bias=modT[:, 24 + kk, 0:1], scale=ops2[:, kk, 0:1]), r=[u2p, modT, ops2], w=[u2b])
            k.dma("sp", d_U2T[:, t0:t0 + TB].rearrange("(kk p) t -> p kk t", p=128), u2b[:], r=[u2b], w=[d_U2T])
        k.pop()
    if "X1s" in dbg:
        dd = scratch("X1s", [256, 1024])
        k.dma("sp", dd[0:128, :], d_X1[0:128, :], r=[d_X1], w=[dd])
        k.dma("sp", dd[128:256, :], d_X1[T - 128:T, :], r=[d_X1], w=[dd])
    if "U2s" in dbg:
        dd = scratch("U2s", [1024, 256], BF16)
        k.dma("sp", dd[:, 0:128], d_U2T[:, 0:128], r=[d_U2T], w=[dd])
        k.dma("sp", dd[:, 128:256], d_U2T[:, T - 128:T], r=[d_U2T], w=[dd])

    d_VB = scratch("VB", [16384, 1024], BF16)
    d_UT = scratch("UT", [128, 128, 1024], BF16)
    d_Q2 = scratch("Q2", [2048, T], BF16)
    d_FFN = scratch("FFN", [T, 1024])
    run5 = sel is None and stages >= 5 or (sel is not None and "p" in sel)
    if run5:
        k.push()
        vst = [k.sb("p_vst%d" % i, [128, 4, 1024], F32) for i in range(3)]
        vbf = [k.sb("p_vbf%d" % i, [128, 4, 1024], BF16) for i in range(3)]
        utb = [k.sb("p_utb%d" % i, [128, 4, 1024], BF16) for i in range(3)]
        ctp = [k.ps("p_ctp%d" % i, [128, 8, 128], BF16) for i in range(4)]
        pv_v = peer_v.rearrange("(i p) d -> p i d", p=128)
        pu_v = peer_u.rearrange("(i p) d -> p i d", p=128)
        d_VB_v = d_VB.t.rearrange("(i p) d -> p i d", p=128)
        d_UT_v = d_UT.t.rearrange("i p e -> p i e")
        for it in range(32):
            j = it % 3
            i0 = it * 4
            k.dma("sp", vst[j][:], pv_v[:, i0:i0 + 4, :], w=[vst[j]])
            for q in range(4):
                k.op("pool" if q % 2 == 0 else "dve", lambda e, q=q: e.tensor_copy(out=vbf[j][:, q, :], in_=vst[j][:, q, :]), r=[vst[j]], w=[vbf[j]])
            k.dma("sp", d_VB_v[:, i0:i0 + 4, :], vbf[j][:], r=[vbf[j]], w=[d_VB])
        for it in range(32):
            j = it % 3
            i0 = it * 4
            k.dma("sp", vst[j][:], pu_v[:, i0:i0 + 4, :], w=[vst[j]])
            for q in range(4):
                k.op("pool" if q % 2 == 0 else "dve", lambda e, q=q: e.tensor_copy(out=vbf[j][:, q, :], in_=vst[j][:, q, :]), r=[vst[j]], w=[vbf[j]])
            for q in range(4):
                tp = ctp[q % 4]
                for kk in range(8):
                    k.op("pe", lambda e, kk=kk, q=q: e.transpose(out=tp[:, kk, :], in_=vbf[j][:, q, kk * 128:(kk + 1) * 128], identity=ident_b[:]),
                         r=[vbf[j], ident_b], w=[tp])
                k.op("act", lambda e, q=q: e.copy(out=utb[j][:, q, :].rearrange("p (kk j) -> p kk j", j=128), in_=tp[:]), r=[tp], w=[utb[j]])
            k.dma("sp", d_UT_v[:, i0:i0 + 4, :], utb[j][:], r=[utb[j]], w=[d_UT])
        k.pop()
        k.push()
        wqs = k.sb("p_wqs", [128, 8, 512], F32)
        wqb = k.sb("p_wqb", [128, 8, 2048], BF16)
        wq_v = peer_wq.rearrange("(kk p) e -> p kk e", p=128)
        for pc in range(4):
            k.dma("sp", wqs[:], wq_v[:, :, pc * 512:(pc + 1) * 512], w=[wqs])
            for kk in range(8):
                k.op("pool" if kk % 2 == 0 else "dve", lambda e, kk=kk: e.tensor_copy(out=wqb[:, kk, pc * 512:(pc + 1) * 512], in_=wqs[:, kk, :]), r=[wqs], w=[wqb])
        u2l = [k.sb("p_u2l%d" % i, [128, 8, 512], BF16) for i in range(2)]
        qst = [k.sb("p_qst%d" % i, [128, 512], BF16) for i in range(2)]
        qps = [k.ps("p_qps%d" % i, [128, 512], F32) for i in range(2)]
        n_ = 0
        for b_ in range(T // 512):
            j = b_ % 2
            t0 = b_ * 512
            k.dma("sp", u2l[j][:], d_U2T[:, t0:t0 + 512].rearrange("(kk p) t -> p kk t", p=128), r=[d_U2T], w=[u2l[j]])
            for g in range(16):
                jj = n_ % 2
                n_ += 1
                for kk in range(8):
                    k.op("pe", lambda e, kk=kk: e.matmul(qps[jj][:], lhsT=wqb[:, kk, g * 128:(g + 1) * 128], rhs=u2l[j][:, kk, :], start=(kk == 0), stop=(kk == 7)),
                         r=[wqb, u2l[j]], w=[qps[jj]])
                k.op("act" if g % 2 == 0 else "dve", (lambda e: e.copy(out=qst[jj][:], in_=qps[jj][:])) if g % 2 == 0 else
                     (lambda e: e.tensor_copy(out=qst[jj][:], in_=qps[jj][:])), r=[qps[jj]], w=[qst[jj]])
                k.dma("sp", d_Q2[g * 128:(g + 1) * 128, t0:t0 + 512], qst[jj][:], r=[qst[jj]], w=[d_Q2])
        k.pop()

    if run5:
        k.push()
        subT = k.sb("p_subT", [128, 16, 128], BF16)
        io128 = k.sb("p_io128", [128, 128], F32)
        g2b = k.sb("p_g2b", [128, 1024], F32)
        l2g = k.sb("p_l2g", [128, 1024], F32)
        l2b = k.sb("p_l2b", [128, 1024], F32)
        k.dma("sp", io128[:], iota_c.partition_broadcast(128), w=[io128])
        k.dma("sp", l2g[:], ln2_g.partition_broadcast(128), w=[l2g])
        k.dma("sp", l2b[:], ln2_b.partition_broadcast(128), w=[l2b])
        bcast_rows(modT[:, 40:48, 0], g2b, "g2")
        k.push()
        sks = k.sb("p_sks", [128, 16, 128], F32)
        skb = k.sb("p_skb", [128, 16, 128], BF16)
        stp = k.ps("p_stp", [128, 8, 128], BF16)
        k.dma("sp", sks[:], peer_sub.rearrange("g n q -> n g q"), w=[sks])
        k.op("dve", lambda e: e.tensor_copy(out=skb[:], in_=sks[:]), r=[sks], w=[skb])
        for hf in range(2):
            for q in range(8):
                k.op("pe", lambda e, q=q: e.transpose(out=stp[:, q, :], in_=skb[:, hf * 8 + q, :], identity=ident_b[:]), r=[skb, ident_b], w=[stp])
            k.op("act", lambda e: e.copy(out=subT[:, hf * 8:(hf + 1) * 8, :], in_=stp[:]), r=[stp], w=[subT])
        k.pop()
        TB5 = 256
        pb = [k.ps("p_pb%d" % i, [128, 512], F32) for i in range(8)]
        qb2 = [k.sb("p_qb%d" % i, [128, 16, TB5], BF16) for i in range(2)]
        u2b_ = k.sb("p_u2b", [128, 8, TB5], BF16)
        W1 = k.sb("p_W1", [128, 2048], F32)
        W2 = k.sb("p_W2", [128, 2048], F32)
        vals = k.sb("p_vals", [128, 16, 16], F32)
        idxu = k.sb("p_idxu", [128, 16, 16], U32)
        idxf = k.sb("p_idxf", [128, 16, 16], F32)
        tv = k.sb("p_tv", [128, 8, 16], F32)
        posu = k.sb("p_posu", [128, 8, 16], U32)
        pau = k.sb("p_pau", [128, 8, 16], U32)
        pbu = k.sb("p_pbu", [128, 8, 16], U32)
        paf = k.sb("p_paf", [128, 8, 16], F32)
        pbf = k.sb("p_pbf", [128, 8, 16], F32)
        gate = k.sb("p_gate", [128, 8, 16], F32)
        ssum = k.sb("p_ssum", [128, 8], F32)
        iij = k.sb("p_iij", [128, 2, 128], F32)
        trT2 = [[k.sb("p_trT%d_%d" % (i, a_), [128, 3, 128], F32) for a_ in range(2)] for i in range(2)]
        Aoh2 = [k.sb("p_A%d" % i, [128, 32, 128], BF16) for i in range(2)]
        Boh2 = [k.sb("p_B%d" % i, [128, 32, 128], BF16) for i in range(2)]
        trTb = k.sb("p_trTb", [128, 3, 128], BF16)
        io128b = k.sb("p_io128b", [128, 128], BF16)
        k.op("dve", lambda e: e.tensor_copy(out=io128b[:], in_=io128[:]), r=[io128], w=[io128b])
        Wt = k.sb("p_Wt", [128, 128, TB5], BF16)
        ut = [k.sb("p_ut%d" % i, [128, 2, 1024], BF16) for i in range(3)]
        vb = [k.sb("p_vb%d" % i, [128, 2, 1024], BF16) for i in range(3)]
        Gf = [k.sb("p_G%d" % i, [128, TB5], F32) for i in range(4)]
        Gw = [k.sb("p_Gw%d" % i, [128, TB5], BF16) for i in range(4)]
        x1l = k.sb("p_x1l", [128, 1024], F32)
        t5 = k.sb("p_t5", [128, 1024], F32)
        xr5 = k.sb("p_xr5", [128, 1024], F32)
        st6_ = k.sb("p_st6", [128, 2, 6], F32); mv_ = k.sb("p_mv", [128, 2], F32)
        rstd_ = k.sb("p_rstd", [128, 1], F32); nmr_ = k.sb("p_nmr", [128, 1], F32)
        S_v = W1[:].rearrange("p (g n) -> p g n", n=128)
        S2_v = W2[:].rearrange("p (g n) -> p g n", n=128)
        cand_v = W1[:].rearrange("p (h c) -> p h c", c=256)
        cand2_v = W2[:].rearrange("p (h c) -> p h c", c=256)
        vals_v = vals[:].rearrange("p (h f) a -> p h f a", f=2)
        idxf_v = idxf[:].rearrange("p (h f) a -> p h f a", f=2)
        oh_v = W1[:].rearrange("p (h q a) -> p h q a", q=16, a=16)
        NEG = -1.0e30
        if "FFNs" in dbg:
            d_FFNs = scratch("FFNs", [256, 1024])
            ffs = xr5
        if "PKs" in dbg:
            d_PK = scratch("PKs", [128, 3, 128])
        nblk5 = T // TB5
        blist = list(range(nblk5)) if not isinstance(stages, (set, list, tuple)) or "P" not in stages else [0, nblk5 - 1]

        def gen_5b1(b_, par):
            t0 = b_ * TB5
            qb_ = qb2[par]
            k.dma("sp", qb_[:], d_Q2[:, t0:t0 + TB5].rearrange("(g q) t -> q g t", q=128), r=[d_Q2], w=[qb_])
            for a in range(TB5 // 128):
                ts_ = slice(a * 128, (a + 1) * 128)
                for q in range(4):
                    for gq in range(4):
                        g = q * 4 + gq
                        k.op("pe", lambda e, g=g, gq=gq: e.matmul(pb[7][:, gq * 128:(gq + 1) * 128], lhsT=qb_[:, g, ts_], rhs=subT[:, g, :], start=True, stop=True),
                             r=[qb_, subT], w=[pb[7]])
                    k.op("act", lambda e, q=q: e.copy(out=W1[:, q * 512:(q + 1) * 512], in_=pb[7][:]), r=[pb[7]], w=[W1])
                    yield
                for g in range(16):
                    k.op("dve", lambda e, g=g: e.max(out=vals[:, g, 0:8], in_=S_v[:, g, :]), r=[W1], w=[vals])
                    k.op("dve", lambda e, g=g: e.match_replace(out=S2_v[:, g, :], in_to_replace=vals[:, g, 0:8], in_values=S_v[:, g, :], imm_value=NEG),
                         r=[W1, vals], w=[W2])
                    k.op("dve", lambda e, g=g: e.max(out=vals[:, g, 8:16], in_=S2_v[:, g, :]), r=[W2], w=[vals])
                    k.op("dve", lambda e, g=g: e.max_index(out=idxu[:, g, 0:8], in_max=vals[:, g, 0:8], in_values=S_v[:, g, :]), r=[W1, vals], w=[idxu])
                    k.op("dve", lambda e, g=g: e.max_index(out=idxu[:, g, 8:16], in_max=vals[:, g, 8:16], in_values=S2_v[:, g, :]), r=[W2, vals], w=[idxu])
                    yield
                k.op("dve", lambda e: e.tensor_copy(out=idxf[:], in_=idxu[:]), r=[idxu], w=[idxf])
                k.op("dve", lambda e: e.tensor_tensor(out=W1[:].rearrange("p (h a b) -> p h a b", a=16, b=16),
                                                      in0=vals_v[:, :, 0, :].unsqueeze(3).to_broadcast([128, 8, 16, 16]),
                                                      in1=vals_v[:, :, 1, :].unsqueeze(2).to_broadcast([128, 8, 16, 16]), op=ALU.add), r=[vals], w=[W1])
                yield
                for h in range(8):
                    k.op("dve", lambda e, h=h: e.max(out=tv[:, h, 0:8], in_=cand_v[:, h, :]), r=[W1], w=[tv])
                    k.op("dve", lambda e, h=h: e.match_replace(out=cand2_v[:, h, :], in_to_replace=tv[:, h, 0:8], in_values=cand_v[:, h, :], imm_value=NEG),
                         r=[W1, tv], w=[W2])
                    k.op("dve", lambda e, h=h: e.max(out=tv[:, h, 8:16], in_=cand2_v[:, h, :]), r=[W2], w=[tv])
                    k.op("dve", lambda e, h=h: e.max_index(out=posu[:, h, 0:8], in_max=tv[:, h, 0:8], in_values=cand_v[:, h, :]), r=[W1, tv], w=[posu])
                    k.op("dve", lambda e, h=h: e.max_index(out=posu[:, h, 8:16], in_max=tv[:, h, 8:16], in_values=cand2_v[:, h, :]), r=[W2, tv], w=[posu])
                    yield
                k.op("dve", lambda e: e.tensor_tensor(out=gate[:], in0=tv[:], in1=tv[:, :, 0:1].to_broadcast([128, 8, 16]), op=ALU.subtract), r=[tv], w=[gate])
                k.op("act", lambda e: e.activation(out=gate[:], in_=gate[:], func=AF.Exp), r=[gate], w=[gate])
                k.op("dve", lambda e: e.tensor_reduce(out=ssum[:], in_=gate[:], axis=AX.X, op=ALU.add), r=[gate], w=[ssum])
                k.op("dve", lambda e: e.reciprocal(out=ssum[:], in_=ssum[:]), r=[ssum], w=[ssum])
                k.op("dve", lambda e: e.tensor_tensor(out=gate[:], in0=gate[:], in1=ssum[:].unsqueeze(2).to_broadcast([128, 8, 16]), op=ALU.mult), r=[gate, ssum], w=[gate])
                yield
                k.op("dve", lambda e: e.tensor_single_scalar(out=pau[:], in_=posu[:], scalar=4, op=ALU.logical_shift_right), r=[posu], w=[pau])
                k.op("dve", lambda e: e.tensor_single_scalar(out=pbu[:], in_=posu[:], scalar=15, op=ALU.bitwise_and), r=[posu], w=[pbu])
                k.op("dve", lambda e: e.tensor_copy(out=paf[:], in_=pau[:]), r=[pau], w=[paf])
                k.op("dve", lambda e: e.tensor_copy(out=pbf[:], in_=pbu[:]), r=[pbu], w=[pbf])
                yield
                for w_, pf in ((0, paf), (1, pbf)):
                    k.op("dve", lambda e: e.tensor_tensor(out=oh_v, in0=pf[:].unsqueeze(3).to_broadcast([128, 8, 16, 16]),
                                                          in1=io128[:, 0:16].unsqueeze(1).unsqueeze(1).to_broadcast([128, 8, 16, 16]), op=ALU.is_equal),
                         r=[pf, io128], w=[W1])
                    yield
                    k.op("dve", lambda e: e.tensor_tensor(out=oh_v, in0=oh_v, in1=idxf_v[:, :, w_, :].unsqueeze(2).to_broadcast([128, 8, 16, 16]), op=ALU.mult),
                         r=[W1, idxf], w=[W1])
                    yield
                    k.op("dve", lambda e: e.tensor_reduce(out=iij[:, w_, :].rearrange("p (h q) -> p h q", q=16), in_=oh_v, axis=AX.X, op=ALU.add), r=[W1], w=[iij])
                    yield
                tr_ = trT2[par][a]
                k.op("pe", lambda e: e.transpose(out=pb[7][:, 0:128], in_=iij[:, 0, :], identity=ident_f[:]), r=[iij, ident_f], w=[pb[7]])
                k.op("pe", lambda e: e.transpose(out=pb[7][:, 128:256], in_=iij[:, 1, :], identity=ident_f[:]), r=[iij, ident_f], w=[pb[7]])
                k.op("pe", lambda e: e.transpose(out=pb[7][:, 256:384], in_=gate[:].rearrange("p h q -> p (h q)"), identity=ident_f[:]), r=[gate, ident_f], w=[pb[7]])
                k.op("act", lambda e: e.copy(out=tr_[:].rearrange("p a t -> p (a t)"), in_=pb[7][:, 0:384]), r=[pb[7]], w=[tr_])
                if "PKs" in dbg and b_ == 0 and a == 0:
                    k.dma("sp", d_PK[:, :, :], tr_[:], r=[tr_], w=[d_PK])
                yield

        def drain(gen):
            if gen is not None:
                for _ in gen:
                    pass

        drain(gen_5b1(blist[0], 0))
        for bidx, b_ in enumerate(blist):
            par = bidx % 2
            t0 = b_ * TB5
            k.dma("sp", u2b_[:], d_U2T[:, t0:t0 + TB5].rearrange("(kk p) t -> p kk t", p=128), r=[d_U2T], w=[u2b_])
            for a in range(TB5 // 128):
                trT = trT2[par][a]
                k.op("act", lambda e: e.copy(out=trTb[:], in_=trT[:]), r=[trT], w=[trTb])
                for hq in range(4):
                    hs_ = slice(hq * 32, (hq + 1) * 32)
                    Bq = Boh2[hq % 2]
                    Aq = Aoh2[hq % 2]
                    k.op("dve", lambda e: e.tensor_tensor(out=Bq[:], in0=io128b[:].unsqueeze(1).to_broadcast([128, 32, 128]),
                                                          in1=trTb[:, 1, hs_].unsqueeze(2).to_broadcast([128, 32, 128]), op=ALU.is_equal), r=[io128b, trTb], w=[Bq])
                    k.op("dve", lambda e: e.tensor_tensor(out=Aq[:], in0=io128b[:].unsqueeze(1).to_broadcast([128, 32, 128]),
                                                          in1=trTb[:, 0, hs_].unsqueeze(2).to_broadcast([128, 32, 128]), op=ALU.is_equal), r=[io128b, trTb], w=[Aq])
                    k.op("dve", lambda e: e.tensor_tensor(out=Aq[:], in0=Aq[:], in1=trTb[:, 2, hs_].unsqueeze(2).to_broadcast([128, 32, 128]), op=ALU.mult),
                         r=[Aq, trTb], w=[Aq])
                    for tq in range(8):
                        wp = pb[6 + tq % 2]
                        for t4 in range(4):
                            tl = tq * 4 + t4
                            k.op("pe", lambda e, t4=t4, tl=tl: e.matmul(wp[:, t4 * 128:(t4 + 1) * 128], lhsT=Bq[:, tl, :], rhs=Aq[:, tl, :], start=True, stop=True),
                                 r=[Bq, Aq], w=[wp])
                        tk0 = a * 128 + hq * 32 + tq * 4
                        ov = Wt[:, :, tk0:tk0 + 4].rearrange("j i t -> j t i")
                        iv_ = wp[:].rearrange("j (t i) -> j t i", i=128)
                        k.op("act", lambda e: e.copy(out=ov, in_=iv_), r=[wp], w=[Wt])
            gen = gen_5b1(blist[bidx + 1], 1 - par) if bidx + 1 < len(blist) else None
            pend = []
            for ig in range(64):
                j = ig % 3
                k.dma("sp", ut[j][:], d_UT_v[:, ig * 2:(ig + 1) * 2, :], r=[d_UT], w=[ut[j]])
                k.dma("pool", vb[j][:], d_VB_v[:, ig * 2:(ig + 1) * 2, :], r=[d_VB], w=[vb[j]])
                for q in range(2):
                    i = ig * 2 + q
                    aps = pb[4 + i % 3]
                    for kk in range(8):
                        k.op("pe", lambda e, kk=kk: e.matmul(aps[:, 0:TB5], lhsT=ut[j][:, q, kk * 128:(kk + 1) * 128], rhs=u2b_[:, kk, :], start=(kk == 0), stop=(kk == 7)),
                             r=[ut[j], u2b_], w=[aps])
                    k.op("act", lambda e: e.activation(out=Gf[i % 4][:], in_=aps[:, 0:TB5], func=AF.Gelu), r=[aps], w=[Gf[i % 4]])
                    k.op("dve", lambda e: e.tensor_tensor(out=Gw[i % 4][:], in0=Gf[i % 4][:], in1=Wt[:, i, :], op=ALU.mult), r=[Gf[i % 4], Wt], w=[Gw[i % 4]])
                    pend.append((i, j, q))
                    while len(pend) > (0 if i == 127 else 2):
                        i2, j2, q2 = pend.pop(0)
                        for a in range(2):
                            for hf in range(2):
                                k.op("pe", lambda e, a=a, hf=hf: e.matmul(pb[a * 2 + hf][:], lhsT=Gw[i2 % 4][:, a * 128:(a + 1) * 128], rhs=vb[j2][:, q2, hf * 512:(hf + 1) * 512],
                                                                          start=(i2 == 0), stop=(i2 == 127)), r=[Gw[i2 % 4], vb[j2]], w=[pb[a * 2 + hf]])
                if gen is not None:
                    for _ in range(3):
                        try:
                            next(gen)
                        except StopIteration:
                            gen = None
                            break
            drain(gen)
            for a in range(2):
                r0 = t0 + a * 128
                k.dma("sp", x1l[:], d_X1[r0:r0 + 128, :], r=[d_X1], w=[x1l])
                if "FFNs" in dbg and b_ in (0, nblk5 - 1) and a == (0 if b_ == 0 else 1):
                    for hf in range(2):
                        k.op("dve", lambda e, hf=hf: e.tensor_copy(out=ffs[:, hf * 512:(hf + 1) * 512], in_=pb[a * 2 + hf][:]), r=[pb[a * 2 + hf]], w=[ffs])
                    o_ = 0 if b_ == 0 else 128
                    k.dma("sp", d_FFNs[o_:o_ + 128, :], ffs[:], r=[ffs], w=[d_FFNs])
                for hf in range(2):
                    k.op("dve", lambda e, hf=hf: e.tensor_tensor(out=t5[:, hf * 512:(hf + 1) * 512], in0=pb[a * 2 + hf][:], in1=g2b[:, hf * 512:(hf + 1) * 512], op=ALU.mult),
                         r=[pb[a * 2 + hf], g2b], w=[t5])
                k.op("dve", lambda e: e.scalar_tensor_tensor(out=xr5[:], in0=x1l[:], scalar=DN_ALPHA, in1=t5[:], op0=ALU.mult, op1=ALU.add), r=[x1l, t5], w=[xr5])
                ln_sb(xr5, t5[:], t5, st6_, mv_, rstd_, nmr_)
                k.op("pool", lambda e: e.tensor_tensor(out=t5[:], in0=t5[:], in1=l2g[:], op=ALU.mult), r=[t5, l2g], w=[t5])
                k.op("pool", lambda e: e.tensor_tensor(out=xr5[:], in0=t5[:], in1=l2b[:], op=ALU.add), r=[t5, l2b], w=[xr5])
                k.dma("sp", out[r0:r0 + 128, :], xr5[:], r=[xr5], w=[out_buf], is_out=True)
            k.barrier()
        k.pop()
    if "Q2s" in dbg:
        dd = scratch("Q2s", [2048, 256], BF16)
        k.dma("sp", dd[:, 0:128], d_Q2[:, 0:128], r=[d_Q2], w=[dd])
        k.dma("sp", dd[:, 128:256], d_Q2[:, T - 128:T], r=[d_Q2], w=[dd])
    if "UTs" in dbg:
        dd = scratch("UTs", [2, 128, 1024], BF16)
        k.dma("sp", dd[0], d_UT[0], r=[d_UT], w=[dd])
        k.dma("sp", dd[1], d_UT[127], r=[d_UT], w=[dd])
    for nm_, src_ in (("OFs", d_OF), ("OBs", d_OB)):
        if nm_ in dbg:
            dd = scratch(nm_, [1024, 256])
            k.dma("sp", dd[:, 0:128], src_[:, 0:128], r=[src_], w=[dd])
            k.dma("sp", dd[:, 128:256], src_[:, T - 128:T], r=[src_], w=[dd])
    k.finish()
    es.close()
    return nc


def make_in_maps(inputs):
    cons = make_consts()
    maps = []
    for b in range(8):
        cv = np.stack([feat(inputs["c"][b]), feat(inputs["c_ctx"])], axis=-1)
        m = {
            "x": np.ascontiguousarray(inputs["x"][b]),
            "ctx": np.ascontiguousarray(inputs["ctx"][b]),
            "cvec": np.ascontiguousarray(cv),
            "w_ada": np.ascontiguousarray(inputs["w_ada"][0]),
            "b_ada": feat(inputs["b_ada"][0]),
            "w_in": np.ascontiguousarray(inputs["w_in"][0]),
            "ident": cons["ident"],
            "hg_lb": np.ascontiguousarray(inputs["hg_lb_logits"][:, :, :]),
            "hg_nw": np.ascontiguousarray(inputs["hg_norm_w"][0].reshape(128, 1)),
            "conv_w": np.ascontiguousarray(inputs["ma_conv_w"][0].reshape(32, 128, 5).transpose(1, 0, 2)),
            "conv_b": feat(inputs["ma_conv_b"][0]),
            "ma_nw": np.ascontiguousarray(inputs["ma_norm_w"][0]),
            "w_ba": np.ascontiguousarray(inputs["w_branch_a"][0]), "w_bb": np.ascontiguousarray(inputs["w_branch_b"][0]),
            "w_o": np.ascontiguousarray(inputs["w_out"][0]),
            "ln1_g": np.ascontiguousarray(inputs["ln1_g"][0]), "ln1_b": np.ascontiguousarray(inputs["ln1_b"][0]),
            "peer_wq": np.ascontiguousarray(inputs["peer_wq"][0]),
            "peer_sub": np.ascontiguousarray(inputs["peer_subkeys"][0].reshape(16, 128, 128)),
            "peer_u": np.ascontiguousarray(inputs["peer_u"][0]), "peer_v": np.ascontiguousarray(inputs["peer_v"][0]),
            "ln2_g": np.ascontiguousarray(inputs["ln2_g"][0]), "ln2_b": np.ascontiguousarray(inputs["ln2_b"][0]),
            "iota_c": np.arange(128, dtype=np.float32),
            "ssd_vec": np.ascontiguousarray(np.stack([inputs["ma_dt_bias"][0].reshape(64), inputs["ma_a_log"][0].reshape(64),
                                                      np.concatenate([inputs["ma_d"][0], np.zeros(32, np.float32)])]).astype(np.float32)),
            "incl_f": cons["incl_f"], "incl_b": cons["incl_b"], "strict_f": cons["strict_f"], "strict_b": cons["strict_b"],
            "incl_f128": cons["incl_f128"], "incl_b128": cons["incl_b128"], "strict_f128": cons["strict_f128"], "strict_b128": cons["strict_b128"],
        }
        maps.append(m)
    return maps


def kernel(**inputs):
    inputs = {k_: np.asarray(v) for k_, v in inputs.items()}
    nc = build()
    maps = make_in_maps(inputs)
    res = run_bass_kernel_spmd(nc, maps, core_ids=list(range(8)))
    return np.stack([r["out"] for r in res.results], axis=0)
```

```python
import numpy as np
from contextlib import ExitStack
import concourse.bass as bass
import concourse.mybir as mybir
from concourse.bass_utils import run_bass_kernel_spmd

F32 = mybir.dt.float32
BF16 = mybir.dt.bfloat16
I32 = mybir.dt.int32
U32 = mybir.dt.uint32
AF = mybir.ActivationFunctionType
ALU = mybir.AluOpType
AX = mybir.AxisListType

D = 1024
T = 8192
CTX = 256
TT = T + CTX
GW = 64
ROWS = T // GW
CH = 64
NCH = T // CH
STATE_DIM = 6208
IN_DIM = 13376
EPS = 1e-6
DN_ALPHA = 2.0 ** 0.25

C_FF, C_FB, C_IV, C_XB, C_DTF, C_DTB = 0, 1024, 2048, 3072, 6144, 6176
C_Q, C_OG, C_CM, C_Z, C_GA, C_GB = 6208, 7232, 8256, 9280, 11328, 12352


class Buf:
    __slots__ = ("t", "lw", "rd", "name")

    def __init__(self, t, name):
        self.t = t
        self.name = name
        self.lw = None
        self.rd = {}

    def __getitem__(self, idx):
        return self.t[idx]


class K:
    def __init__(self, nc, es, n_dma_sems=40):
        self.nc = nc
        self.es = es
        self.eng = {"pe": nc.tensor, "act": nc.scalar, "dve": nc.vector, "pool": nc.gpsimd, "sp": nc.sync}
        self.epoch = 0
        self.sem = {e: es.enter_context(nc.semaphore("prog0_" + e)) for e in self.eng}
        self.cnt = {e: 0 for e in self.eng}
        self.waited = {e: {} for e in self.eng}
        self.dsem = [es.enter_context(nc.semaphore("dma%d" % i)) for i in range(n_dma_sems)]
        self.dval = [0] * n_dma_sems
        self.dnext = 0
        self.out_dmas = []
        self.scopes = [es]

    def sb(self, name, shape, dt):
        t = self.scopes[-1].enter_context(self.nc.sbuf_tensor(name, list(shape), dt))
        return Buf(t, name)

    def ps(self, name, shape, dt=F32):
        t = self.scopes[-1].enter_context(self.nc.psum_tensor(name, list(shape), dt))
        return Buf(t, name)

    def push(self):
        self.scopes.append(ExitStack())

    def pop(self):
        self.barrier()
        self.scopes.pop().close()

    def barrier(self):
        for e in self.eng:
            for i, v in enumerate(self.dval):
                self._wait(e, ("d", i), v)
            for f in self.eng:
                if f != e:
                    self._wait(e, ("e", f, self.epoch), self.cnt[f])
        if max(self.cnt.values()) < 25000:
            return
        self.epoch += 1
        self.sem = {e: self.es.enter_context(self.nc.semaphore("prog%d_%s" % (self.epoch, e))) for e in self.eng}
        self.cnt = {e: 0 for e in self.eng}
        for e in self.eng:
            self.waited[e] = {kk: vv for kk, vv in self.waited[e].items() if kk[0] == "d"}

    def dram(self, name, shape, dt, kind="Internal"):
        t = self.nc.dram_tensor(name, list(shape), dt, kind=kind)
        return Buf(t.ap(), name)

    def _wait(self, e, key, val):
        if val <= 0:
            return
        w = self.waited[e]
        if w.get(key, 0) >= val:
            return
        w[key] = val
        if key[0] == "d":
            self.eng[e].wait_ge(self.dsem[key[1]], val)
        else:
            assert key[2] == self.epoch
            self.eng[e].wait_ge(self.sem[key[1]], val)

    def _deps(self, e, r, w):
        deps = {}
        for b in r:
            if b.lw is not None:
                k, v = b.lw
                deps[k] = max(deps.get(k, 0), v)
        for b in w:
            if b.lw is not None:
                k, v = b.lw
                deps[k] = max(deps.get(k, 0), v)
            for k, v in b.rd.items():
                deps[k] = max(deps.get(k, 0), v)
        for k, v in deps.items():
            if k[0] == "e":
                if k[2] != self.epoch:
                    continue
                if k[1] == "pe" and e == "pe":
                    continue
            self._wait(e, k, v)

    def _mark(self, key, val, r, w):
        for b in r:
            b.rd[key] = max(b.rd.get(key, 0), val)
        for b in w:
            b.lw = (key, val)
            b.rd = {}

    def op(self, e, fn, r=(), w=()):
        self._deps(e, r, w)
        inst = fn(self.eng[e])
        self.cnt[e] += 1
        inst.then_inc(self.sem[e], 1)
        assert self.cnt[e] < 60000, "semaphore epoch overflow: add a barrier"
        self._mark(("e", e, self.epoch), self.cnt[e], r, w)
        return inst

    def dma(self, q, out, in_, r=(), w=(), is_out=False, **kw):
        self._deps(q, r, w)
        i = self.dnext
        self.dnext = (self.dnext + 1) % len(self.dsem)
        self._wait(q, ("d", i), self.dval[i])
        inst = self.eng[q].dma_start(out=out, in_=in_, **kw)
        self.dval[i] += 16
        inst.then_inc(self.dsem[i], 16)
        self._mark(("d", i), self.dval[i], r, w)
        if is_out:
            self.out_dmas.append((i, self.dval[i]))

    def finish(self):
        for i, v in enumerate(self.dval):
            self._wait("sp", ("d", i), v)
        for e in self.eng:
            if e != "sp":
                self._wait("sp", ("e", e, self.epoch), self.cnt[e])


def feat(v):
    v = np.asarray(v, np.float32).reshape(-1, 128)
    return np.ascontiguousarray(v.T)


def make_consts():
    c = {}
    c["ident"] = np.eye(128, dtype=np.float32)
    s = np.arange(64)
    c["incl_f"] = (s[:, None] <= s[None, :]).astype(np.float32)
    c["incl_b"] = (s[:, None] >= s[None, :]).astype(np.float32)
    c["strict_f"] = (s[:, None] > s[None, :]).astype(np.float32)
    c["strict_b"] = (s[:, None] < s[None, :]).astype(np.float32)
    c["ones64"] = np.ones((64, 128), np.float32)
    s2 = np.arange(128)
    c["incl_f128"] = (s2[:, None] <= s2[None, :]).astype(np.float32)
    c["incl_b128"] = (s2[:, None] >= s2[None, :]).astype(np.float32)
    c["strict_f128"] = (s2[:, None] > s2[None, :]).astype(np.float32)
    c["strict_b128"] = (s2[:, None] < s2[None, :]).astype(np.float32)
    return c


def build(debug=(), stages=99):
    nc = bass.Bass("TRN2", target_bir_lowering=False)
    es = ExitStack()
    k = K(nc, es)
    dbg = set(debug)

    def din(name, shape, dt=F32):
        return nc.dram_tensor(name, list(shape), dt, kind="ExternalInput").ap()

    def scratch(name, shape, dt=F32):
        kind = "ExternalOutput" if name in dbg else "Internal"
        return k.dram("d_" + name, shape, dt, kind=kind)

    x = din("x", [T, D])
    ctx = din("ctx", [CTX, D])
    cvec = din("cvec", [128, 8, 2])
    w_ada = din("w_ada", [D, 6 * D])
    b_ada = din("b_ada", [128, 48])
    w_in = din("w_in", [D, IN_DIM])
    ident_d = din("ident", [128, 128])
    hg_lb = din("hg_lb", [2, 2, 1024])
    hg_nw = din("hg_nw", [128, 1])
    conv_w = din("conv_w", [128, 32, 5])
    conv_b = din("conv_b", [128, 32])
    ssd_vec = din("ssd_vec", [3, 64])
    ma_nw = din("ma_nw", [2048])
    w_ba = din("w_ba", [1024, 1024]); w_bb = din("w_bb", [2048, 1024]); w_o = din("w_o", [1024, 1024])
    ln1_g = din("ln1_g", [1024]); ln1_b = din("ln1_b", [1024])
    peer_wq = din("peer_wq", [1024, 2048]); peer_sub = din("peer_sub", [16, 128, 128])
    peer_u = din("peer_u", [16384, 1024]); peer_v = din("peer_v", [16384, 1024])
    ln2_g = din("ln2_g", [1024]); ln2_b = din("ln2_b", [1024])
    iota_c = din("iota_c", [128])
    cmask = {nm: din(nm, [64, 64]) for nm in ("incl_f", "incl_b", "strict_f", "strict_b")}
    cmask128 = {nm: din(nm + "128", [128, 128]) for nm in ("incl_f", "incl_b", "strict_f", "strict_b")}

    out = nc.dram_tensor("out", [T, D], F32, kind="ExternalOutput").ap()
    out_buf = Buf(out, "out")

    ident_f = k.sb("ident_f", [128, 128], F32)
    ident_b = k.sb("ident_b", [128, 128], BF16)
    k.dma("sp", ident_f[:], ident_d[:, :], w=[ident_f])
    k.op("dve", lambda e: e.tensor_copy(out=ident_b[:], in_=ident_f[:]), r=[ident_f], w=[ident_b])

    modT = k.sb("modT", [128, 48, 2], F32)
    ops1 = k.sb("ops1", [128, 8, 2], F32)
    ops2 = k.sb("ops2", [128, 8, 2], F32)
    eps_t = k.sb("eps_t", [128, 1], F32)
    k.op("dve", lambda e: e.memset(eps_t[:], EPS), w=[eps_t])
    k.push()
    cv = k.sb("cv", [128, 8, 2], F32)
    scv = k.sb("scv", [128, 8, 2], F32)
    bada = k.sb("bada", [128, 48], F32)
    k.dma("sp", cv[:], cvec[:, :, :], w=[cv])
    k.dma("sp", bada[:], b_ada[:, :], w=[bada])
    k.op("act", lambda e: e.activation(out=scv[:], in_=cv[:], func=AF.Silu), r=[cv], w=[scv])
    wada_v = w_ada.rearrange("(kk p) e -> p kk e", p=128)
    NP0 = 8
    PW = 6 * D // NP0
    wst = [k.sb("wada_st%d" % i, [128, 8, PW], F32) for i in range(2)]
    mod_ps = k.ps("mod_ps", [128, 48, 2], F32)
    for pc in range(NP0):
        st = wst[pc % 2]
        k.dma("sp", st[:], wada_v[:, :, pc * PW:(pc + 1) * PW], w=[st])
        for bb in range(PW // 128):
            blk = pc * (PW // 128) + bb
            for kk in range(8):
                k.op("pe", lambda e, st=st, bb=bb, kk=kk, blk=blk: e.matmul(
                    mod_ps[:, blk, :], lhsT=st[:, kk, bb * 128:(bb + 1) * 128], rhs=scv[:, kk, :],
                    start=(kk == 0), stop=(kk == 7)), r=[st, scv], w=[mod_ps])
    k.op("dve", lambda e: e.tensor_tensor(out=modT[:], in0=mod_ps[:], in1=bada[:].unsqueeze(2).to_broadcast([128, 48, 2]),
                                          op=ALU.add), r=[mod_ps, bada], w=[modT])
    k.op("dve", lambda e: e.tensor_scalar(out=ops1[:], in0=modT[:, 8:16, :], scalar1=1.0, scalar2=None, op0=ALU.add),
         r=[modT], w=[ops1])
    k.op("dve", lambda e: e.tensor_scalar(out=ops2[:], in0=modT[:, 32:40, :], scalar1=1.0, scalar2=None, op0=ALU.add),
         r=[modT], w=[ops2])

    if "modT" in dbg:
        modT_d = scratch("modT", [128, 96])
        k.dma("sp", modT_d[:, :], modT[:].rearrange("p a b -> p (a b)"), r=[modT], w=[modT_d], is_out=True)
    k.pop()

    k.push()
    uT = k.sb("uT", [128, 8, TT], BF16)
    k.push()
    xt = [k.sb("xt%d" % i, [128, D], F32) for i in range(2)]
    xn = [k.sb("xn%d" % i, [128, D], BF16) for i in range(2)]
    st6 = [k.sb("st6_%d" % i, [128, 2, 6], F32) for i in range(2)]
    mv = [k.sb("mv%d" % i, [128, 2], F32) for i in range(2)]
    rstd = [k.sb("rstd%d" % i, [128, 1], F32) for i in range(2)]
    nmr = [k.sb("nmr%d" % i, [128, 1], F32) for i in range(2)]
    tp_ps = [k.ps("tp_ps%d" % i, [128, 8, 128], BF16) for i in range(2)]

    def ln_tile(src_ap, i, xt_, xn_, st6_, mv_, rstd_, nmr_):
        k.dma("sp", xt_[:], src_ap, w=[xt_])
        for h in range(2):
            k.op("dve", lambda e, h=h: e.bn_stats(out=st6_[:, h, :], in_=xt_[:, h * 512:(h + 1) * 512]), r=[xt_], w=[st6_])
        k.op("dve", lambda e: e.bn_aggr(out=mv_[:], in_=st6_[:].rearrange("p a b -> p (a b)")), r=[st6_], w=[mv_])
        k.op("act", lambda e: e.activation(out=rstd_[:], in_=mv_[:, 1:2], func=AF.Sqrt, bias=eps_t[:], scale=1.0), r=[mv_, eps_t], w=[rstd_])
        k.op("dve", lambda e: e.reciprocal(out=rstd_[:], in_=rstd_[:]), r=[rstd_], w=[rstd_])
        k.op("dve", lambda e: e.scalar_tensor_tensor(out=nmr_[:], in0=mv_[:, 0:1], scalar=-1.0, in1=rstd_[:], op0=ALU.mult, op1=ALU.mult),
             r=[mv_, rstd_], w=[nmr_])
        k.op("act", lambda e: e.activation(out=xn_[:], in_=xt_[:], func=AF.Identity, bias=nmr_[:], scale=rstd_[:]),
             r=[xt_, nmr_, rstd_], w=[xn_])

    NT = T // 128
    x_cm = x.rearrange("(r c) d -> c r d", c=GW)

    def make_uT(order):
      for i in range(NT + CTX // 128):
          j = i % 2
          if i < NT:
              src = x[i * 128:(i + 1) * 128, :] if order == "row" else x_cm[i]
              col = 0
              t0 = i * 128
          else:
              src = ctx[(i - NT) * 128:(i - NT + 1) * 128, :]
              col = 1
              t0 = T + (i - NT) * 128
          ln_tile(src, i, xt[j], xn[j], st6[j], mv[j], rstd[j], nmr[j])
          for kk in range(8):
              k.op("pe", lambda e, kk=kk, j=j: e.transpose(out=tp_ps[j][:, kk, :], in_=xn[j][:, kk * 128:(kk + 1) * 128], identity=ident_b[:]),
                   r=[xn[j], ident_b], w=[tp_ps[j]])
          for kk in range(8):
              eng = "act" if kk % 2 == 0 else "dve"
              if eng == "act":
                  k.op("act", lambda e, kk=kk, j=j, col=col, t0=t0: e.activation(
                      out=uT[:, kk, t0:t0 + 128], in_=tp_ps[j][:, kk, :], func=AF.Identity,
                      bias=modT[:, kk, col:col + 1], scale=ops1[:, kk, col:col + 1]), r=[tp_ps[j], modT, ops1], w=[uT])
              else:
                  k.op("dve", lambda e, kk=kk, j=j, col=col, t0=t0: e.tensor_scalar(
                      out=uT[:, kk, t0:t0 + 128], in0=tp_ps[j][:, kk, :], scalar1=ops1[:, kk, col:col + 1],
                      scalar2=modT[:, kk, col:col + 1], op0=ALU.mult, op1=ALU.add), r=[tp_ps[j], modT, ops1], w=[uT])


    make_uT("row")
    k.pop()
    if "uT" in dbg:
        uT_d = scratch("uT", [128, 8 * TT], BF16)
        k.dma("sp", uT_d[:, :], uT[:].rearrange("p a b -> p (a b)"), r=[uT], w=[uT_d], is_out=True)


    d_FF = scratch("FF", [TT, 1024]); d_FB = scratch("FB", [TT, 1024]); d_IV = scratch("IV", [TT, 1024], BF16)
    d_QT = scratch("QT", [1024, T]); d_OGT = scratch("OGT", [1024, T])
    d_GAT = scratch("GAT", [1024, T]); d_GBT = scratch("GBT", [1024, T])
    d_XBT = scratch("XBT", [3072, TT]); d_DT = scratch("DT", [TT, 64]); d_CT = scratch("CT", [1024, T])
    d_Z = scratch("Z", [T, 2048])
    k.push()
    w_in_v = w_in.rearrange("(kk p) e -> p kk e", p=128)
    wst = [k.sb("win_st%d" % i, [128, 8, 512], F32) for i in range(1)]
    wbf = [k.sb("win_bf%d" % i, [128, 8, 512], BF16) for i in range(2)]
    stg = [k.sb("stg%d" % i, [128, 2048], F32) for i in range(2)]
    stgb = [k.sb("stgb%d" % i, [128, 512], BF16) for i in range(2)]
    pp = [k.ps("pp%d" % i, [128, 512], F32) for i in range(3)]
    ptmp = k.sb("ptmp", [128, T], BF16)
    uT_cm = uT[:, :, 0:T].rearrange("p k (r c) -> p k c r", c=GW)
    cnt = {"pc": 0, "pp": 0, "stg": 0, "stgb": 0}

    def load_piece(c0, ncols):
        i = cnt["pc"] % 2
        cnt["pc"] += 1
        k.dma("sp", wst[0][:, :, 0:ncols], w_in_v[:, :, c0:c0 + ncols], w=[wst[0]])
        for kk in range(8):
            eng = "pool" if kk % 2 == 0 else "dve"
            k.op(eng, lambda e, kk=kk, i=i: e.tensor_copy(out=wbf[i][:, kk, 0:ncols], in_=wst[0][:, kk, 0:ncols]),
                 r=[wst[0]], w=[wbf[i]])
        return wbf[i]

    def evac(dst_ap, src_ap, func, r, w):
        if func is None:
            k.op("dve", lambda e: e.tensor_copy(out=dst_ap, in_=src_ap), r=r, w=w)
        else:
            k.op("act", lambda e: e.activation(out=dst_ap, in_=src_ap, func=func), r=r, w=w)

    def proj_fm(c0, ncols, dst, drow0, order, func, with_ctx):
        for p0 in range(0, ncols, 512):
            wb = load_piece(c0 + p0, 512)
            for eb in range(4):
                nblk = 16 + (1 if with_ctx else 0)
                for tb in range(nblk):
                    q = tb % 4
                    if q == 0:
                        sg = stg[cnt["stg"] % 2]
                        cnt["stg"] += 1
                    ps_ = pp[cnt["pp"] % 3]
                    cnt["pp"] += 1
                    n = 512 if tb < 16 else CTX
                    for kk in range(8):
                        if tb == 16:
                            rhs = uT[:, kk, T:TT]
                        elif order == "row":
                            rhs = uT[:, kk, tb * 512:(tb + 1) * 512]
                        else:
                            rhs = uT[:, kk, tb * 512:(tb + 1) * 512]
                        k.op("pe", lambda e, kk=kk, rhs=rhs, ps_=ps_, n=n, wb=wb, eb=eb: e.matmul(
                            ps_[:, 0:n], lhsT=wb[:, kk, eb * 128:(eb + 1) * 128], rhs=rhs, start=(kk == 0), stop=(kk == 7)),
                            r=[wb, uT], w=[ps_])
                    evac(sg[:, q * 512:q * 512 + n], ps_[:, 0:n], func, [ps_], [sg])
                    r0 = drow0 + p0 + eb * 128
                    if tb == 16:
                        k.dma("sp", dst[r0:r0 + 128, T:TT], sg[:, 0:CTX], r=[sg], w=[dst])
                    elif q == 3:
                        k.dma("sp", dst[r0:r0 + 128, (tb - 3) * 512:(tb + 1) * 512], sg[:, :], r=[sg], w=[dst])

    def proj_tm(c0, ncols, dsts, order, func, with_ctx, bf=False):
        for p0 in range(0, ncols, 512):
            pw = min(512, ncols - p0)
            wb = load_piece(c0 + p0, pw)
            ntile = NT + (CTX // 128 if with_ctx else 0)
            for tt in range(ntile):
                ps_ = pp[cnt["pp"] % 3]
                cnt["pp"] += 1
                for kk in range(8):
                    if tt >= NT:
                        lhsT = uT[:, kk, T + (tt - NT) * 128:T + (tt - NT + 1) * 128]
                    elif order == "row":
                        lhsT = uT[:, kk, tt * 128:(tt + 1) * 128]
                    else:
                        lhsT = uT[:, kk, tt * 128:(tt + 1) * 128]
                    k.op("pe", lambda e, kk=kk, lhsT=lhsT, ps_=ps_, wb=wb, pw=pw: e.matmul(
                        ps_[:, 0:pw], lhsT=lhsT, rhs=wb[:, kk, 0:pw], start=(kk == 0), stop=(kk == 7)),
                        r=[wb, uT], w=[ps_])
                if bf:
                    sg = stgb[cnt["stgb"] % 2]
                    cnt["stgb"] += 1
                else:
                    sg = stg[cnt["stg"] % 2]
                    cnt["stg"] += 1
                evac(sg[:, 0:pw], ps_[:, 0:pw], func, [ps_], [sg])
                pos0 = tt * 128
                for (dst, lo, hi, dcol0) in dsts:
                    a = max(lo, p0)
                    b = min(hi, p0 + pw)
                    if a < b:
                        k.dma("sp", dst[pos0:pos0 + 128, dcol0 + a - lo:dcol0 + b - lo], sg[:, a - p0:b - p0], r=[sg], w=[dst])

    sel = stages if isinstance(stages, (set, list, tuple)) else None
    def on(name):
        return sel is None and stages >= 1 or (sel is not None and name in sel)
    if on("a"):
        proj_tm(C_FF, 2048, [(d_FF, 0, 1024, 0), (d_FB, 1024, 2048, 0)], "row", None, True)
    if on("b"):
        proj_tm(C_IV, 1024, [(d_IV, 0, 1024, 0)], "row", None, True, bf=True)
    if on("c"):
        proj_fm(C_Q, 1024, d_QT, 0, "row", AF.Silu, False)
        proj_fm(C_OG, 1024, d_OGT, 0, "row", AF.Silu, False)
        proj_fm(C_GA, 1024, d_GAT, 0, "row", AF.Sigmoid, False)
        proj_fm(C_GB, 1024, d_GBT, 0, "row", AF.Sigmoid, False)
    for kk in range(8):
        k.op("dve", lambda e, kk=kk: e.tensor_copy(out=ptmp[:], in_=uT[:, kk, 0:T]), r=[uT], w=[ptmp])
        k.op("pool", lambda e, kk=kk: e.tensor_copy(out=uT[:, kk, 0:T].rearrange("p (c r) -> p c r", r=ROWS),
                                                   in_=ptmp[:].rearrange("p (r c) -> p c r", c=GW)), r=[ptmp], w=[uT])
    if on("d"):
        proj_fm(C_XB, 3072, d_XBT, 0, "col", None, True)
        proj_fm(C_CM, 1024, d_CT, 0, "col", None, False)
    if on("e"):
        proj_tm(C_DTF, 64, [(d_DT, 0, 64, 0)], "col", None, True)
        proj_tm(C_Z, 2048, [(d_Z, 0, 2048, 0)], "col", AF.Silu, False)
    k.pop()
    k.pop()

    d_OF = scratch("OF", [1024, T]); d_OB = scratch("OB", [1024, T])
    d_ONT = scratch("ONT", [1024, T], BF16)
    if sel is None and stages >= 2 or (sel is not None and "h" in sel):
        k.push()
        lbl = [k.sb("lbl%d" % i, [64, 2, 1024], F32) for i in range(2)]
        lb = [k.sb("lb%d" % i, [64, 1024], F32) for i in range(2)]
        oml = [k.sb("oml%d" % i, [64, 1024], F32) for i in range(2)]
        msk = {}
        for nm in ("incl_f", "incl_b", "strict_f", "strict_b"):
            msk[nm] = k.sb("m_" + nm, [64, 64], F32)
            k.dma("sp", msk[nm][:], cmask[nm][:, :], w=[msk[nm]])
        for d_ in range(2):
            k.dma("sp", lbl[d_][:], hg_lb[d_].partition_broadcast(64), w=[lbl[d_]])
            k.op("dve", lambda e, d_=d_: e.tensor_tensor(out=lb[d_][:], in0=lbl[d_][:, 0, :], in1=lbl[d_][:, 1, :], op=ALU.subtract),
                 r=[lbl[d_]], w=[lb[d_]])
            k.op("act", lambda e, d_=d_: e.activation(out=lb[d_][:], in_=lb[d_][:], func=AF.Sigmoid), r=[lb[d_]], w=[lb[d_]])
            k.op("dve", lambda e, d_=d_: e.tensor_scalar(out=oml[d_][:], in0=lb[d_][:], scalar1=-1.0, scalar2=1.0, op0=ALU.mult, op1=ALU.add),
                 r=[lb[d_]], w=[oml[d_]])
        NB = 4
        fr = [k.sb("fr%d" % i, [64, NB, 1024], F32) for i in range(2)]
        vv = [k.sb("vv%d" % i, [64, NB, 1024], BF16) for i in range(2)]
        qs = [k.sb("qs%d" % i, [128, 8, NB * 64], F32) for i in range(2)]
        lf = k.sb("lf", [64, NB, 1024], F32)
        km = k.sb("km", [64, NB, 1024], F32)
        ost = k.sb("ost", [128, 8, NB * 64], F32)
        E1 = k.sb("E1", [128, 8, 64], F32)
        E2 = k.sb("E2", [64, 1024], F32)
        QtT = k.sb("QtT", [128, 8, 64], BF16)
        Kt = k.sb("Kt", [64, 1024], BF16)
        Kh = k.sb("Kh", [64, 1024], BF16)
        KtT = k.sb("KtT", [128, 8, 64], BF16)
        attm = k.sb("attm", [64, 8, 64], BF16)
        S = k.sb("S", [128, 8, 128], F32)
        Sbf = k.sb("Sbf", [128, 8, 128], BF16)
        gcT_ps = k.ps("gcT_ps", [128, 8, 64], F32)
        gs_ps = k.ps("gs_ps", [64, 1024], F32)
        ktT_ps = k.ps("ktT_ps", [128, 8, 128], BF16)
        att_ps = k.ps("att_ps", [64, 8, 64], F32)
        oT_ps = k.ps("oT_ps", [128, 8, 64], F32)
        dS_ps = k.ps("dS_ps", [128, 8, 128], F32)

        for d_ in range(2):
            incl = msk["incl_f" if d_ == 0 else "incl_b"]
            strict = msk["strict_f" if d_ == 0 else "strict_b"]
            d_F = d_FF if d_ == 0 else d_FB
            d_O = d_OF if d_ == 0 else d_OB
            glast_col = 63 if d_ == 0 else 0
            k.op("pool", lambda e: e.memset(S[:], 0.0), w=[S])
            k.op("pool", lambda e: e.memset(Sbf[:], 0.0), w=[Sbf])
            if d_ == 0:
                blocks = [(T, CTX // 64, True)] + [(b * NB * 64, NB, False) for b in range(NCH // NB)]
            else:
                blocks = [(T, CTX // 64, True)] + [(b * NB * 64, NB, False) for b in reversed(range(NCH // NB))]
            for bi, (t0, nc_, is_ctx) in enumerate(blocks):
                j = bi % 2
                nt = nc_ * 64
                k.dma("sp", fr[j][:, 0:nc_, :], d_F[t0:t0 + nt, :].rearrange("(c s) e -> s c e", s=64), r=[d_F], w=[fr[j]])
                k.dma("sp", vv[j][:, 0:nc_, :], d_IV[t0:t0 + nt, :].rearrange("(c s) e -> s c e", s=64), r=[d_IV], w=[vv[j]])
                if not is_ctx:
                    k.dma("sp", qs[j][:], d_QT[:, t0:t0 + nt].rearrange("(h p) t -> p h t", p=128), r=[d_QT], w=[qs[j]])
                k.op("act", lambda e: e.activation(out=fr[j][:, 0:nc_, :], in_=fr[j][:, 0:nc_, :], func=AF.Sigmoid), r=[fr[j]], w=[fr[j]])
                k.op("dve", lambda e: e.tensor_tensor(out=fr[j][:, 0:nc_, :], in0=fr[j][:, 0:nc_, :],
                                                      in1=oml[d_][:].unsqueeze(1).to_broadcast([64, nc_, 1024]), op=ALU.mult),
                     r=[fr[j], oml[d_]], w=[fr[j]])
                k.op("dve", lambda e: e.tensor_tensor(out=fr[j][:, 0:nc_, :], in0=fr[j][:, 0:nc_, :],
                                                      in1=lb[d_][:].unsqueeze(1).to_broadcast([64, nc_, 1024]), op=ALU.add),
                     r=[fr[j], lb[d_]], w=[fr[j]])
                k.op("act", lambda e: e.activation(out=lf[:, 0:nc_, :], in_=fr[j][:, 0:nc_, :], func=AF.Ln), r=[fr[j]], w=[lf])
                k.op("dve", lambda e: e.tensor_scalar(out=km[:, 0:nc_, :], in0=fr[j][:, 0:nc_, :], scalar1=-1.0, scalar2=1.0,
                                                      op0=ALU.mult, op1=ALU.add), r=[fr[j]], w=[km])
                corder = list(range(nc_)) if d_ == 0 else list(reversed(range(nc_)))
                for c in corder:
                    for h in range(8):
                        k.op("pe", lambda e, h=h, c=c: e.matmul(gcT_ps[:, h, :], lhsT=lf[:, c, h * 128:(h + 1) * 128], rhs=incl[:],
                                                                start=True, stop=True), r=[lf, incl], w=[gcT_ps])
                    k.op("act", lambda e: e.activation(out=E1[:], in_=gcT_ps[:], func=AF.Exp), r=[gcT_ps], w=[E1])
                    if not is_ctx:
                        for hf in range(2):
                            k.op("pe", lambda e, hf=hf, c=c: e.matmul(gs_ps[:, hf * 512:(hf + 1) * 512], lhsT=incl[:],
                                                                      rhs=lf[:, c, hf * 512:(hf + 1) * 512], start=True, stop=True),
                                 r=[lf, incl], w=[gs_ps])
                        k.op("act", lambda e: e.activation(out=E2[:], in_=gs_ps[:], func=AF.Exp, scale=-1.0), r=[gs_ps], w=[E2])
                        k.op("dve", lambda e, c=c: e.tensor_tensor(out=Kt[:], in0=km[:, c, :], in1=E2[:], op=ALU.mult), r=[km, E2], w=[Kt])
                        k.op("dve", lambda e, c=c: e.tensor_tensor(out=QtT[:], in0=qs[j][:, :, c * 64:(c + 1) * 64], in1=E1[:], op=ALU.mult),
                             r=[qs[j], E1], w=[QtT])
                        for h in range(8):
                            k.op("pe", lambda e, h=h: e.transpose(out=ktT_ps[:, h, 0:64], in_=Kt[:, h * 128:(h + 1) * 128], identity=ident_b[0:64, 0:64]),
                                 r=[Kt, ident_b], w=[ktT_ps])
                        k.op("act", lambda e: e.copy(out=KtT[:], in_=ktT_ps[:, :, 0:64]), r=[ktT_ps], w=[KtT])
                        for h in range(8):
                            k.op("pe", lambda e, h=h: e.matmul(att_ps[:, h, :], lhsT=KtT[:, h, :], rhs=QtT[:, h, :], start=True, stop=True),
                                 r=[KtT, QtT], w=[att_ps])
                        k.op("dve", lambda e: e.tensor_tensor(out=attm[:], in0=att_ps[:], in1=incl[:].unsqueeze(1).to_broadcast([64, 8, 64]),
                                                              op=ALU.mult), r=[att_ps, incl], w=[attm])
                        for h in range(8):
                            k.op("pe", lambda e, h=h, c=c: e.matmul(oT_ps[:, h, :], lhsT=vv[j][:, c, h * 128:(h + 1) * 128], rhs=attm[:, h, :],
                                                                    start=True, stop=False), r=[vv[j], attm], w=[oT_ps])
                            k.op("pe", lambda e, h=h: e.matmul(oT_ps[:, h, :], lhsT=Sbf[:, h, :], rhs=QtT[:, h, :], start=False, stop=True),
                                 r=[Sbf, QtT], w=[oT_ps])
                        k.op("act", lambda e, c=c: e.copy(out=ost[:, :, c * 64:(c + 1) * 64], in_=oT_ps[:]), r=[oT_ps], w=[ost])
                    for hf in range(2):
                        k.op("pe", lambda e, hf=hf, c=c: e.matmul(gs_ps[:, hf * 512:(hf + 1) * 512], lhsT=strict[:],
                                                                  rhs=lf[:, c, hf * 512:(hf + 1) * 512], start=True, stop=True),
                             r=[lf, strict], w=[gs_ps])
                    k.op("act", lambda e: e.activation(out=E2[:], in_=gs_ps[:], func=AF.Exp), r=[gs_ps], w=[E2])
                    k.op("dve", lambda e, c=c: e.tensor_tensor(out=Kh[:], in0=km[:, c, :], in1=E2[:], op=ALU.mult), r=[km, E2], w=[Kh])
                    for h in range(8):
                        k.op("pe", lambda e, h=h, c=c: e.matmul(dS_ps[:, h, :], lhsT=Kh[:, h * 128:(h + 1) * 128], rhs=vv[j][:, c, h * 128:(h + 1) * 128],
                                                                start=True, stop=True), r=[Kh, vv[j]], w=[dS_ps])
                    k.op("dve", lambda e: e.tensor_tensor(out=S[:], in0=S[:], in1=E1[:, :, glast_col:glast_col + 1].to_broadcast([128, 8, 128]),
                                                          op=ALU.mult), r=[S, E1], w=[S])
                    k.op("dve", lambda e: e.tensor_tensor(out=S[:], in0=S[:], in1=dS_ps[:], op=ALU.add), r=[S, dS_ps], w=[S])
                    k.op("act", lambda e: e.copy(out=Sbf[:], in_=S[:]), r=[S], w=[Sbf])
                if not is_ctx:
                    k.dma("sp", d_O[:, t0:t0 + nt].rearrange("(h p) t -> p h t", p=128), ost[:], r=[ost], w=[d_O])
            k.barrier()
            if "S%d" % d_ in dbg:
                dS_d = scratch("S%d" % d_, [128, 1024])
                k.dma("sp", dS_d[:, :], S[:].rearrange("p a b -> p (a b)"), r=[S], w=[dS_d])
        k.pop()

    if sel is None and stages >= 2 or (sel is not None and "n" in sel):
        k.push()
        ones_f = k.sb("ones_f", [128, 128], F32)
        k.op("pool", lambda e: e.memset(ones_f[:], 1.0), w=[ones_f])
        nw = k.sb("nw", [128, 1], F32)
        k.dma("sp", nw[:], hg_nw[:, :], w=[nw])
        ofb = [k.sb("ofb%d" % i, [128, 8, 512], F32) for i in range(2)]
        obb = [k.sb("obb%d" % i, [128, 8, 512], F32) for i in range(2)]
        ogb = [k.sb("ogb%d" % i, [128, 8, 512], F32) for i in range(2)]
        sq = k.sb("sq", [128, 8, 512], F32)
        rr = [k.sb("rr%d" % i, [128, 512], F32) for i in range(2)]
        onb = [k.sb("onb%d" % i, [128, 8, 512], BF16) for i in range(2)]
        ssq_ps = [k.ps("ssq_ps%d" % i, [128, 512], F32) for i in range(2)]
        for b_ in range(T // 512):
            j = b_ % 2
            t0 = b_ * 512
            k.dma("sp", ofb[j][:], d_OF[:, t0:t0 + 512].rearrange("(h p) t -> p h t", p=128), r=[d_OF], w=[ofb[j]])
            k.dma("sp", obb[j][:], d_OB[:, t0:t0 + 512].rearrange("(h p) t -> p h t", p=128), r=[d_OB], w=[obb[j]])
            k.dma("sp", ogb[j][:], d_OGT[:, t0:t0 + 512].rearrange("(h p) t -> p h t", p=128), r=[d_OGT], w=[ogb[j]])
            k.op("pool", lambda e: e.tensor_tensor(out=ofb[j][:], in0=ofb[j][:], in1=obb[j][:], op=ALU.add), r=[ofb[j], obb[j]], w=[ofb[j]])
            k.op("act", lambda e: e.activation(out=sq[:], in_=ofb[j][:], func=AF.Square), r=[ofb[j]], w=[sq])
            for h in range(8):
                jj = h % 2
                k.op("pe", lambda e: e.matmul(ssq_ps[jj][:], lhsT=ones_f[:], rhs=sq[:, h, :], start=True, stop=True), r=[ones_f, sq], w=[ssq_ps[jj]])
                k.op("act", lambda e: e.activation(out=rr[jj][:], in_=ssq_ps[jj][:], func=AF.Sqrt, bias=eps_t[:], scale=1.0 / 128.0),
                     r=[ssq_ps[jj], eps_t], w=[rr[jj]])
                k.op("dve", lambda e: e.reciprocal(out=rr[jj][:], in_=rr[jj][:]), r=[rr[jj]], w=[rr[jj]])
                k.op("dve", lambda e: e.tensor_tensor(out=rr[jj][:], in0=rr[jj][:], in1=ofb[j][:, h, :], op=ALU.mult), r=[rr[jj], ofb[j]], w=[rr[jj]])
                k.op("dve", lambda e: e.scalar_tensor_tensor(out=onb[j][:, h, :], in0=rr[jj][:], scalar=nw[:, 0:1], in1=ogb[j][:, h, :],
                                                             op0=ALU.mult, op1=ALU.mult), r=[rr[jj], nw, ogb[j]], w=[onb[j]])
            k.dma("sp", d_ONT[:, t0:t0 + 512].rearrange("(h p) t -> p h t", p=128), onb[j][:], r=[onb[j]], w=[d_ONT])
        k.pop()
    if "ONs" in dbg:
        dd = scratch("ONs", [1024, 256], BF16)
        k.dma("sp", dd[:, 0:128], d_ONT[:, 0:128], r=[d_ONT], w=[dd])
        k.dma("sp", dd[:, 128:256], d_ONT[:, T - 128:T], r=[d_ONT], w=[dd])

    d_XH = scratch("XH", [TT, 2048], BF16)
    d_BM = scratch("BM", [TT, 1024], BF16)
    d_BT = scratch("BT", [1024, TT], BF16)
    d_CT2 = scratch("CT2", [1024, T], BF16)
    if sel is None and stages >= 3 or (sel is not None and "v" in sel):
        k.push()
        cw = k.sb("cw", [128, 32, 5], F32)
        cb = k.sb("cb", [128, 32], F32)
        k.dma("sp", cw[:], conv_w[:, :, :], w=[cw])
        k.dma("sp", cb[:], conv_b[:, :], w=[cb])
        pad = [k.sb("pad%d" % i, [128, 4, 132], F32) for i in range(4)]
        padc = [k.sb("padc%d" % i, [128, 1, 260], F32) for i in range(4)]
        acc = [k.sb("acc%d" % i, [128, 512], F32) for i in range(4)]
        res = [k.sb("res%d" % i, [128, 512], BF16) for i in range(4)]
        tok = [k.sb("tok%d" % i, [128, 4, 3072], BF16) for i in range(2)]
        tps = [k.ps("tps%d" % i, [128, 4, 128], BF16) for i in range(4)]
        for i in range(4):
            k.op("pool", lambda e, i=i: e.memset(pad[i][:], 0.0), w=[pad[i]])
            k.op("pool", lambda e, i=i: e.memset(padc[i][:], 0.0), w=[padc[i]])
        it = 0
        for blk in range(T // 512 + 1):
            is_ctx = blk == T // 512
            ncol, L = (1, 256) if is_ctx else (4, 128)
            p0 = T if is_ctx else blk * 512
            n = ncol * L
            npt = n // 128
            tk = tok[blk % 2]
            for ct in range(32):
                if is_ctx and ct >= 24:
                    continue
                j = it % 4
                it += 1
                pb = padc[j] if is_ctx else pad[j]
                src = d_XBT[ct * 128:(ct + 1) * 128, p0:p0 + n] if ct < 24 else d_CT[(ct - 24) * 128:(ct - 23) * 128, p0:p0 + n]
                k.dma("sp", pb[:, :, 2:2 + L], src.rearrange("p (c r) -> p c r", r=L), r=[d_XBT if ct < 24 else d_CT], w=[pb])
                av = acc[j][:, 0:n].rearrange("p (c r) -> p c r", r=L)
                k.op("dve", lambda e: e.tensor_scalar(out=av, in0=pb[:, :, 0:L], scalar1=cw[:, ct, 0:1], scalar2=cb[:, ct:ct + 1],
                                                      op0=ALU.mult, op1=ALU.add), r=[pb, cw, cb], w=[acc[j]])
                for kk in range(1, 5):
                    k.op("dve", lambda e, kk=kk: e.scalar_tensor_tensor(out=av, in0=pb[:, :, kk:kk + L], scalar=cw[:, ct, kk:kk + 1], in1=av,
                                                                        op0=ALU.mult, op1=ALU.add), r=[pb, cw, acc[j]], w=[acc[j]])
                k.op("act", lambda e: e.activation(out=res[j][:, 0:n], in_=acc[j][:, 0:n], func=AF.Silu), r=[acc[j]], w=[res[j]])
                if ct >= 16:
                    if ct < 24:
                        k.dma("sp", d_BT[(ct - 16) * 128:(ct - 15) * 128, p0:p0 + n], res[j][:, 0:n], r=[res[j]], w=[d_BT])
                    else:
                        k.dma("sp", d_CT2[(ct - 24) * 128:(ct - 23) * 128, p0:p0 + n], res[j][:, 0:n], r=[res[j]], w=[d_CT2])
                if ct < 24:
                    tp = tps[j]
                    for a in range(npt):
                        k.op("pe", lambda e, a=a: e.transpose(out=tp[:, a, :], in_=res[j][:, a * 128:(a + 1) * 128], identity=ident_b[:]),
                             r=[res[j], ident_b], w=[tp])
                    k.op("act", lambda e: e.copy(out=tk[:, 0:npt, ct * 128:(ct + 1) * 128], in_=tp[:, 0:npt, :]), r=[tp], w=[tk])
            k.dma("sp", d_XH[p0:p0 + n, :].rearrange("(a p) e -> p a e", p=128), tk[:, 0:npt, 0:2048], r=[tk], w=[d_XH])
            k.dma("sp", d_BM[p0:p0 + n, :].rearrange("(a p) e -> p a e", p=128), tk[:, 0:npt, 2048:3072], r=[tk], w=[d_BM])
        k.pop()
    for nm_, src_, w_ in (("XHs", d_XH, 2048), ("BMs", d_BM, 1024)):
        if nm_ in dbg:
            dd = scratch(nm_, [384, w_], BF16)
            k.dma("sp", dd[0:128, :], src_[0:128, :], r=[src_], w=[dd])
            k.dma("sp", dd[128:256, :], src_[T - 128:T, :], r=[src_], w=[dd])
            k.dma("sp", dd[256:384, :], src_[TT - 128:TT, :], r=[src_], w=[dd])
    if "CTs" in dbg:
        dd = scratch("CTs", [1024, 256], BF16)
        k.dma("sp", dd[:, 0:128], d_CT2[:, 0:128], r=[d_CT2], w=[dd])
        k.dma("sp", dd[:, 128:256], d_CT2[:, T - 128:T], r=[d_CT2], w=[dd])

    d_YF = scratch("YF", [T, 2048]); d_YB = scratch("YB", [T, 2048])
    if sel is None and stages >= 3 or (sel is not None and "s" in sel):
        k.push()
        msk = {}
        for nm in ("incl_f", "incl_b", "strict_f", "strict_b"):
            msk[nm] = k.sb("m2_" + nm, [128, 128], F32)
            k.dma("sp", msk[nm][:], cmask128[nm][:, :], w=[msk[nm]])
        ones_s = k.sb("ones_s", [128, 128], F32)
        k.op("pool", lambda e: e.memset(ones_s[:], 1.0), w=[ones_s])
        sv = k.sb("sv", [128, 3, 64], F32)
        k.dma("sp", sv[:], ssd_vec.partition_broadcast(128), w=[sv])
        na = k.sb("na", [128, 64], F32)
        k.op("act", lambda e: e.activation(out=na[:], in_=sv[:, 1, :], func=AF.Exp), r=[sv], w=[na])
        k.op("dve", lambda e: e.tensor_scalar(out=na[:], in0=na[:], scalar1=-1.0, scalar2=None, op0=ALU.mult), r=[na], w=[na])
        NB = 2
        CS = 128
        xh_b = [k.sb("xh_b%d" % i, [128, NB, 2048], BF16) for i in range(2)]
        bm_b = [k.sb("bm_b%d" % i, [128, NB, 1024], BF16) for i in range(2)]
        BT_b = [k.sb("BT_b%d" % i, [128, 8, NB * CS], BF16) for i in range(2)]
        CT_b = [k.sb("CT_b%d" % i, [128, 8, NB * CS], BF16) for i in range(2)]
        dt_b = [k.sb("dt_b%d" % i, [128, NB, 32], F32) for i in range(2)]
        la = k.sb("la", [128, NB, 32], F32)
        yst = k.sb("yst", [128, NB, 2048], F32)
        xdt = k.sb("xdt", [128, 32, 64], BF16)
        xdd = k.sb("xdd", [128, 32, 64], BF16)
        Am = k.sb("Am", [128, 32, 128], F32)
        cbm = [k.sb("cbm%d" % i, [128, 2, 128], F32) for i in range(2)]
        Lh = [k.sb("Lh%d" % i, [128, 8, 128], F32) for i in range(2)]
        Mt = [k.sb("Mt%d" % i, [128, 8, 128], BF16) for i in range(2)]
        tmp = [k.sb("tmp%d" % i, [128, 8, 64], F32) for i in range(2)]
        ex = k.sb("ex", [128, 2, 32], F32)
        EL = k.sb("EL", [128, 32], F32)
        hT = k.sb("hT", [128, 32, 64], F32)
        hTbf = k.sb("hTbf", [128, 32, 64], BF16)
        small_ps = k.ps("small_ps", [128, 3, 32], F32)
        cb_ps = k.ps("cb_ps", [128, 2, 128], F32)
        X_ps = k.ps("X_ps", [128, 8, 128], F32)
        Y_ps = k.ps("Y_ps", [128, 512], F32)
        Z_ps = k.ps("Z_ps", [128, 512], F32)
        S_ps = k.ps("S_ps", [128, 512], F32)

        for d_ in range(2):
            incl = msk["incl_f" if d_ == 0 else "incl_b"]
            strict = msk["strict_f" if d_ == 0 else "strict_b"]
            d_Y = d_YF if d_ == 0 else d_YB
            k.op("pool", lambda e: e.memset(hT[:], 0.0), w=[hT])
            k.op("pool", lambda e: e.memset(hTbf[:], 0.0), w=[hTbf])
            nblk = T // (NB * CS)
            if d_ == 0:
                blocks = [(T, True)] + [(b * NB * CS, False) for b in range(nblk)]
            else:
                blocks = [(T, True)] + [(b * NB * CS, False) for b in reversed(range(nblk))]
            for bi, (p0, is_ctx) in enumerate(blocks):
                j = bi % 2
                n = NB * CS
                k.dma("sp", xh_b[j][:], d_XH[p0:p0 + n, :].rearrange("(c s) e -> s c e", s=CS), r=[d_XH], w=[xh_b[j]])
                k.dma("sp", bm_b[j][:], d_BM[p0:p0 + n, :].rearrange("(c s) e -> s c e", s=CS), r=[d_BM], w=[bm_b[j]])
                k.dma("sp", dt_b[j][:], d_DT[p0:p0 + n, d_ * 32:(d_ + 1) * 32].rearrange("(c s) h -> s c h", s=CS), r=[d_DT], w=[dt_b[j]])
                if not is_ctx:
                    k.dma("sp", BT_b[j][:], d_BT[:, p0:p0 + n].rearrange("(g q) s -> q g s", q=128), r=[d_BT], w=[BT_b[j]])
                    k.dma("sp", CT_b[j][:], d_CT2[:, p0:p0 + n].rearrange("(g q) s -> q g s", q=128), r=[d_CT2], w=[CT_b[j]])
                k.op("dve", lambda e: e.tensor_tensor(out=dt_b[j][:], in0=dt_b[j][:],
                                                      in1=sv[:, 0, d_ * 32:(d_ + 1) * 32].unsqueeze(1).to_broadcast([128, NB, 32]), op=ALU.add),
                     r=[dt_b[j], sv], w=[dt_b[j]])
                k.op("act", lambda e: e.activation(out=dt_b[j][:], in_=dt_b[j][:], func=AF.Exp), r=[dt_b[j]], w=[dt_b[j]])
                k.op("act", lambda e: e.activation(out=dt_b[j][:], in_=dt_b[j][:], func=AF.Ln, bias=1.0, scale=1.0), r=[dt_b[j]], w=[dt_b[j]])
                k.op("dve", lambda e: e.tensor_tensor(out=la[:], in0=dt_b[j][:],
                                                      in1=na[:, d_ * 32:(d_ + 1) * 32].unsqueeze(1).to_broadcast([128, NB, 32]), op=ALU.mult),
                     r=[dt_b[j], na], w=[la])
                corder = list(range(NB)) if d_ == 0 else list(reversed(range(NB)))
                for c in corder:
                    cs = slice(c * CS, (c + 1) * CS)
                    k.op("pe", lambda e: e.matmul(small_ps[:, 0, :], lhsT=incl[:], rhs=la[:, c, :], start=True, stop=True), r=[incl, la], w=[small_ps])
                    k.op("pe", lambda e: e.matmul(small_ps[:, 1, :], lhsT=strict[:], rhs=la[:, c, :], start=True, stop=True), r=[strict, la], w=[small_ps])
                    k.op("pe", lambda e: e.matmul(small_ps[:, 2, :], lhsT=ones_s[:], rhs=la[:, c, :], start=True, stop=True), r=[ones_s, la], w=[small_ps])
                    k.op("act", lambda e: e.activation(out=ex[:], in_=small_ps[:, 0:2, :], func=AF.Exp), r=[small_ps], w=[ex])
                    k.op("act", lambda e: e.activation(out=EL[:], in_=small_ps[:, 2, :], func=AF.Exp), r=[small_ps], w=[EL])
                    k.op("dve", lambda e: e.tensor_tensor(out=xdt[:], in0=xh_b[j][:, c, :].rearrange("s (h p) -> s h p", p=64),
                                                          in1=dt_b[j][:, c, :].unsqueeze(2).to_broadcast([128, 32, 64]), op=ALU.mult),
                         r=[xh_b[j], dt_b[j]], w=[xdt])
                    k.op("pool", lambda e: e.tensor_tensor(out=xdd[:], in0=xdt[:], in1=ex[:, 1, :].unsqueeze(2).to_broadcast([128, 32, 64]), op=ALU.mult),
                         r=[xdt, ex], w=[xdd])
                    if not is_ctx:
                        k.op("dve", lambda e: e.tensor_tensor(out=Am[:], in0=la[:, c, :].unsqueeze(2).to_broadcast([128, 32, 128]),
                                                              in1=strict[:].unsqueeze(1).to_broadcast([128, 32, 128]), op=ALU.mult), r=[la, strict], w=[Am])
                    for q in range(4):
                        hs = slice(q * 8, (q + 1) * 8)
                        qq = q % 2
                        if not is_ctx:
                            for gg in range(2):
                                g = q * 2 + gg
                                k.op("pe", lambda e, gg=gg, g=g: e.matmul(cb_ps[:, gg, :], lhsT=BT_b[j][:, g, cs], rhs=CT_b[j][:, g, cs], start=True, stop=True),
                                     r=[BT_b[j], CT_b[j]], w=[cb_ps])
                            k.op("dve", lambda e: e.tensor_tensor(out=cbm[qq][:], in0=cb_ps[:], in1=incl[:].unsqueeze(1).to_broadcast([128, 2, 128]), op=ALU.mult),
                                 r=[cb_ps, incl], w=[cbm[qq]])
                            for hh in range(8):
                                k.op("pe", lambda e, hh=hh: e.matmul(X_ps[:, hh, :], lhsT=Am[:, q * 8 + hh, :], rhs=incl[:], start=True, stop=True),
                                     r=[Am, incl], w=[X_ps])
                            k.op("act", lambda e: e.activation(out=Lh[qq][:], in_=X_ps[:], func=AF.Exp), r=[X_ps], w=[Lh[qq]])
                            k.op("dve", lambda e: e.tensor_tensor(out=Mt[qq][:].rearrange("s (g a) t -> s g a t", a=4),
                                                                  in0=Lh[qq][:].rearrange("s (g a) t -> s g a t", a=4),
                                                                  in1=cbm[qq][:].unsqueeze(2).to_broadcast([128, 2, 4, 128]), op=ALU.mult),
                                 r=[Lh[qq], cbm[qq]], w=[Mt[qq]])
                            for hh in range(8):
                                k.op("pe", lambda e, hh=hh: e.matmul(Y_ps[:, hh * 64:(hh + 1) * 64], lhsT=Mt[qq][:, hh, :], rhs=xdt[:, q * 8 + hh, :],
                                                                     start=True, stop=True), r=[Mt[qq], xdt], w=[Y_ps])
                            for gg in range(2):
                                g = q * 2 + gg
                                k.op("pe", lambda e, gg=gg, g=g: e.matmul(Z_ps[:, gg * 256:(gg + 1) * 256], lhsT=CT_b[j][:, g, cs],
                                                                          rhs=hTbf[:, g * 4:(g + 1) * 4, :].rearrange("q a p -> q (a p)"),
                                                                          start=True, stop=True), r=[CT_b[j], hTbf], w=[Z_ps])
                            k.op("dve", lambda e: e.tensor_tensor(out=tmp[qq][:], in0=Z_ps[:].rearrange("s (h p) -> s h p", p=64),
                                                                  in1=ex[:, 0, hs].unsqueeze(2).to_broadcast([128, 8, 64]), op=ALU.mult),
                                 r=[Z_ps, ex], w=[tmp[qq]])
                            yv = yst[:, c, q * 512:(q + 1) * 512]
                            k.op("dve", lambda e: e.tensor_tensor(out=yv, in0=Y_ps[:], in1=tmp[qq][:].rearrange("s h p -> s (h p)"), op=ALU.add),
                                 r=[Y_ps, tmp[qq]], w=[yst])
                            if d_ == 0:
                                k.op("pool", lambda e: e.tensor_tensor(out=tmp[qq][:], in0=xh_b[j][:, c, q * 512:(q + 1) * 512].rearrange("s (h p) -> s h p", p=64),
                                                                      in1=sv[:, 2, hs].unsqueeze(2).to_broadcast([128, 8, 64]), op=ALU.mult),
                                     r=[xh_b[j], sv], w=[tmp[qq]])
                                k.op("pool", lambda e: e.tensor_tensor(out=yv, in0=yv, in1=tmp[qq][:].rearrange("s h p -> s (h p)"), op=ALU.add),
                                     r=[yst, tmp[qq]], w=[yst])
                        for gg in range(2):
                            g = q * 2 + gg
                            k.op("pe", lambda e, gg=gg, g=g: e.matmul(S_ps[:, gg * 256:(gg + 1) * 256], lhsT=bm_b[j][:, c, g * 128:(g + 1) * 128],
                                                                      rhs=xdd[:, g * 4:(g + 1) * 4, :].rearrange("s a p -> s (a p)"),
                                                                      start=True, stop=True), r=[bm_b[j], xdd], w=[S_ps])
                        k.op("dve", lambda e: e.tensor_tensor(out=hT[:, hs, :], in0=hT[:, hs, :], in1=EL[:, hs].unsqueeze(2).to_broadcast([128, 8, 64]),
                                                              op=ALU.mult), r=[hT, EL], w=[hT])
                        k.op("dve", lambda e: e.tensor_tensor(out=hT[:, hs, :], in0=hT[:, hs, :], in1=S_ps[:].rearrange("q (h p) -> q h p", p=64),
                                                              op=ALU.add), r=[hT, S_ps], w=[hT])
                        k.op("act", lambda e: e.copy(out=hTbf[:, hs, :], in_=hT[:, hs, :]), r=[hT], w=[hTbf])
                if not is_ctx:
                    k.dma("sp", d_Y[p0:p0 + n, :].rearrange("(c s) e -> s c e", s=CS), yst[:], r=[yst], w=[d_Y])
            k.barrier()
        k.pop()
    for nm_, src_ in (("YFs", d_YF), ("YBs", d_YB)):
        if nm_ in dbg:
            dd = scratch(nm_, [256, 2048])
            k.dma("sp", dd[0:128, :], src_[0:128, :], r=[src_], w=[dd])
            k.dma("sp", dd[128:256, :], src_[T - 128:T, :], r=[src_], w=[dd])

    d_YZ = scratch("YZ", [T, 2048], BF16)
    if sel is None and stages >= 3 or (sel is not None and "g" in sel):
        k.push()
        mnw = k.sb("mnw", [128, 2048], F32)
        k.dma("sp", mnw[:], ma_nw.partition_broadcast(128), w=[mnw])
        yfb = [k.sb("yfb%d" % i, [128, 2048], F32) for i in range(2)]
        ybb = [k.sb("ybb%d" % i, [128, 2048], F32) for i in range(2)]
        zb = [k.sb("zb%d" % i, [128, 2048], F32) for i in range(2)]
        junk = k.sb("junk", [128, 256], F32)
        ssq = [k.sb("ssq%d" % i, [128, 8], F32) for i in range(2)]
        yzo = [k.sb("yzo%d" % i, [128, 2048], BF16) for i in range(2)]
        d_YZ_v = d_YZ.t.rearrange("(r c) e -> r c e", c=GW)
        for c in range(GW):
            j = c % 2
            k.dma("sp", yfb[j][:], d_YF[c * 128:(c + 1) * 128, :], r=[d_YF], w=[yfb[j]])
            k.dma("sp", ybb[j][:], d_YB[c * 128:(c + 1) * 128, :], r=[d_YB], w=[ybb[j]])
            k.dma("sp", zb[j][:], d_Z[c * 128:(c + 1) * 128, :], r=[d_Z], w=[zb[j]])
            k.op("pool", lambda e: e.tensor_tensor(out=yfb[j][:], in0=yfb[j][:], in1=ybb[j][:], op=ALU.add), r=[yfb[j], ybb[j]], w=[yfb[j]])
            k.op("dve", lambda e: e.tensor_tensor(out=yfb[j][:], in0=yfb[j][:], in1=zb[j][:], op=ALU.mult), r=[yfb[j], zb[j]], w=[yfb[j]])
            for g in range(8):
                k.op("act", lambda e, g=g: e.activation(out=junk[:], in_=yfb[j][:, g * 256:(g + 1) * 256], func=AF.Square, accum_out=ssq[j][:, g:g + 1]),
                     r=[yfb[j]], w=[junk, ssq[j]])
            k.op("act", lambda e: e.activation(out=ssq[j][:], in_=ssq[j][:], func=AF.Sqrt, bias=eps_t[:], scale=1.0 / 256.0), r=[ssq[j], eps_t], w=[ssq[j]])
            k.op("dve", lambda e: e.reciprocal(out=ssq[j][:], in_=ssq[j][:]), r=[ssq[j]], w=[ssq[j]])
            k.op("dve", lambda e: e.tensor_tensor(out=yfb[j][:].rearrange("p (g q) -> p g q", q=256), in0=yfb[j][:].rearrange("p (g q) -> p g q", q=256),
                                                  in1=ssq[j][:].unsqueeze(2).to_broadcast([128, 8, 256]), op=ALU.mult), r=[yfb[j], ssq[j]], w=[yfb[j]])
            k.op("pool", lambda e: e.tensor_tensor(out=yzo[j][:], in0=yfb[j][:], in1=mnw[:], op=ALU.mult), r=[yfb[j], mnw], w=[yzo[j]])
            k.dma("sp", d_YZ_v[:, c, :], yzo[j][:], r=[yzo[j]], w=[d_YZ])
        k.pop()
    if "YZs" in dbg:
        dd = scratch("YZs", [256, 2048], BF16)
        k.dma("sp", dd[0:128, :], d_YZ[0:128, :], r=[d_YZ], w=[dd])
        k.dma("sp", dd[128:256, :], d_YZ[T - 128:T, :], r=[d_YZ], w=[dd])

    d_X1 = scratch("X1", [T, 1024]); d_U2T = scratch("U2T", [1024, T], BF16)

    def bcast_rows(cols_ap, dst, tag):
        k.push()
        tpp = k.ps("bc_tp" + tag, [8, 128], F32)
        bcp = k.ps("bc_ps" + tag, [128, 1024], F32)
        gT = k.sb("bc_gT" + tag, [8, 128], F32)
        blk = k.sb("bc_blk" + tag, [8, 8, 128], F32)
        on8 = k.sb("bc_on" + tag, [8, 128], F32)
        k.op("pool", lambda e: e.memset(on8[:], 1.0), w=[on8])
        k.op("pe", lambda e: e.transpose(out=tpp[:], in_=cols_ap, identity=ident_f[:]), r=[modT, ident_f], w=[tpp])
        k.op("dve", lambda e: e.tensor_copy(out=gT[:], in_=tpp[:]), r=[tpp], w=[gT])
        k.op("dve", lambda e: e.tensor_tensor(out=blk[:], in0=gT[:].unsqueeze(1).to_broadcast([8, 8, 128]),
                                              in1=ident_f[0:8, 0:8].unsqueeze(2).to_broadcast([8, 8, 128]), op=ALU.mult), r=[gT, ident_f], w=[blk])
        for hf in range(2):
            k.op("pe", lambda e, hf=hf: e.matmul(bcp[:, hf * 512:(hf + 1) * 512], lhsT=on8[:], rhs=blk[:].rearrange("a b c -> a (b c)")[:, hf * 512:(hf + 1) * 512],
                                                 start=True, stop=True), r=[on8, blk], w=[bcp])
            k.op("dve", lambda e, hf=hf: e.tensor_copy(out=dst[:, hf * 512:(hf + 1) * 512], in_=bcp[:, hf * 512:(hf + 1) * 512]), r=[bcp], w=[dst])
        k.pop()

    def ln_sb(src, dst_ap, dst, st6_, mv_, rstd_, nmr_):
        for h in range(2):
            k.op("dve", lambda e, h=h: e.bn_stats(out=st6_[:, h, :], in_=src[:, h * 512:(h + 1) * 512]), r=[src], w=[st6_])
        k.op("dve", lambda e: e.bn_aggr(out=mv_[:], in_=st6_[:].rearrange("p a b -> p (a b)")), r=[st6_], w=[mv_])
        k.op("act", lambda e: e.activation(out=rstd_[:], in_=mv_[:, 1:2], func=AF.Sqrt, bias=eps_t[:], scale=1.0), r=[mv_, eps_t], w=[rstd_])
        k.op("dve", lambda e: e.reciprocal(out=rstd_[:], in_=rstd_[:]), r=[rstd_], w=[rstd_])
        k.op("dve", lambda e: e.scalar_tensor_tensor(out=nmr_[:], in0=mv_[:, 0:1], scalar=-1.0, in1=rstd_[:], op0=ALU.mult, op1=ALU.mult),
             r=[mv_, rstd_], w=[nmr_])
        k.op("act", lambda e: e.activation(out=dst_ap, in_=src[:], func=AF.Identity, bias=nmr_[:], scale=rstd_[:]), r=[src, nmr_, rstd_], w=[dst])

    if sel is None and stages >= 4 or (sel is not None and "m" in sel):
        k.push()
        wa = k.sb("wa", [128, 8, 1024], BF16)
        wb = k.sb("wb", [128, 16, 1024], BF16)
        wo = k.sb("wo", [128, 8, 1024], BF16)
        g1b = k.sb("g1b", [128, 1024], F32)
        l1g = k.sb("l1g", [128, 1024], F32)
        l1b = k.sb("l1b", [128, 1024], F32)
        k.dma("sp", l1g[:], ln1_g.partition_broadcast(128), w=[l1g])
        k.dma("sp", l1b[:], ln1_b.partition_broadcast(128), w=[l1b])
        k.push()
        wstg = [k.sb("wstg%d" % i, [128, 4, 1024], F32) for i in range(2)]
        ci = 0
        for (dst, src, nk) in ((wa, w_ba, 8), (wb, w_bb, 16), (wo, w_o, 8)):
            sv_ = src.rearrange("(kk p) e -> p kk e", p=128)
            for k0 in range(0, nk, 4):
                st = wstg[ci % 2]
                ci += 1
                k.dma("sp", st[:], sv_[:, k0:k0 + 4, :], w=[st])
                for q in range(4):
                    k.op("pool" if q % 2 == 0 else "dve", lambda e, q=q: e.tensor_copy(out=dst[:, k0 + q, :], in_=st[:, q, :]), r=[st], w=[dst])
        k.pop()
        bcast_rows(modT[:, 16:24, 0], g1b, "g1")
        TB = 256
        ont2 = [k.sb("ont%d" % i, [128, 8, TB], BF16) for i in range(2)]
        gat2 = [k.sb("gat%d" % i, [128, 8, TB], F32) for i in range(2)]
        gbt2 = [k.sb("gbt%d" % i, [128, 8, TB], F32) for i in range(2)]
        yzt2 = [k.sb("yzt%d" % i, [128, 2, 2048], BF16) for i in range(2)]
        xtl2 = [k.sb("xtl%d" % i, [128, 2, 1024], F32) for i in range(2)]
        yzT = k.sb("yzT", [128, 16, TB], BF16)
        m1_2 = [k.sb("s4_m1_%d" % i, [128, TB], F32) for i in range(2)]
        m2_2 = [k.sb("s4_m2_%d" % i, [128, TB], F32) for i in range(2)]
        mT = k.sb("mT", [128, 8, TB], BF16)
        t1 = k.sb("s4_t1", [128, 1024], F32)
        xr = k.sb("s4_xr", [128, 1024], F32)
        xn1 = k.sb("s4_xn1", [128, 1024], F32)
        x1t = k.sb("x1t", [128, 1024], F32)
        xn2 = k.sb("s4_xn2", [128, 1024], BF16)
        u2b = k.sb("u2b", [128, 8, TB], BF16)
        st6_ = k.sb("s4_st6", [128, 2, 6], F32); mv_ = k.sb("s4_mv", [128, 2], F32)
        rstd_ = k.sb("s4_rstd", [128, 1], F32); nmr_ = k.sb("s4_nmr", [128, 1], F32)
        ya_ps2 = [k.ps("ya_ps%d" % i, [128, TB], F32) for i in range(2)]
        yb_ps2 = [k.ps("yb_ps%d" % i, [128, TB], F32) for i in range(2)]
        trp = k.ps("trp", [128, 8, 128], BF16)
        mix_ps = k.ps("mix_ps", [128, 1024], F32)
        u2p = k.ps("u2p", [128, 8, 128], BF16)
        if "MIXs" in dbg:
            d_MIX = scratch("MIXs", [256, 1024])
            mixs = k.sb("mixs", [128, 1024], F32)
        for b_ in range(T // TB):
            t0 = b_ * TB
            ont, gat, gbt, yzt, xtl = ont2[b_ % 2], gat2[b_ % 2], gbt2[b_ % 2], yzt2[b_ % 2], xtl2[b_ % 2]
            k.dma("sp", ont[:], d_ONT[:, t0:t0 + TB].rearrange("(kk p) t -> p kk t", p=128), r=[d_ONT], w=[ont])
            k.dma("sp", gat[:], d_GAT[:, t0:t0 + TB].rearrange("(kk p) t -> p kk t", p=128), r=[d_GAT], w=[gat])
            k.dma("sp", gbt[:], d_GBT[:, t0:t0 + TB].rearrange("(kk p) t -> p kk t", p=128), r=[d_GBT], w=[gbt])
            k.dma("sp", yzt[:], d_YZ[t0:t0 + TB, :].rearrange("(a p) e -> p a e", p=128), r=[d_YZ], w=[yzt])
            k.dma("sp", xtl[:], x[t0:t0 + TB, :].rearrange("(a p) e -> p a e", p=128), w=[xtl])
            for a in range(TB // 128):
                for hf in range(2):
                    for q in range(8):
                        k.op("pe", lambda e, q=q: e.transpose(out=trp[:, q, :], in_=yzt[:, a, (hf * 8 + q) * 128:(hf * 8 + q + 1) * 128], identity=ident_b[:]),
                             r=[yzt, ident_b], w=[trp])
                    k.op("act", lambda e: e.copy(out=yzT[:, hf * 8:(hf + 1) * 8, a * 128:(a + 1) * 128], in_=trp[:]), r=[trp], w=[yzT])
            for db in range(8):
                ya_ps, yb_ps, m1, m2 = ya_ps2[db % 2], yb_ps2[db % 2], m1_2[db % 2], m2_2[db % 2]
                for kk in range(8):
                    k.op("pe", lambda e, kk=kk: e.matmul(ya_ps[:], lhsT=wa[:, kk, db * 128:(db + 1) * 128], rhs=ont[:, kk, :], start=(kk == 0), stop=(kk == 7)),
                         r=[wa, ont], w=[ya_ps])
                for kk in range(16):
                    k.op("pe", lambda e, kk=kk: e.matmul(yb_ps[:], lhsT=wb[:, kk, db * 128:(db + 1) * 128], rhs=yzT[:, kk, :], start=(kk == 0), stop=(kk == 15)),
                         r=[wb, yzT], w=[yb_ps])
                k.op("dve", lambda e: e.tensor_tensor(out=m1[:], in0=ya_ps[:], in1=gat[:, db, :], op=ALU.mult), r=[ya_ps, gat], w=[m1])
                k.op("dve", lambda e: e.tensor_tensor(out=m2[:], in0=yb_ps[:], in1=gbt[:, db, :], op=ALU.mult), r=[yb_ps, gbt], w=[m2])
                k.op("pool", lambda e: e.tensor_tensor(out=mT[:, db, :], in0=m1[:], in1=m2[:], op=ALU.add), r=[m1, m2], w=[mT])
            for a in range(TB // 128):
                for hf in range(2):
                    for kk in range(8):
                        k.op("pe", lambda e, kk=kk: e.matmul(mix_ps[:, hf * 512:(hf + 1) * 512], lhsT=mT[:, kk, a * 128:(a + 1) * 128],
                                                             rhs=wo[:, kk, hf * 512:(hf + 1) * 512], start=(kk == 0), stop=(kk == 7)), r=[mT, wo], w=[mix_ps])
                if "MIXs" in dbg and (b_ == 0 or b_ == T // TB - 1) and a == (0 if b_ == 0 else 1):
                    for hf in range(2):
                        k.op("dve", lambda e, hf=hf: e.tensor_copy(out=mixs[:, hf * 512:(hf + 1) * 512], in_=mix_ps[:, hf * 512:(hf + 1) * 512]), r=[mix_ps], w=[mixs])
                    o_ = 0 if b_ == 0 else 128
                    k.dma("sp", d_MIX[o_:o_ + 128, :], mixs[:], r=[mixs], w=[d_MIX])
                for hf in range(2):
                    k.op("dve", lambda e, hf=hf: e.tensor_tensor(out=t1[:, hf * 512:(hf + 1) * 512], in0=mix_ps[:, hf * 512:(hf + 1) * 512],
                                                                 in1=g1b[:, hf * 512:(hf + 1) * 512], op=ALU.mult), r=[mix_ps, g1b], w=[t1])
                k.op("dve", lambda e: e.scalar_tensor_tensor(out=xr[:], in0=xtl[:, a, :], scalar=DN_ALPHA, in1=t1[:], op0=ALU.mult, op1=ALU.add),
                     r=[xtl, t1], w=[xr])
                ln_sb(xr, xn1[:], xn1, st6_, mv_, rstd_, nmr_)
                k.op("pool", lambda e: e.tensor_tensor(out=xn1[:], in0=xn1[:], in1=l1g[:], op=ALU.mult), r=[xn1, l1g], w=[xn1])
                k.op("pool", lambda e: e.tensor_tensor(out=x1t[:], in0=xn1[:], in1=l1b[:], op=ALU.add), r=[xn1, l1b], w=[x1t])
                k.dma("sp", d_X1[t0 + a * 128:t0 + (a + 1) * 128, :], x1t[:], r=[x1t], w=[d_X1])
                ln_sb(x1t, xn2[:], xn2, st6_, mv_, rstd_, nmr_)
                for kk in range(8):
                    k.op("pe", lambda e, kk=kk: e.transpose(out=u2p[:, kk, :], in_=xn2[:, kk * 128:(kk + 1) * 128], identity=ident_b[:]),
                         r=[xn2, ident_b], w=[u2p])
                for kk in range(8):
                    k.op("act", lambda e, kk=kk: e.activation(out=u2b[:, kk, a * 128:(a + 1) * 128], in_=u2p[:, kk, :], func=AF.Identity,
                                                              bias=modT[:, 24 + kk, 0:1], scale=ops2[:, kk, 0:1]), r=[u2p, modT, ops2], w=[u2b])
            k.dma("sp", d_U2T[:, t0:t0 + TB].rearrange("(kk p) t -> p kk t", p=128), u2b[:], r=[u2b], w=[d_U2T])
        k.pop()
    if "X1s" in dbg:
        dd = scratch("X1s", [256, 1024])
        k.dma("sp", dd[0:128, :], d_X1[0:128, :], r=[d_X1], w=[dd])
        k.dma("sp", dd[128:256, :], d_X1[T - 128:T, :], r=[d_X1], w=[dd])
    if "U2s" in dbg:
        dd = scratch("U2s", [1024, 256], BF16)
        k.dma("sp", dd[:, 0:128], d_U2T[:, 0:128], r=[d_U2T], w=[dd])
        k.dma("sp", dd[:, 128:256], d_U2T[:, T - 128:T], r=[d_U2T], w=[dd])

    d_VB = scratch("VB", [16384, 1024], BF16)
    d_UT = scratch("UT", [128, 128, 1024], BF16)
    d_Q2 = scratch("Q2", [2048, T], BF16)
    d_FFN = scratch("FFN", [T, 1024])
    run5 = sel is None and stages >= 5 or (sel is not None and "p" in sel)
    if run5:
        k.push()
        vst = [k.sb("p_vst%d" % i, [128, 4, 1024], F32) for i in range(3)]
        vbf = [k.sb("p_vbf%d" % i, [128, 4, 1024], BF16) for i in range(3)]
        utb = [k.sb("p_utb%d" % i, [128, 4, 1024], BF16) for i in range(3)]
        ctp = [k.ps("p_ctp%d" % i, [128, 8, 128], BF16) for i in range(4)]
        pv_v = peer_v.rearrange("(i p) d -> p i d", p=128)
        pu_v = peer_u.rearrange("(i p) d -> p i d", p=128)
        d_VB_v = d_VB.t.rearrange("(i p) d -> p i d", p=128)
        d_UT_v = d_UT.t.rearrange("i p e -> p i e")
        for it in range(32):
            j = it % 3
            i0 = it * 4
            k.dma("sp", vst[j][:], pv_v[:, i0:i0 + 4, :], w=[vst[j]])
            for q in range(4):
                k.op("pool" if q % 2 == 0 else "dve", lambda e, q=q: e.tensor_copy(out=vbf[j][:, q, :], in_=vst[j][:, q, :]), r=[vst[j]], w=[vbf[j]])
            k.dma("sp", d_VB_v[:, i0:i0 + 4, :], vbf[j][:], r=[vbf[j]], w=[d_VB])
        for it in range(32):
            j = it % 3
            i0 = it * 4
            k.dma("sp", vst[j][:], pu_v[:, i0:i0 + 4, :], w=[vst[j]])
            for q in range(4):
                k.op("pool" if q % 2 == 0 else "dve", lambda e, q=q: e.tensor_copy(out=vbf[j][:, q, :], in_=vst[j][:, q, :]), r=[vst[j]], w=[vbf[j]])
            for q in range(4):
                tp = ctp[q % 4]
                for kk in range(8):
                    k.op("pe", lambda e, kk=kk, q=q: e.transpose(out=tp[:, kk, :], in_=vbf[j][:, q, kk * 128:(kk + 1) * 128], identity=ident_b[:]),
                         r=[vbf[j], ident_b], w=[tp])
                k.op("act", lambda e, q=q: e.copy(out=utb[j][:, q, :].rearrange("p (kk j) -> p kk j", j=128), in_=tp[:]), r=[tp], w=[utb[j]])
            k.dma("sp", d_UT_v[:, i0:i0 + 4, :], utb[j][:], r=[utb[j]], w=[d_UT])
        k.pop()
        k.push()
        wqs = k.sb("p_wqs", [128, 8, 512], F32)
        wqb = k.sb("p_wqb", [128, 8, 2048], BF16)
        wq_v = peer_wq.rearrange("(kk p) e -> p kk e", p=128)
        for pc in range(4):
            k.dma("sp", wqs[:], wq_v[:, :, pc * 512:(pc + 1) * 512], w=[wqs])
            for kk in range(8):
                k.op("pool" if kk % 2 == 0 else "dve", lambda e, kk=kk: e.tensor_copy(out=wqb[:, kk, pc * 512:(pc + 1) * 512], in_=wqs[:, kk, :]), r=[wqs], w=[wqb])
        u2l = [k.sb("p_u2l%d" % i, [128, 8, 512], BF16) for i in range(2)]
        qst = [k.sb("p_qst%d" % i, [128, 512], BF16) for i in range(2)]
        qps = [k.ps("p_qps%d" % i, [128, 512], F32) for i in range(2)]
        n_ = 0
        for b_ in range(T // 512):
            j = b_ % 2
            t0 = b_ * 512
            k.dma("sp", u2l[j][:], d_U2T[:, t0:t0 + 512].rearrange("(kk p) t -> p kk t", p=128), r=[d_U2T], w=[u2l[j]])
            for g in range(16):
                jj = n_ % 2
                n_ += 1
                for kk in range(8):
                    k.op("pe", lambda e, kk=kk: e.matmul(qps[jj][:], lhsT=wqb[:, kk, g * 128:(g + 1) * 128], rhs=u2l[j][:, kk, :], start=(kk == 0), stop=(kk == 7)),
                         r=[wqb, u2l[j]], w=[qps[jj]])
                k.op("act" if g % 2 == 0 else "dve", (lambda e: e.copy(out=qst[jj][:], in_=qps[jj][:])) if g % 2 == 0 else
                     (lambda e: e.tensor_copy(out=qst[jj][:], in_=qps[jj][:])), r=[qps[jj]], w=[qst[jj]])
                k.dma("sp", d_Q2[g * 128:(g + 1) * 128, t0:t0 + 512], qst[jj][:], r=[qst[jj]], w=[d_Q2])
        k.pop()

    if run5:
        k.push()
        subT = k.sb("p_subT", [128, 16, 128], BF16)
        io128 = k.sb("p_io128", [128, 128], F32)
        g2b = k.sb("p_g2b", [128, 1024], F32)
        l2g = k.sb("p_l2g", [128, 1024], F32)
        l2b = k.sb("p_l2b", [128, 1024], F32)
        k.dma("sp", io128[:], iota_c.partition_broadcast(128), w=[io128])
        k.dma("sp", l2g[:], ln2_g.partition_broadcast(128), w=[l2g])
        k.dma("sp", l2b[:], ln2_b.partition_broadcast(128), w=[l2b])
        bcast_rows(modT[:, 40:48, 0], g2b, "g2")
        k.push()
        sks = k.sb("p_sks", [128, 16, 128], F32)
        skb = k.sb("p_skb", [128, 16, 128], BF16)
        stp = k.ps("p_stp", [128, 8, 128], BF16)
        k.dma("sp", sks[:], peer_sub.rearrange("g n q -> n g q"), w=[sks])
        k.op("dve", lambda e: e.tensor_copy(out=skb[:], in_=sks[:]), r=[sks], w=[skb])
        for hf in range(2):
            for q in range(8):
                k.op("pe", lambda e, q=q: e.transpose(out=stp[:, q, :], in_=skb[:, hf * 8 + q, :], identity=ident_b[:]), r=[skb, ident_b], w=[stp])
            k.op("act", lambda e: e.copy(out=subT[:, hf * 8:(hf + 1) * 8, :], in_=stp[:]), r=[stp], w=[subT])
        k.pop()
        TB5 = 256
        pb = [k.ps("p_pb%d" % i, [128, 512], F32) for i in range(8)]
        qb2 = [k.sb("p_qb%d" % i, [128, 16, TB5], BF16) for i in range(2)]
        u2b_ = k.sb("p_u2b", [128, 8, TB5], BF16)
        W1 = k.sb("p_W1", [128, 2048], F32)
        W2 = k.sb("p_W2", [128, 2048], F32)
        vals = k.sb("p_vals", [128, 16, 16], F32)
        idxu = k.sb("p_idxu", [128, 16, 16], U32)
        idxf = k.sb("p_idxf", [128, 16, 16], F32)
        tv = k.sb("p_tv", [128, 8, 16], F32)
        posu = k.sb("p_posu", [128, 8, 16], U32)
        pau = k.sb("p_pau", [128, 8, 16], U32)
        pbu = k.sb("p_pbu", [128, 8, 16], U32)
        paf = k.sb("p_paf", [128, 8, 16], F32)
        pbf = k.sb("p_pbf", [128, 8, 16], F32)
        gate = k.sb("p_gate", [128, 8, 16], F32)
        ssum = k.sb("p_ssum", [128, 8], F32)
        iij = k.sb("p_iij", [128, 2, 128], F32)
        trT2 = [[k.sb("p_trT%d_%d" % (i, a_), [128, 3, 128], F32) for a_ in range(2)] for i in range(2)]
        Aoh2 = [k.sb("p_A%d" % i, [128, 32, 128], BF16) for i in range(2)]
        Boh2 = [k.sb("p_B%d" % i, [128, 32, 128], BF16) for i in range(2)]
        trTb = k.sb("p_trTb", [128, 3, 128], BF16)
        io128b = k.sb("p_io128b", [128, 128], BF16)
        k.op("dve", lambda e: e.tensor_copy(out=io128b[:], in_=io128[:]), r=[io128], w=[io128b])
        Wt = k.sb("p_Wt", [128, 128, TB5], BF16)
        ut = [k.sb("p_ut%d" % i, [128, 2, 1024], BF16) for i in range(3)]
        vb = [k.sb("p_vb%d" % i, [128, 2, 1024], BF16) for i in range(3)]
        Gf = [k.sb("p_G%d" % i, [128, TB5], F32) for i in range(4)]
        Gw = [k.sb("p_Gw%d" % i, [128, TB5], BF16) for i in range(4)]
        x1l = k.sb("p_x1l", [128, 1024], F32)
        t5 = k.sb("p_t5", [128, 1024], F32)
        xr5 = k.sb("p_xr5", [128, 1024], F32)
        st6_ = k.sb("p_st6", [128, 2, 6], F32); mv_ = k.sb("p_mv", [128, 2], F32)
        rstd_ = k.sb("p_rstd", [128, 1], F32); nmr_ = k.sb("p_nmr", [128, 1], F32)
        S_v = W1[:].rearrange("p (g n) -> p g n", n=128)
        S2_v = W2[:].rearrange("p (g n) -> p g n", n=128)
        cand_v = W1[:].rearrange("p (h c) -> p h c", c=256)
        cand2_v = W2[:].rearrange("p (h c) -> p h c", c=256)
        vals_v = vals[:].rearrange("p (h f) a -> p h f a", f=2)
        idxf_v = idxf[:].rearrange("p (h f) a -> p h f a", f=2)
        oh_v = W1[:].rearrange("p (h q a) -> p h q a", q=16, a=16)
        NEG = -1.0e30
        if "FFNs" in dbg:
            d_FFNs = scratch("FFNs", [256, 1024])
            ffs = xr5
        if "PKs" in dbg:
            d_PK = scratch("PKs", [128, 3, 128])
        nblk5 = T // TB5
        blist = list(range(nblk5)) if not isinstance(stages, (set, list, tuple)) or "P" not in stages else [0, nblk5 - 1]

        def gen_5b1(b_, par):
            t0 = b_ * TB5
            qb_ = qb2[par]
            k.dma("sp", qb_[:], d_Q2[:, t0:t0 + TB5].rearrange("(g q) t -> q g t", q=128), r=[d_Q2], w=[qb_])
            for a in range(TB5 // 128):
                ts_ = slice(a * 128, (a + 1) * 128)
                for q in range(4):
                    for gq in range(4):
                        g = q * 4 + gq
                        k.op("pe", lambda e, g=g, gq=gq: e.matmul(pb[7][:, gq * 128:(gq + 1) * 128], lhsT=qb_[:, g, ts_], rhs=subT[:, g, :], start=True, stop=True),
                             r=[qb_, subT], w=[pb[7]])
                    k.op("act", lambda e, q=q: e.copy(out=W1[:, q * 512:(q + 1) * 512], in_=pb[7][:]), r=[pb[7]], w=[W1])
                    yield
                for g in range(16):
                    k.op("dve", lambda e, g=g: e.max(out=vals[:, g, 0:8], in_=S_v[:, g, :]), r=[W1], w=[vals])
                    k.op("dve", lambda e, g=g: e.match_replace(out=S2_v[:, g, :], in_to_replace=vals[:, g, 0:8], in_values=S_v[:, g, :], imm_value=NEG),
                         r=[W1, vals], w=[W2])
                    k.op("dve", lambda e, g=g: e.max(out=vals[:, g, 8:16], in_=S2_v[:, g, :]), r=[W2], w=[vals])
                    k.op("dve", lambda e, g=g: e.max_index(out=idxu[:, g, 0:8], in_max=vals[:, g, 0:8], in_values=S_v[:, g, :]), r=[W1, vals], w=[idxu])
                    k.op("dve", lambda e, g=g: e.max_index(out=idxu[:, g, 8:16], in_max=vals[:, g, 8:16], in_values=S2_v[:, g, :]), r=[W2, vals], w=[idxu])
                    yield
                k.op("dve", lambda e: e.tensor_copy(out=idxf[:], in_=idxu[:]), r=[idxu], w=[idxf])
                k.op("dve", lambda e: e.tensor_tensor(out=W1[:].rearrange("p (h a b) -> p h a b", a=16, b=16),
                                                      in0=vals_v[:, :, 0, :].unsqueeze(3).to_broadcast([128, 8, 16, 16]),
                                                      in1=vals_v[:, :, 1, :].unsqueeze(2).to_broadcast([128, 8, 16, 16]), op=ALU.add), r=[vals], w=[W1])
                yield
                for h in range(8):
                    k.op("dve", lambda e, h=h: e.max(out=tv[:, h, 0:8], in_=cand_v[:, h, :]), r=[W1], w=[tv])
                    k.op("dve", lambda e, h=h: e.match_replace(out=cand2_v[:, h, :], in_to_replace=tv[:, h, 0:8], in_values=cand_v[:, h, :], imm_value=NEG),
                         r=[W1, tv], w=[W2])
                    k.op("dve", lambda e, h=h: e.max(out=tv[:, h, 8:16], in_=cand2_v[:, h, :]), r=[W2], w=[tv])
                    k.op("dve", lambda e, h=h: e.max_index(out=posu[:, h, 0:8], in_max=tv[:, h, 0:8], in_values=cand_v[:, h, :]), r=[W1, tv], w=[posu])
                    k.op("dve", lambda e, h=h: e.max_index(out=posu[:, h, 8:16], in_max=tv[:, h, 8:16], in_values=cand2_v[:, h, :]), r=[W2, tv], w=[posu])
                    yield
                k.op("dve", lambda e: e.tensor_tensor(out=gate[:], in0=tv[:], in1=tv[:, :, 0:1].to_broadcast([128, 8, 16]), op=ALU.subtract), r=[tv], w=[gate])
                k.op("act", lambda e: e.activation(out=gate[:], in_=gate[:], func=AF.Exp), r=[gate], w=[gate])
                k.op("dve", lambda e: e.tensor_reduce(out=ssum[:], in_=gate[:], axis=AX.X, op=ALU.add), r=[gate], w=[ssum])
                k.op("dve", lambda e: e.reciprocal(out=ssum[:], in_=ssum[:]), r=[ssum], w=[ssum])
                k.op("dve", lambda e: e.tensor_tensor(out=gate[:], in0=gate[:], in1=ssum[:].unsqueeze(2).to_broadcast([128, 8, 16]), op=ALU.mult), r=[gate, ssum], w=[gate])
                yield
                k.op("dve", lambda e: e.tensor_single_scalar(out=pau[:], in_=posu[:], scalar=4, op=ALU.logical_shift_right), r=[posu], w=[pau])
                k.op("dve", lambda e: e.tensor_single_scalar(out=pbu[:], in_=posu[:], scalar=15, op=ALU.bitwise_and), r=[posu], w=[pbu])
                k.op("dve", lambda e: e.tensor_copy(out=paf[:], in_=pau[:]), r=[pau], w=[paf])
                k.op("dve", lambda e: e.tensor_copy(out=pbf[:], in_=pbu[:]), r=[pbu], w=[pbf])
                yield
                for w_, pf in ((0, paf), (1, pbf)):
                    k.op("dve", lambda e: e.tensor_tensor(out=oh_v, in0=pf[:].unsqueeze(3).to_broadcast([128, 8, 16, 16]),
                                                          in1=io128[:, 0:16].unsqueeze(1).unsqueeze(1).to_broadcast([128, 8, 16, 16]), op=ALU.is_equal),
                         r=[pf, io128], w=[W1])
                    yield
                    k.op("dve", lambda e: e.tensor_tensor(out=oh_v, in0=oh_v, in1=idxf_v[:, :, w_, :].unsqueeze(2).to_broadcast([128, 8, 16, 16]), op=ALU.mult),
                         r=[W1, idxf], w=[W1])
                    yield
                    k.op("dve", lambda e: e.tensor_reduce(out=iij[:, w_, :].rearrange("p (h q) -> p h q", q=16), in_=oh_v, axis=AX.X, op=ALU.add), r=[W1], w=[iij])
                    yield
                tr_ = trT2[par][a]
                k.op("pe", lambda e: e.transpose(out=pb[7][:, 0:128], in_=iij[:, 0, :], identity=ident_f[:]), r=[iij, ident_f], w=[pb[7]])
                k.op("pe", lambda e: e.transpose(out=pb[7][:, 128:256], in_=iij[:, 1, :], identity=ident_f[:]), r=[iij, ident_f], w=[pb[7]])
                k.op("pe", lambda e: e.transpose(out=pb[7][:, 256:384], in_=gate[:].rearrange("p h q -> p (h q)"), identity=ident_f[:]), r=[gate, ident_f], w=[pb[7]])
                k.op("act", lambda e: e.copy(out=tr_[:].rearrange("p a t -> p (a t)"), in_=pb[7][:, 0:384]), r=[pb[7]], w=[tr_])
                if "PKs" in dbg and b_ == 0 and a == 0:
                    k.dma("sp", d_PK[:, :, :], tr_[:], r=[tr_], w=[d_PK])
                yield

        def drain(gen):
            if gen is not None:
                for _ in gen:
                    pass

        drain(gen_5b1(blist[0], 0))
        for bidx, b_ in enumerate(blist):
            par = bidx % 2
            t0 = b_ * TB5
            k.dma("sp", u2b_[:], d_U2T[:, t0:t0 + TB5].rearrange("(kk p) t -> p kk t", p=128), r=[d_U2T], w=[u2b_])
            for a in range(TB5 // 128):
                trT = trT2[par][a]
                k.op("act", lambda e: e.copy(out=trTb[:], in_=trT[:]), r=[trT], w=[trTb])
                for hq in range(4):
                    hs_ = slice(hq * 32, (hq + 1) * 32)
                    Bq = Boh2[hq % 2]
                    Aq = Aoh2[hq % 2]
                    k.op("dve", lambda e: e.tensor_tensor(out=Bq[:], in0=io128b[:].unsqueeze(1).to_broadcast([128, 32, 128]),
                                                          in1=trTb[:, 1, hs_].unsqueeze(2).to_broadcast([128, 32, 128]), op=ALU.is_equal), r=[io128b, trTb], w=[Bq])
                    k.op("dve", lambda e: e.tensor_tensor(out=Aq[:], in0=io128b[:].unsqueeze(1).to_broadcast([128, 32, 128]),
                                                          in1=trTb[:, 0, hs_].unsqueeze(2).to_broadcast([128, 32, 128]), op=ALU.is_equal), r=[io128b, trTb], w=[Aq])
                    k.op("dve", lambda e: e.tensor_tensor(out=Aq[:], in0=Aq[:], in1=trTb[:, 2, hs_].unsqueeze(2).to_broadcast([128, 32, 128]), op=ALU.mult),
                         r=[Aq, trTb], w=[Aq])
                    for tq in range(8):
                        wp = pb[6 + tq % 2]
                        for t4 in range(4):
                            tl = tq * 4 + t4
                            k.op("pe", lambda e, t4=t4, tl=tl: e.matmul(wp[:, t4 * 128:(t4 + 1) * 128], lhsT=Bq[:, tl, :], rhs=Aq[:, tl, :], start=True, stop=True),
                                 r=[Bq, Aq], w=[wp])
                        tk0 = a * 128 + hq * 32 + tq * 4
                        ov = Wt[:, :, tk0:tk0 + 4].rearrange("j i t -> j t i")
                        iv_ = wp[:].rearrange("j (t i) -> j t i", i=128)
                        k.op("act", lambda e: e.copy(out=ov, in_=iv_), r=[wp], w=[Wt])
            gen = gen_5b1(blist[bidx + 1], 1 - par) if bidx + 1 < len(blist) else None
            pend = []
            for ig in range(64):
                j = ig % 3
                k.dma("sp", ut[j][:], d_UT_v[:, ig * 2:(ig + 1) * 2, :], r=[d_UT], w=[ut[j]])
                k.dma("sp", vb[j][:], d_VB_v[:, ig * 2:(ig + 1) * 2, :], r=[d_VB], w=[vb[j]])
                for q in range(2):
                    i = ig * 2 + q
                    aps = pb[4 + i % 3]
                    for kk in range(8):
                        k.op("pe", lambda e, kk=kk: e.matmul(aps[:, 0:TB5], lhsT=ut[j][:, q, kk * 128:(kk + 1) * 128], rhs=u2b_[:, kk, :], start=(kk == 0), stop=(kk == 7)),
                             r=[ut[j], u2b_], w=[aps])
                    k.op("act", lambda e: e.activation(out=Gf[i % 4][:], in_=aps[:, 0:TB5], func=AF.Gelu), r=[aps], w=[Gf[i % 4]])
                    k.op("dve", lambda e: e.tensor_tensor(out=Gw[i % 4][:], in0=Gf[i % 4][:], in1=Wt[:, i, :], op=ALU.mult), r=[Gf[i % 4], Wt], w=[Gw[i % 4]])
                    pend.append((i, j, q))
                    while len(pend) > (0 if i == 127 else 2):
                        i2, j2, q2 = pend.pop(0)
                        for a in range(2):
                            for hf in range(2):
                                k.op("pe", lambda e, a=a, hf=hf: e.matmul(pb[a * 2 + hf][:], lhsT=Gw[i2 % 4][:, a * 128:(a + 1) * 128], rhs=vb[j2][:, q2, hf * 512:(hf + 1) * 512],
                                                                          start=(i2 == 0), stop=(i2 == 127)), r=[Gw[i2 % 4], vb[j2]], w=[pb[a * 2 + hf]])
                if gen is not None:
                    for _ in range(2):
                        try:
                            next(gen)
                        except StopIteration:
                            gen = None
                            break
            drain(gen)
            for a in range(2):
                r0 = t0 + a * 128
                k.dma("sp", x1l[:], d_X1[r0:r0 + 128, :], r=[d_X1], w=[x1l])
                if "FFNs" in dbg and b_ in (0, nblk5 - 1) and a == (0 if b_ == 0 else 1):
                    for hf in range(2):
                        k.op("dve", lambda e, hf=hf: e.tensor_copy(out=ffs[:, hf * 512:(hf + 1) * 512], in_=pb[a * 2 + hf][:]), r=[pb[a * 2 + hf]], w=[ffs])
                    o_ = 0 if b_ == 0 else 128
                    k.dma("sp", d_FFNs[o_:o_ + 128, :], ffs[:], r=[ffs], w=[d_FFNs])
                for hf in range(2):
                    k.op("dve", lambda e, hf=hf: e.tensor_tensor(out=t5[:, hf * 512:(hf + 1) * 512], in0=pb[a * 2 + hf][:], in1=g2b[:, hf * 512:(hf + 1) * 512], op=ALU.mult),
                         r=[pb[a * 2 + hf], g2b], w=[t5])
                k.op("dve", lambda e: e.scalar_tensor_tensor(out=xr5[:], in0=x1l[:], scalar=DN_ALPHA, in1=t5[:], op0=ALU.mult, op1=ALU.add), r=[x1l, t5], w=[xr5])
                ln_sb(xr5, t5[:], t5, st6_, mv_, rstd_, nmr_)
                k.op("pool", lambda e: e.tensor_tensor(out=t5[:], in0=t5[:], in1=l2g[:], op=ALU.mult), r=[t5, l2g], w=[t5])
                k.op("pool", lambda e: e.tensor_tensor(out=xr5[:], in0=t5[:], in1=l2b[:], op=ALU.add), r=[t5, l2b], w=[xr5])
                k.dma("sp", out[r0:r0 + 128, :], xr5[:], r=[xr5], w=[out_buf], is_out=True)
            k.barrier()
        k.pop()
    if "Q2s" in dbg:
        dd = scratch("Q2s", [2048, 256], BF16)
        k.dma("sp", dd[:, 0:128], d_Q2[:, 0:128], r=[d_Q2], w=[dd])
        k.dma("sp", dd[:, 128:256], d_Q2[:, T - 128:T], r=[d_Q2], w=[dd])
    if "UTs" in dbg:
        dd = scratch("UTs", [2, 128, 1024], BF16)
        k.dma("sp", dd[0], d_UT[0], r=[d_UT], w=[dd])
        k.dma("sp", dd[1], d_UT[127], r=[d_UT], w=[dd])
    for nm_, src_ in (("OFs", d_OF), ("OBs", d_OB)):
        if nm_ in dbg:
            dd = scratch(nm_, [1024, 256])
            k.dma("sp", dd[:, 0:128], src_[:, 0:128], r=[src_], w=[dd])
            k.dma("sp", dd[:, 128:256], src_[:, T - 128:T], r=[src_], w=[dd])
    k.finish()
    es.close()
    return nc


def make_in_maps(inputs):
    cons = make_consts()
    maps = []
    for b in range(8):
        cv = np.stack([feat(inputs["c"][b]), feat(inputs["c_ctx"])], axis=-1)
        m = {
            "x": np.ascontiguousarray(inputs["x"][b]),
            "ctx": np.ascontiguousarray(inputs["ctx"][b]),
            "cvec": np.ascontiguousarray(cv),
            "w_ada": np.ascontiguousarray(inputs["w_ada"][0]),
            "b_ada": feat(inputs["b_ada"][0]),
            "w_in": np.ascontiguousarray(inputs["w_in"][0]),
            "ident": cons["ident"],
            "hg_lb": np.ascontiguousarray(inputs["hg_lb_logits"][:, :, :]),
            "hg_nw": np.ascontiguousarray(inputs["hg_norm_w"][0].reshape(128, 1)),
            "conv_w": np.ascontiguousarray(inputs["ma_conv_w"][0].reshape(32, 128, 5).transpose(1, 0, 2)),
            "conv_b": feat(inputs["ma_conv_b"][0]),
            "ma_nw": np.ascontiguousarray(inputs["ma_norm_w"][0]),
            "w_ba": np.ascontiguousarray(inputs["w_branch_a"][0]), "w_bb": np.ascontiguousarray(inputs["w_branch_b"][0]),
            "w_o": np.ascontiguousarray(inputs["w_out"][0]),
            "ln1_g": np.ascontiguousarray(inputs["ln1_g"][0]), "ln1_b": np.ascontiguousarray(inputs["ln1_b"][0]),
            "peer_wq": np.ascontiguousarray(inputs["peer_wq"][0]),
            "peer_sub": np.ascontiguousarray(inputs["peer_subkeys"][0].reshape(16, 128, 128)),
            "peer_u": np.ascontiguousarray(inputs["peer_u"][0]), "peer_v": np.ascontiguousarray(inputs["peer_v"][0]),
            "ln2_g": np.ascontiguousarray(inputs["ln2_g"][0]), "ln2_b": np.ascontiguousarray(inputs["ln2_b"][0]),
            "iota_c": np.arange(128, dtype=np.float32),
            "ssd_vec": np.ascontiguousarray(np.stack([inputs["ma_dt_bias"][0].reshape(64), inputs["ma_a_log"][0].reshape(64),
                                                      np.concatenate([inputs["ma_d"][0], np.zeros(32, np.float32)])]).astype(np.float32)),
            "incl_f": cons["incl_f"], "incl_b": cons["incl_b"], "strict_f": cons["strict_f"], "strict_b": cons["strict_b"],
            "incl_f128": cons["incl_f128"], "incl_b128": cons["incl_b128"], "strict_f128": cons["strict_f128"], "strict_b128": cons["strict_b128"],
        }
        maps.append(m)
    return maps


def kernel(**inputs):
    inputs = {k_: np.asarray(v) for k_, v in inputs.items()}
    nc = build()
    maps = make_in_maps(inputs)
    res = run_bass_kernel_spmd(nc, maps, core_ids=list(range(8)))
    return np.stack([r["out"] for r in res.results], axis=0)
```
